# Optimizing a Trainium2 kernel written in Bass

```python
import math
import jax, jax.numpy as jnp
from jax import lax
import numpy as np

D_MODEL = 2048
BATCH = 4
SEQ = 4096
DEPTH = 1
DEC_BATCH = 16
DEC_SEQ = 64
PAST_LEN = 4096

CHUNK = 64
D_MIX = D_MODEL
D_LRU = D_MIX // 2
LRU_HEADS = 8
LRU_BLOCK = D_LRU // LRU_HEADS
CONV_W = 4
LRU_C = 8.0
D_ATT = D_MIX - D_LRU
N_HEADS = 8
HEAD_DIM = D_ATT // N_HEADS
IDX_HEADS = 16
IDX_DIM = 64
TOPK_MAX = 256
QBLOCK = 128
N_BUCKETS = 32
MAX_DIST = 128
PEER_HEADS = 8
N_KEYS = 128
N_EXPERTS = N_KEYS * N_KEYS
PEER_DK = 128
PEER_TOPK = 16
PEER_TBLOCK = 128
EPS = 1e-6

O_Y = D_LRU
O_Q = 2 * D_LRU
O_K = O_Q + D_ATT
O_V = O_K + D_ATT
O_QI = O_V + D_ATT
O_KI = O_QI + IDX_HEADS * IDX_DIM
O_WI = O_KI + IDX_DIM
D_IN = O_WI + IDX_HEADS

kernel_name = 'hymba_rglru_dsa_peer_stream_step'

F32 = jnp.float32


def rmsnorm(x, g):
    xf = x.astype(F32)
    return xf * lax.rsqrt(jnp.mean(xf * xf, axis=-1, keepdims=True) + EPS) * g.astype(F32)


def t5_bucket(rel):
    half = N_BUCKETS // 2
    max_exact = half // 2
    ret = jnp.where(rel > 0, half, 0)
    n = jnp.abs(rel)
    nf = jnp.maximum(n, 1).astype(F32)
    large = max_exact + (jnp.log(nf / max_exact) / math.log(MAX_DIST / max_exact)
                         * (half - max_exact)).astype(jnp.int32)
    large = jnp.minimum(large, half - 1)
    return ret + jnp.where(n < max_exact, n, large)


def causal_conv(x, buf, w, b):
    T = x.shape[1]
    xp = jnp.concatenate([buf.astype(x.dtype), x], axis=1)
    xf = xp.astype(F32)
    wf = w.astype(F32)
    y = b.astype(F32) + sum(xf[:, j:j + T] * wf[j] for j in range(CONV_W))
    return y, xp[:, -(CONV_W - 1):]


def rg_lru(xc, h0, pos, wa, ba, wx, bx, lam):
    B, T, _ = xc.shape
    xb = xc.reshape(B, T, LRU_HEADS, LRU_BLOCK)
    r = jax.nn.sigmoid(jnp.einsum('bthi,hij->bthj', xb, wa.astype(F32)).reshape(B, T, D_LRU) + ba.astype(F32))
    i = jax.nn.sigmoid(jnp.einsum('bthi,hij->bthj', xb, wx.astype(F32)).reshape(B, T, D_LRU) + bx.astype(F32))
    log_a = -LRU_C * r * jax.nn.softplus(-lam.astype(F32))
    a = jnp.exp(log_a)
    mult = jnp.sqrt(-jnp.expm1(2.0 * log_a))
    mult = jnp.where((pos == 0)[None, :, None], 1.0, mult)
    b = mult * (i * xc)
    b = b.at[:, 0].add(a[:, 0] * h0.astype(F32))

    def combine(left, right):
        a_l, b_l = left
        a_r, b_r = right
        return a_l * a_r, a_r * b_l + b_r

    _, h = lax.associative_scan(combine, (a, b), axis=1)
    return h, h[:, -1]


def dsa_attention(q, k, v, qi, wi, ki, q_pos, k_pos, rel_bias, topk):
    B, Tq = q.shape[:2]
    qb = min(QBLOCK, Tq)
    nb = Tq // qb
    k_chunk = k_pos // CHUNK
    bias_table = rel_bias.astype(F32)

    def blocks(a):
        return a.reshape((B, nb, qb) + a.shape[2:]).swapaxes(0, 1)

    def one_block(args):
        q_b, qi_b, wi_b, pos_b = args
        q_chunk = pos_b // CHUNK
        sc = jnp.einsum('bqhd,bsd->bqhs', qi_b.astype(F32), ki.astype(F32)) * IDX_DIM ** -0.5
        score = jnp.einsum('bqhs,bqh->bqs', jax.nn.relu(sc), wi_b.astype(F32)) * IDX_HEADS ** -0.5
        allowed = k_chunk[None, :] <= q_chunk[:, None]
        score = jnp.where(allowed[None], score, -jnp.inf)
        _, sel = lax.top_k(score, topk)
        sel_pos = k_pos[sel]
        valid = (sel_pos // CHUNK) <= q_chunk[None, :, None]
        k_sel = jax.vmap(lambda kk, ss: kk[ss])(k, sel)
        v_sel = jax.vmap(lambda vv, ss: vv[ss])(v, sel)
        logits = jnp.einsum('bqhd,bqkhd->bqhk', q_b.astype(F32), k_sel.astype(F32)) * HEAD_DIM ** -0.5
        bias = bias_table[t5_bucket(sel_pos - pos_b[None, :, None])]
        logits = logits + jnp.moveaxis(bias, -1, 2)
        logits = jnp.where(valid[:, :, None, :], logits, -jnp.inf)
        p = jax.nn.softmax(logits, axis=-1)
        return jnp.einsum('bqhk,bqkhd->bqhd', p, v_sel.astype(F32))

    out = lax.map(one_block, (blocks(q), blocks(qi), blocks(wi), q_pos.reshape(nb, qb)))
    return out.swapaxes(0, 1).reshape(B, Tq, N_HEADS * HEAD_DIM)


def peer(h, wq, sub_keys, u, v):
    B, T, D = h.shape
    n = B * T
    nblk = -(-n // PEER_TBLOCK)
    flat = jnp.pad(h.reshape(n, D), ((0, nblk * PEER_TBLOCK - n), (0, 0)))
    keys = sub_keys.astype(F32)

    def one_block(xb):
        xf = xb.astype(F32)
        q = (xb @ wq).astype(F32).reshape(PEER_TBLOCK, PEER_HEADS, 2, PEER_DK // 2)
        s1 = jnp.einsum('thd,hnd->thn', q[:, :, 0], keys[0])
        s2 = jnp.einsum('thd,hnd->thn', q[:, :, 1], keys[1])
        v1, i1 = lax.top_k(s1, PEER_TOPK)
        v2, i2 = lax.top_k(s2, PEER_TOPK)
        cand = (v1[..., :, None] + v2[..., None, :]).reshape(PEER_TBLOCK, PEER_HEADS, PEER_TOPK * PEER_TOPK)
        cand_idx = (i1[..., :, None] * N_KEYS + i2[..., None, :]).reshape(PEER_TBLOCK, PEER_HEADS, PEER_TOPK * PEER_TOPK)
        top_s, top_j = lax.top_k(cand, PEER_TOPK)
        eidx = jnp.take_along_axis(cand_idx, top_j, axis=-1)
        g = jax.nn.softmax(top_s, axis=-1)
        act = jax.nn.gelu(jnp.einsum('td,thkd->thk', xf, u[eidx].astype(F32)))
        return jnp.einsum('thk,thkd->td', g * act, v[eidx].astype(F32))

    out = lax.map(one_block, flat.reshape(nblk, PEER_TBLOCK, D))
    return out.reshape(nblk * PEER_TBLOCK, D)[:n].reshape(B, T, D)


def hybrid_layer(x, c, past_k, past_v, past_ki, h0, conv_buf,
                 w_ada, b_ada, g1, g2, w_in, conv_w, conv_b,
                 lru_wa, lru_ba, lru_wx, lru_bx, lru_lam, w_out,
                 peer_wq, peer_keys, peer_u, peer_v, rel_bias):
    B, T, _ = x.shape
    past = 0 if past_k is None else past_k.shape[1]
    q_pos = past + jnp.arange(T, dtype=jnp.int32)
    if h0 is None:
        h0 = jnp.zeros((B, D_LRU), F32)
        conv_buf = jnp.zeros((B, CONV_W - 1, D_LRU), x.dtype)

    mod = (jax.nn.silu(c.astype(F32)) @ w_ada.astype(F32) + b_ada.astype(F32)).reshape(B, 6, D_MODEL)
    sh1, sc1, gt1, sh2, sc2, gt2 = [mod[:, j, None, :] for j in range(6)]

    hn = (rmsnorm(x, g1) * (1.0 + sc1) + sh1).astype(x.dtype)
    z = hn @ w_in
    xr, yr, q, k, v, qi, ki, wi = jnp.split(z, [O_Y, O_Q, O_K, O_V, O_QI, O_KI, O_WI], axis=-1)

    xc, new_buf = causal_conv(xr, conv_buf, conv_w, conv_b)
    h, h_last = rg_lru(xc, h0, q_pos, lru_wa, lru_ba, lru_wx, lru_bx, lru_lam)
    a_out = h * jax.nn.gelu(yr.astype(F32))

    q = q.reshape(B, T, N_HEADS, HEAD_DIM)
    k = k.reshape(B, T, N_HEADS, HEAD_DIM)
    v = v.reshape(B, T, N_HEADS, HEAD_DIM)
    qi = qi.reshape(B, T, IDX_HEADS, IDX_DIM)
    if past_k is None:
        k_all, v_all, ki_all = k, v, ki
    else:
        k_all = jnp.concatenate([past_k.astype(k.dtype), k], axis=1)
        v_all = jnp.concatenate([past_v.astype(v.dtype), v], axis=1)
        ki_all = jnp.concatenate([past_ki.astype(ki.dtype), ki], axis=1)
    S = past + T
    k_pos = jnp.arange(S, dtype=jnp.int32)
    att = dsa_attention(q, k_all, v_all, qi, wi, ki_all, q_pos, k_pos, rel_bias, min(TOPK_MAX, S // 4))

    mix = (jnp.concatenate([a_out, att], axis=-1).astype(x.dtype) @ w_out).astype(F32)
    x = (x.astype(F32) + gt1 * mix).astype(x.dtype)

    hn2 = (rmsnorm(x, g2) * (1.0 + sc2) + sh2).astype(x.dtype)
    x = (x.astype(F32) + gt2 * peer(hn2, peer_wq, peer_keys, peer_u, peer_v)).astype(x.dtype)
    return x, k, v, ki, h_last.astype(x.dtype), new_buf


def setup_inputs(seed: int = 0) -> dict:
    key = jax.random.key(seed)
    ks = jax.random.split(key, 32)

    def nrm(k, shape, s):
        return jax.random.normal(k, shape, jnp.float32) * s

    a_c = jax.random.uniform(ks[19], (DEPTH, D_LRU), jnp.float32, 0.9, 0.999)
    s_lam = a_c ** (1.0 / LRU_C)
    return {
        'x_prompt': nrm(ks[0], (BATCH, SEQ, D_MODEL), 1.0),
        'x_sample': nrm(ks[1], (DEC_BATCH, DEC_SEQ, D_MODEL), 1.0),
        'cache_k': nrm(ks[2], (DEPTH, DEC_BATCH, PAST_LEN, N_HEADS, HEAD_DIM), 1.0),
        'cache_v': nrm(ks[3], (DEPTH, DEC_BATCH, PAST_LEN, N_HEADS, HEAD_DIM), 1.0),
        'cache_kidx': nrm(ks[4], (DEPTH, DEC_BATCH, PAST_LEN, IDX_DIM), 1.0),
        'state_lru': nrm(ks[5], (DEPTH, DEC_BATCH, D_LRU), 0.5),
        'state_conv': nrm(ks[6], (DEPTH, DEC_BATCH, CONV_W - 1, D_LRU), 1.0),
        'c_prompt': nrm(ks[7], (BATCH, D_MODEL), 1.0),
        'c_sample': nrm(ks[8], (DEC_BATCH, D_MODEL), 1.0),
        'w_ada': nrm(ks[9], (DEPTH, D_MODEL, 6 * D_MODEL), 0.5 * D_MODEL ** -0.5),
        'b_ada': nrm(ks[10], (DEPTH, 6 * D_MODEL), 0.1),
        'g_norm1': 1.0 + nrm(ks[11], (DEPTH, D_MODEL), 0.05),
        'g_norm2': 1.0 + nrm(ks[12], (DEPTH, D_MODEL), 0.05),
        'w_in': nrm(ks[13], (DEPTH, D_MODEL, D_IN), D_MODEL ** -0.5),
        'conv_w': nrm(ks[14], (DEPTH, CONV_W, D_LRU), CONV_W ** -0.5),
        'conv_b': nrm(ks[15], (DEPTH, D_LRU), 0.02),
        'lru_wa': nrm(ks[16], (DEPTH, LRU_HEADS, LRU_BLOCK, LRU_BLOCK), LRU_BLOCK ** -0.5),
        'lru_ba': nrm(ks[17], (DEPTH, D_LRU), 0.1),
        'lru_wx': nrm(ks[18], (DEPTH, LRU_HEADS, LRU_BLOCK, LRU_BLOCK), LRU_BLOCK ** -0.5),
        'lru_bx': nrm(ks[20], (DEPTH, D_LRU), 0.1),
        'lru_lam': jnp.log(s_lam) - jnp.log1p(-s_lam),
        'w_out': nrm(ks[21], (DEPTH, D_MIX, D_MODEL), D_MIX ** -0.5),
        'peer_wq': nrm(ks[22], (DEPTH, D_MODEL, PEER_HEADS * PEER_DK), D_MODEL ** -0.5),
        'peer_keys': nrm(ks[23], (DEPTH, 2, PEER_HEADS, N_KEYS, PEER_DK // 2), (PEER_DK // 2) ** -0.5),
        'peer_u': nrm(ks[24], (DEPTH, N_EXPERTS, D_MODEL), D_MODEL ** -0.5),
        'peer_v': nrm(ks[25], (DEPTH, N_EXPERTS, D_MODEL), PEER_HEADS ** -0.5),
        'rel_bias': nrm(ks[26], (N_BUCKETS, N_HEADS), 0.5),
        'g_final': 1.0 + nrm(ks[27], (D_MODEL,), 0.05),
    }


def reference(x_prompt, x_sample, cache_k, cache_v, cache_kidx, state_lru, state_conv,
              c_prompt, c_sample, w_ada, b_ada, g_norm1, g_norm2, w_in, conv_w, conv_b,
              lru_wa, lru_ba, lru_wx, lru_bx, lru_lam, w_out, peer_wq, peer_keys,
              peer_u, peer_v, rel_bias, g_final):
    xp, xs = x_prompt, x_sample
    st_p = [[], [], [], [], []]
    st_s = [[], [], [], [], []]
    for l in range(DEPTH):
        lw = (w_ada[l], b_ada[l], g_norm1[l], g_norm2[l], w_in[l], conv_w[l], conv_b[l],
              lru_wa[l], lru_ba[l], lru_wx[l], lru_bx[l], lru_lam[l], w_out[l],
              peer_wq[l], peer_keys[l], peer_u[l], peer_v[l], rel_bias)
        xp, *new_p = hybrid_layer(xp, c_prompt, None, None, None, None, None, *lw)
        xs, *new_s = hybrid_layer(xs, c_sample, cache_k[l], cache_v[l], cache_kidx[l],
                                  state_lru[l], state_conv[l], *lw)
        for lst, a in zip(st_p, new_p):
            lst.append(a)
        for lst, a in zip(st_s, new_s):
            lst.append(a)
    y_prompt = rmsnorm(xp, g_final).astype(x_prompt.dtype)
    y_sample = rmsnorm(xs, g_final).astype(x_sample.dtype)
    k_p, v_p, ki_p, lru_p, conv_p = [jnp.stack(a) for a in st_p]
    k_s, v_s, ki_s, lru_s, conv_s = [jnp.stack(a) for a in st_s]
    return (y_prompt, y_sample, k_p, v_p, ki_p, lru_p, conv_p, k_s, v_s, ki_s, lru_s, conv_s)
```

```python
import math
import os
from contextlib import ExitStack

import numpy as np
import concourse.bass as bass
import concourse.mybir as mybir
from concourse.bass_utils import run_bass_kernel_spmd

F32 = mybir.dt.float32
BF16 = mybir.dt.bfloat16
I32 = mybir.dt.int32
U32 = mybir.dt.uint32
AF = mybir.ActivationFunctionType
ALU = mybir.AluOpType
AX = mybir.AxisListType

D = 2048
KC = 16
DIN = 6224
NEG = -1.0e30
EPS = 1e-6
NIT = 20
PW = 256
SAME_ENGINE_SYNC = os.environ.get("SES", "1") == "1"


class Buf:
    __slots__ = ("w", "r", "x")

    def __init__(self, x=False):
        self.w = None
        self.r = []
        self.x = x


class Sched:
    ENG = ("pe", "act", "dve", "pool", "sp")

    def __init__(self, n_lanes=10):
        self.q = {e: [] for e in self.ENG}
        self.cnt = {e: 0 for e in self.ENG}
        self.known = {e: {} for e in self.ENG}
        self.n_lanes = n_lanes
        self.lane_val = {}
        self.lane_next = {"sp": 0, "act": 0, "pool": 0}
        self.ninst = 0

    def semkey_list(self):
        keys = list(self.ENG[:4])
        for qn in ("sp", "act", "pool"):
            for l in range(self.n_lanes):
                keys.append(("lane", qn, l))
        return keys

    def _deps(self, eng, reads, writes):
        need = {}

        def add(d):
            if d is None:
                return
            k, v = d
            if k == eng and (eng == "pe" or not SAME_ENGINE_SYNC):
                return
            if need.get(k, 0) < v:
                need[k] = v

        for b in reads:
            add(b.w)
            if b.x:
                for d in b.r:
                    if d[0] != eng:
                        add(d)
        for b in writes:
            add(b.w)
            for d in b.r:
                add(d)
        out = []
        kn = self.known[eng]
        for k, v in need.items():
            if kn.get(k, 0) >= v:
                continue
            kn[k] = v
            out.append((k, v))
        return out

    def op(self, eng, fn, reads=(), writes=()):
        waits = self._deps(eng, reads, writes)
        self.cnt[eng] += 1
        v = self.cnt[eng]
        self.q[eng].append((waits, fn, (eng, 1)))
        for b in reads:
            b.r.append((eng, v))
        for b in writes:
            b.w = (eng, v)
            b.r = []
        self.ninst += 1

    def dma(self, qeng, fn, reads=(), writes=()):
        lane = self.lane_next[qeng]
        self.lane_next[qeng] = (lane + 1) % self.n_lanes
        key = ("lane", qeng, lane)
        waits = self._deps(qeng, reads, writes)
        prev = self.lane_val.get(key, 0)
        if prev > 0 and self.known[qeng].get(key, 0) < prev:
            self.known[qeng][key] = prev
            waits.append((key, prev))
        v = prev + 16
        self.lane_val[key] = v
        self.q[qeng].append((waits, fn, (key, 16)))
        for b in reads:
            b.r.append((key, v))
        for b in writes:
            b.w = (key, v)
            b.r = []
        self.ninst += 1

    def final_waits(self, eng, bufs):
        waits = {}
        for b in bufs:
            if b.w is not None:
                k, v = b.w
                waits[k] = max(waits.get(k, 0), v)
        self.q[eng].append((list(waits.items()), None, None))

    def emit(self, eng, eobj, semmap):
        for waits, fn, inc in self.q[eng]:
            for k, v in waits:
                eobj.wait_ge(semmap[k], v)
            if fn is None:
                continue
            ins = fn(eobj)
            ins.then_inc(semmap[inc[0]], inc[1])


def make_cfg(full=True):
    if full:
        return dict(NCT=16, NOT=16, PAST=4096, GT=2, TOPK_P=256, TOPK_S=256)
    return dict(NCT=2, NOT=2, PAST=512, GT=2, TOPK_P=128, TOPK_S=144)


def build(cfg):
    NCT, NOT, PAST, GT = cfg["NCT"], cfg["NOT"], cfg["PAST"], cfg["GT"]
    SP_MAX = (NCT + NOT) * 128
    SS_MAX = PAST + 128
    SMAXT = max(SP_MAX, SS_MAX)
    AW = max(SMAXT, 4224)
    nc = bass.Bass("TRN2", target_bir_lowering=False)
    S = Sched()
    es = ExitStack()

    def din(name, shape, dt=F32):
        return nc.dram_tensor(name, list(shape), dt, kind="ExternalInput")

    def dout(name, shape, dt=F32):
        return nc.dram_tensor(name, list(shape), dt, kind="ExternalOutput")

    def dscr(name, shape, dt):
        return nc.dram_tensor(name, list(shape), dt)

    _cnt = [0]

    def sb(shape, dt, name=None):
        _cnt[0] += 1
        return es.enter_context(nc.sbuf_tensor("sb%d_%s" % (_cnt[0], name or "t"), list(shape), dt))

    def ps(shape, dt, name=None):
        _cnt[0] += 1
        return es.enter_context(nc.psum_tensor("ps%d_%s" % (_cnt[0], name or "t"), list(shape), dt))

    x_ctx = din("x_ctx", [NCT * 128, D])
    x_own = din("x_own", [NOT * 128, D])
    xs_d = din("xs", [2, 64, D])
    ck_d = din("ck", [2, PAST, 1024])
    cv_d = din("cv", [2, PAST, 1024])
    cki_d = din("cki", [2, PAST, 64])
    stl_d = din("st_lru", [2, 128, 8])
    stc_d = din("st_conv", [2, 128, 8, 3])
    c3T_d = din("c3T", [128, KC * 3])
    flags_d = din("flags", [128, 4])
    wada_d = din("w_ada", [D, 6 * D])
    bada_d = din("b_ada", [1, 6 * D])
    g1T_d = din("g1T", [128, KC])
    g2T_d = din("g2T", [128, KC])
    win_d = din("w_in", [D, DIN])
    cw_d = din("conv_wT", [128, 4 * 8])
    cb_d = din("conv_bT", [128, 8])
    wa_d = din("lru_waT", [128, 8 * 128])
    wx_d = din("lru_wxT", [128, 8 * 128])
    ba_d = din("lru_baT", [128, 8])
    bx_d = din("lru_bxT", [128, 8])
    lam_d = din("lru_lamT", [128, 8])
    wout_d = din("w_out", [D, D])
    wq_d = din("peer_wq", [D, 1024])
    pk_d = din("peer_keysT", [128, 8 * 128])
    pu_d = din("peer_u", [16384, D])
    pv_d = din("peer_v", [16384, D])
    rb_d = din("rel_bias", [32, 8])
    gf_d = din("g_final", [1, D])
    oh_d = din("oh", [32, 383])

    y_own_d = dout("y_own", [NOT * 128, D])
    ys_d = dout("ys", [2, 64, D])
    k_own_d = dout("k_own", [NOT * 128, 1024])
    v_own_d = dout("v_own", [NOT * 128, 1024])
    ki_own_d = dout("ki_own", [NOT * 128, 64])
    lru_p_d = dout("lru_p", [128, 8])
    conv_p_d = dout("conv_p", [128, 8, 3])
    ks_d = dout("ks", [2, 64, 1024])
    vs_d = dout("vs", [2, 64, 1024])
    kis_d = dout("kis", [2, 64, 64])
    lru_s_d = dout("lru_s", [2, 128, 8])
    conv_s_d = dout("conv_s", [2, 128, 8, 3])
    OUTB = []

    def outbuf():
        b = Buf()
        OUTB.append(b)
        return b

    modrow_d = dscr("modrow", [3, 6 * D], F32)
    B_modrow = Buf()
    bsc_d = dscr("bsc", [8, 128, 383], F32)
    bv_d = dscr("bv", [8, 383], F32)

    def ACT(out, in_, func, R, W, scale=1.0, bias=0.0, accum_out=None):
        if accum_out is None:
            S.op("act", lambda e: e.activation(out=out, in_=in_, func=func, scale=scale, bias=bias), R, W)
        else:
            S.op("act", lambda e: e.activation(out=out, in_=in_, func=func, scale=scale, bias=bias, accum_out=accum_out), R, W)

    def MM(out, lhsT, rhs, st, sp_, R, W, skip=False):
        if skip:
            S.op("pe", lambda e: e.matmul(out, lhsT=lhsT, rhs=rhs, start=st, stop=sp_, skip_group_check=True), R, W)
        else:
            S.op("pe", lambda e: e.matmul(out, lhsT=lhsT, rhs=rhs, start=st, stop=sp_), R, W)

    def TR(out, in_, ident, R, W):
        S.op("pe", lambda e: e.transpose(out=out, in_=in_, identity=ident), R, W)

    def TS(eng, out, in0, s1, s2, op0, op1, R, W, accum_out=None):
        if op1 is None:
            if accum_out is None:
                S.op(eng, lambda e: e.tensor_scalar(out=out, in0=in0, scalar1=s1, scalar2=None, op0=op0), R, W)
            else:
                raise ValueError
        elif accum_out is None:
            S.op(eng, lambda e: e.tensor_scalar(out=out, in0=in0, scalar1=s1, scalar2=s2, op0=op0, op1=op1), R, W)
        else:
            S.op(eng, lambda e: e.tensor_scalar(out=out, in0=in0, scalar1=s1, scalar2=s2, op0=op0, op1=op1, accum_out=accum_out), R, W)

    def TT(eng, out, in0, in1, op, R, W):
        S.op(eng, lambda e: e.tensor_tensor(out=out, in0=in0, in1=in1, op=op), R, W)

    def STT(out, in0, scalar, in1, op0, op1, R, W, accum_out=None):
        if accum_out is None:
            S.op("dve", lambda e: e.scalar_tensor_tensor(out=out, in0=in0, scalar=scalar, in1=in1, op0=op0, op1=op1), R, W)
        else:
            S.op("dve", lambda e: e.scalar_tensor_tensor(out=out, in0=in0, scalar=scalar, in1=in1, op0=op0, op1=op1, accum_out=accum_out), R, W)

    def CP(eng, out, in_, R, W):
        if eng == "act":
            S.op("act", lambda e: e.copy(out=out, in_=in_), R, W)
        else:
            S.op(eng, lambda e: e.tensor_copy(out=out, in_=in_), R, W)

    def MS(eng, ap, val, W):
        S.op(eng, lambda e: e.memset(ap, val), (), W)

    def DMA(q, out, in_, R, W):
        S.dma(q, lambda e: e.dma_start(out=out, in_=in_), R, W)

    identf = sb([128, 128], F32, "identf"); B_identf = Buf()
    ident = sb([128, 128], BF16, "ident"); B_ident = Buf()
    MS("pool", identf[:], 0.0, [B_identf])
    S.op("pool", lambda e: e.affine_select(out=identf[:], in_=identf[:], pattern=[[-1, 128]], compare_op=ALU.not_equal,
                                           fill=1.0, base=0, channel_multiplier=1), [B_identf], [B_identf])
    CP("dve", ident[:], identf[:], [B_identf], [B_ident])
    iota256 = sb([128, 256], F32, "iota"); B_iota = Buf()
    S.op("pool", lambda e: e.iota(iota256[:], pattern=[[1, 256]], base=0, channel_multiplier=0,
                                  allow_small_or_imprecise_dtypes=True), (), [B_iota])
    pow2 = sb([128, NIT + 1], F32, "pow2"); B_pow2 = Buf()
    for k in range(NIT + 1):
        MS("pool", pow2[:, k:k + 1], 2.0 ** (-k), [B_pow2])
    flags = sb([128, 4], F32, "flags"); B_flags = Buf()
    DMA("sp", flags[:], flags_d.ap(), (), [B_flags])

    def ldc(dram, shape, name):
        t = sb(shape, F32, name); b = Buf()
        DMA("sp", t[:], dram.ap(), (), [b])
        return t, b

    cw, B_cw = ldc(cw_d, [128, 32], "cw")
    cb, B_cb = ldc(cb_d, [128, 8], "cb")
    ba, B_ba = ldc(ba_d, [128, 8], "ba")
    bx, B_bx = ldc(bx_d, [128, 8], "bx")
    lam, B_lam = ldc(lam_d, [128, 8], "lam")
    g1T, B_g1T = ldc(g1T_d, [128, KC], "g1T")
    g2T, B_g2T = ldc(g2T_d, [128, KC], "g2T")
    keysT, B_keysT = ldc(pk_d, [128, 1024], "keysT")
    gfin = sb([128, D], F32, "gfin"); B_gfin = Buf()
    DMA("sp", gfin[:], gf_d.ap()[0:1, :].partition_broadcast(128), (), [B_gfin])
    bfar = sb([128, 8], F32, "bfar"); B_bfar = Buf()
    DMA("sp", bfar[:], rb_d.ap()[15:16, :].partition_broadcast(128), (), [B_bfar])
    wa_b = sb([128, 1024], BF16, "wa_b"); B_wa = Buf()
    wx_b = sb([128, 1024], BF16, "wx_b"); B_wx = Buf()
    DMA("pool", wa_b[:], wa_d.ap(), (), [B_wa])
    DMA("pool", wx_b[:], wx_d.ap(), (), [B_wx])
    c1 = sb([128, 8], F32, "c1"); c2 = sb([128, 8], F32, "c2"); B_c12 = Buf()
    ACT(c1[:], lam[:], AF.Exp, [B_lam], [B_c12], scale=-1.0)
    ACT(c1[:], c1[:], AF.Ln, [B_c12], [B_c12], bias=1.0)
    TS("dve", c2[:], c1[:], -16.0, None, ALU.mult, None, [B_c12], [B_c12])
    TS("dve", c1[:], c1[:], -8.0, None, ALU.mult, None, [B_c12], [B_c12])

    psA = ps([128, 512], F32, "psA"); B_psA = Buf(True)
    psB = ps([128, 512], F32, "psB"); B_psB = Buf(True)
    psT = ps([128, 1024], BF16, "psT"); B_psT = Buf(True)
    psT2 = ps([128, 1024], BF16, "psT2"); B_psT2 = Buf(True)
    psACC = ps([128, 2048], F32, "psACC"); B_acc = [Buf(True) for _ in range(4)]
    rot = {"ab": 0, "t": 0}

    def next_ab():
        rot["ab"] ^= 1
        return (psA, B_psA) if rot["ab"] else (psB, B_psB)

    def next_t():
        rot["t"] ^= 1
        return (psT, B_psT) if rot["t"] else (psT2, B_psT2)

    SC = sb([128, AW], F32, "SC"); B_SC = Buf()
    SEL = sb([128, AW], BF16, "SEL"); B_SEL = [Buf(), Buf()]
    PB = sb([128, AW], BF16, "PB"); B_PB = [Buf(), Buf()]
    PTB = sb([128, AW], BF16, "PTB"); B_PTB = [Buf(), Buf()]
    KTB = sb([128, AW], BF16, "KTB"); B_KTB = Buf()
    VB = sb([128, AW], BF16, "VB"); B_VB = Buf()
    small = sb([128, 64], F32, "small"); B_small = Buf()
    B_stg = [Buf(), Buf()]
    uv_d = dscr("uv_scr", [16384, 2 * D], BF16)
    B_uv = Buf()
    wscr = {}
    conv_pending = []
    for wd, ncols in ((win_d, DIN), (wout_d, D), (wq_d, 1024)):
        scr = dscr("bf_" + wd.name, [D, ncols], BF16)
        bufs = {}
        for part, p0 in enumerate(range(0, ncols, 2048)):
            p1 = min(ncols, p0 + 2048)
            for kc in range(KC):
                b_ = Buf()
                bufs[(kc, part)] = b_
                conv_pending.append((wd.ap()[kc * 128:(kc + 1) * 128, p0:p1], scr.ap()[kc * 128:(kc + 1) * 128, p0:p1], p1 - p0, b_))
        wscr[wd.name] = (scr, bufs)
    conv_bufs = []
    for r in range(128):
        for half, src in ((0, pu_d), (1, pv_d)):
            b_ = Buf()
            conv_bufs.append(b_)
            conv_pending.append((src.ap()[r * 128:(r + 1) * 128, :], uv_d.ap()[r * 128:(r + 1) * 128, half * D:(half + 1) * D], D, b_))
    conv_state = {"i": 0}

    def conv_step(n):
        for _ in range(n):
            if not conv_pending:
                return
            src, dst, width, b_ = conv_pending.pop(0)
            i = conv_state["i"]
            conv_state["i"] ^= 1
            stg = VB[:, i * D:i * D + width]
            DMA("pool", stg, src, (), [B_stg[i]])
            DMA("sp", dst, stg, [B_stg[i]], [b_])

    def conv_join():
        conv_step(10 ** 9)
        S.op("dve", lambda e: e.memset(small[:, 63:64], 0.0), conv_bufs, [B_uv, B_VB, B_stg[0], B_stg[1], B_small])

    NPB = 2
    wpb = [sb([128, KC, PW], BF16, "wp%d" % i) for i in range(NPB)]
    B_wpb = [Buf() for _ in range(NPB)]
    wrot = [0]

    def load_piece(wd, c0, width):
        i = wrot[0]
        wrot[0] = (i + 1) % NPB
        if wd.name in wscr:
            scr, bufs = wscr[wd.name]
            part = c0 // 2048
            while any(cp[3] is bufs[(kc, part)] for cp in conv_pending for kc in range(KC)):
                conv_step(1)
            src = scr.ap()[:, c0:c0 + width].rearrange("(kc p) c -> p kc c", p=128)
            DMA("pool", wpb[i][:, :, 0:width], src, [bufs[(kc, part)] for kc in range(KC)], [B_wpb[i]])
        else:
            src = wd.ap()[:, c0:c0 + width].rearrange("(kc p) c -> p kc c", p=128)
            DMA("pool", wpb[i][:, :, 0:width], src, (), [B_wpb[i]])
        conv_step(3)
        return wpb[i], B_wpb[i]

    c3T = sb([128, KC * 3], F32, "c3T"); B_c3T = Buf()
    DMA("sp", c3T[:], c3T_d.ap(), (), [B_c3T])
    sc3 = sb([128, KC * 3], BF16, "sc3"); B_sc3 = Buf()
    ACT(sc3[:], c3T[:], AF.Silu, [B_c3T], [B_sc3])
    modT = sb([128, 4, KC, 3], F32, "modT"); B_modT = Buf()
    mrow = [sb([3, PW], F32, "mrow%d" % i) for i in range(2)]; B_mrow = [Buf(), Buf()]
    bad = [sb([3, PW], F32, "bad%d" % i) for i in range(2)]; B_bad = [Buf(), Buf()]
    for pcs in range(6 * D // PW):
        c0 = pcs * PW
        wp, Bwp = load_piece(wada_d, c0, PW)
        pa, Bpa = next_ab()
        for kc in range(KC):
            MM(pa[0:3, 0:PW], sc3[:, kc * 3:kc * 3 + 3], wp[:, kc, :], kc == 0, kc == KC - 1, [B_sc3, Bwp], [Bpa])
        i = pcs % 2
        DMA("sp", bad[i][:], bada_d.ap()[0:1, c0:c0 + PW].partition_broadcast(3), (), [B_bad[i]])
        TT("dve", mrow[i][:], pa[0:3, 0:PW], bad[i][:], ALU.add, [Bpa, B_bad[i]], [B_mrow[i]])
        DMA("sp", modrow_d.ap()[:, c0:c0 + PW], mrow[i][:], [B_mrow[i]], [B_modrow])
        j6 = c0 // D
        if j6 in (0, 1, 3, 4):
            jj = {0: 0, 1: 1, 3: 2, 4: 3}[j6]
            for cc in range(PW // 128):
                kc = (c0 % D) // 128 + cc
                pa2, Bpa2 = next_ab()
                MM(pa2[:, 0:3], mrow[i][0:3, cc * 128:(cc + 1) * 128], identf[0:3, 0:3], True, True, [B_mrow[i], B_identf], [Bpa2])
                CP("dve", modT[:, jj, kc, :], pa2[:, 0:3], [Bpa2], [B_modT])
    for jj, (gT, Bg) in ((1, (g1T, B_g1T)), (3, (g2T, B_g2T))):
        TS("dve", modT[:, jj, :, :], modT[:, jj, :, :], 1.0, None, ALU.add, None, [B_modT], [B_modT])
        TT("dve", modT[:, jj, :, :], modT[:, jj, :, :], gT[:, :].unsqueeze(2).to_broadcast([128, KC, 3]), ALU.mult, [B_modT, Bg], [B_modT])

    rbt = sb([32, 8], F32, "rbt"); B_rbt = Buf()
    oht = sb([32, 383], F32, "oht"); B_oht = Buf()
    DMA("sp", rbt[:], rb_d.ap(), (), [B_rbt])
    DMA("sp", oht[:], oh_d.ap(), (), [B_oht])
    pa, Bpa = next_ab()
    MM(pa[0:8, 0:383], rbt[:, :], oht[:, :], True, True, [B_rbt, B_oht], [Bpa])
    bvs = sb([8, 383], F32, "bvs"); B_bvs = Buf()
    CP("dve", bvs[:], pa[0:8, 0:383], [Bpa], [B_bvs])
    B_bvd = Buf(); B_bscd = Buf()
    DMA("sp", bv_d.ap(), bvs[:], [B_bvs], [B_bvd])
    bband = sb([128, 8, 256], F32, "bband"); B_bband = Buf()
    brep = sb([128, 383], F32, "brep"); B_brep = Buf()
    for h in range(8):
        DMA("sp", brep[:], bv_d.ap()[h:h + 1, :].partition_broadcast(128), [B_bvd], [B_brep])
        DMA("sp", bsc_d.ap()[h], brep[:], [B_brep], [B_bscd])
        src = bass.AP(bsc_d, h * 128 * 383 + 127, [[382, 128], [1, 256]])
        DMA("sp", bband[:, h, :], src, [B_bscd], [B_bband])
    TT("dve", bband[:], bband[:], bfar[:, :].unsqueeze(2).to_broadcast([128, 8, 256]), ALU.subtract, [B_bband, B_bfar], [B_bband])
    mxdb = sb([128, 8], F32, "mxdb"); B_mxdb = Buf()
    S.op("dve", lambda e: e.tensor_reduce(out=mxdb[:], in_=bband[:], axis=AX.X, op=ALU.max), [B_bband], [B_mxdb])
    TS("dve", mxdb[:], mxdb[:], 0.0, None, ALU.max, None, [B_mxdb], [B_mxdb])

    TG = GT * 128
    hnT = sb([128, KC, TG], BF16, "hnT"); B_hnT = Buf()
    xr_ext = sb([128, 8, 3 + TG], F32, "xr_ext"); B_xr = Buf()
    yrT = sb([128, 8, TG], F32, "yrT"); B_yr = Buf()
    qT = sb([128, 8, TG], BF16, "qT"); B_qT = Buf()
    qiT = sb([128, 8, TG], BF16, "qiT"); B_qiT = Buf()
    kTg = sb([128, 8, TG], BF16, "kTg"); B_kTg = Buf()
    kiTg = sb([64, TG], BF16, "kiTg"); B_kiTg = Buf()
    catT = sb([128, KC, TG], BF16, "catT"); B_cat = [Buf() for _ in range(GT)]
    wi_t = sb([128, GT, 16], F32, "wi_t"); B_wi = Buf()
    hprev = sb([128, 8], F32, "hprev"); B_hprev = Buf()
    xt = sb([128, D], F32, "xt"); B_xt = Buf()
    xsb = sb([128, D], BF16, "xsb"); B_xsb = Buf()
    tm32 = sb([128, GT * 1024], F32, "tm32"); B_tm32 = Buf()
    tm16 = sb([128, 1024], BF16, "tm16"); B_tm16 = Buf()
    gt1b = sb([128, D], F32, "gt1b"); gt2b = sb([128, D], F32, "gt2b"); B_gtb = Buf()

    def rms_hnT(x_rows, P, b, tcol, which):
        jsh, jg = (0, 1) if which == 0 else (2, 3)
        if x_rows is not None:
            DMA("sp", xt[:P, :], x_rows, (), [B_xt])
        ms = small[:, 0:1]
        ACT(xsb[:P, :], xt[:P, :], AF.Square, [B_xt], [B_xsb, B_small], accum_out=ms[:P])
        TS("dve", ms[:P], ms[:P], 1.0 / D, EPS, ALU.mult, ALU.add, [B_small], [B_small])
        ACT(ms[:P], ms[:P], AF.Sqrt, [B_small], [B_small])
        S.op("dve", lambda e: e.reciprocal(out=small[:P, 1:2], in_=ms[:P]), [B_small], [B_small])
        TS("dve", xsb[:P, :], xt[:P, :], small[:P, 1:2], None, ALU.mult, None, [B_xt, B_small], [B_xsb])
        for half in range(2):
            pt, Bpt = next_t()
            for j in range(8):
                kc = half * 8 + j
                TR(pt[:, j * 128:j * 128 + P], xsb[:P, kc * 128:(kc + 1) * 128], ident[:P, :P], [B_xsb, B_ident], [Bpt])
            for j in range(8):
                kc = half * 8 + j
                ACT(hnT[:, kc, tcol:tcol + P], pt[:, j * 128:j * 128 + P], AF.Identity, [Bpt, B_modT], [B_hnT],
                    scale=modT[:, jg, kc, b:b + 1], bias=modT[:, jsh, kc, b:b + 1])

    class Seq:
        pass

    def make_seq(name, b, smax, topk, nreg):
        s = Seq()
        s.b = b
        s.topk = topk
        s.kT = dscr("kT_" + name, [8, 128, smax], BF16)
        s.v = dscr("v_" + name, [8, 128, smax // 128, 128], BF16)
        s.kiT = dscr("kiT_" + name, [128, smax], BF16)
        s.B_reg = [Buf() for _ in range(nreg)]
        return s

    def proj_cm(wp, Bwp, off, M, T, dest, Rd, Wd, eng="act"):
        pa, Bpa = next_ab()
        for kc in range(KC):
            MM(pa[0:M, 0:T], wp[:, kc, off:off + M], hnT[:, kc, 0:T], kc == 0, kc == KC - 1, [Bwp, B_hnT], [Bpa])
        CP(eng, dest, pa[0:M, 0:T], [Bpa] + Rd, Wd)

    def projections(seq, T, tiles, own, kpos0, k_out, v_out, ki_out):
        def pieces(c0, c1):
            return [(c, min(PW, c1 - c)) for c in range(c0, c1, PW)]
        for (c, w) in pieces(0, 1024):
            wp, Bwp = load_piece(win_d, c, w)
            for o in range(0, w, 128):
                blk = (c + o) // 128
                proj_cm(wp, Bwp, o, 128, T, xr_ext[:, blk, 3:3 + T], [], [B_xr])
        if own:
            for (c, w) in pieces(1024, 2048):
                wp, Bwp = load_piece(win_d, c, w)
                for o in range(0, w, 128):
                    blk = (c + o - 1024) // 128
                    proj_cm(wp, Bwp, o, 128, T, yrT[:, blk, 0:T], [], [B_yr])
            for (c, w) in pieces(2048, 3072):
                wp, Bwp = load_piece(win_d, c, w)
                for o in range(0, w, 128):
                    blk = (c + o - 2048) // 128
                    proj_cm(wp, Bwp, o, 128, T, qT[:, blk, 0:T], [], [B_qT])
        SUB4 = cfg.get("SUB4", 99) if own else 99
        if SUB4 < 2:
            return
        for (c, w) in pieces(3072, 4096):
            wp, Bwp = load_piece(win_d, c, w)
            for o in range(0, w, 128):
                blk = (c + o - 3072) // 128
                proj_cm(wp, Bwp, o, 128, T, kTg[:, blk, 0:T], [], [B_kTg], eng="dve")
            if own:
                for ti, (tcol, P, row0) in enumerate(tiles):
                    pa, Bpa = next_ab()
                    for kc in range(KC):
                        MM(pa[:P, 0:w], hnT[:, kc, tcol:tcol + P], wp[:, kc, 0:w], kc == 0, kc == KC - 1, [Bwp, B_hnT], [Bpa])
                    o0 = ti * 1024 + (c - 3072)
                    CP("dve", tm32[:P, o0:o0 + w], pa[:P, 0:w], [Bpa], [B_tm32])
                    if c + w == 4096 and not os.environ.get("NOKDMA"):
                        DMA("pool", k_out[row0:row0 + P, :], tm32[:P, ti * 1024:(ti + 1) * 1024], [B_tm32], [outbuf()])
        regs = [seq.B_reg[(kpos0 + t) // 128] for t in range(0, T, 128)]
        for h in range(8):
            DMA("sp", seq.kT.ap()[h, :, kpos0:kpos0 + T], kTg[:, h, 0:T], [B_kTg], regs)
        if SUB4 < 3:
            return
        for (tcol, P, row0) in tiles:
            for (c, w) in pieces(4096, 5120):
                wp, Bwp = load_piece(win_d, c, w)
                pa, Bpa = next_ab()
                for kc in range(KC):
                    MM(pa[:P, 0:w], hnT[:, kc, tcol:tcol + P], wp[:, kc, 0:w], kc == 0, kc == KC - 1, [Bwp, B_hnT], [Bpa])
                if own:
                    CP("dve", tm32[:P, (c - 4096):(c - 4096) + w], pa[:P, 0:w], [Bpa], [B_tm32])
                CP("act", tm16[:P, (c - 4096):(c - 4096) + w], pa[:P, 0:w], [Bpa], [B_tm16])
            if own:
                DMA("pool", v_out[row0:row0 + P, :], tm32[:P, 0:1024], [B_tm32], [outbuf()])
            kb = (kpos0 + tcol) // 128
            po = (kpos0 + tcol) % 128
            for h in range(8):
                DMA("sp", seq.v.ap()[h, po:po + P, kb, :], tm16[:P, h * 128:(h + 1) * 128], [B_tm16], [seq.B_reg[kb]])
        if SUB4 < 4:
            return
        if own:
            for (c, w) in pieces(5120, 6144):
                wp, Bwp = load_piece(win_d, c, w)
                for o in range(0, w, 128):
                    blk = (c + o - 5120) // 128
                    proj_cm(wp, Bwp, o, 128, T, qiT[:, blk, 0:T], [], [B_qiT])
        if SUB4 < 5:
            return
        wp, Bwp = load_piece(win_d, 6144, 80)
        proj_cm(wp, Bwp, 0, 64, T, kiTg[0:64, 0:T], [], [B_kiTg], eng="dve")
        DMA("sp", seq.kiT.ap()[0:64, kpos0:kpos0 + T], kiTg[0:64, 0:T], [B_kiTg], regs)
        DMA("sp", seq.kiT.ap()[64:128, kpos0:kpos0 + T], kiTg[0:64, 0:T], [B_kiTg], regs)
        if own:
            for ti, (tcol, P, row0) in enumerate(tiles):
                pa, Bpa = next_ab()
                for kc in range(KC):
                    MM(pa[:P, 0:80], hnT[:, kc, tcol:tcol + P], wp[:, kc, 0:80], kc == 0, kc == KC - 1, [Bwp, B_hnT], [Bpa])
                CP("dve", small[:P, 0:64], pa[:P, 0:64], [Bpa], [B_small])
                DMA("pool", ki_out[row0:row0 + P, :], small[:P, 0:64], [B_small], [outbuf()])
                CP("dve", wi_t[:P, ti, :], pa[:P, 64:80], [Bpa], [B_wi])

    def lru_tile(tcol, P, own, first_flag, ti):
        xc = SC[:, 0:1024].rearrange("p (b t) -> p b t", b=8)[:, :, 0:P]
        rr = SC[:, 1024:2048].rearrange("p (b t) -> p b t", b=8)[:, :, 0:P]
        ii = SC[:, 2048:3072].rearrange("p (b t) -> p b t", b=8)[:, :, 0:P]
        aa = SC[:, 3072:4096].rearrange("p (b t) -> p b t", b=8)[:, :, 0:P]
        mm_ = SEL[:, 0:2048].bitcast(F32).rearrange("p (b t) -> p b t", b=8)[:, :, 0:P]
        hh = PB[:, 0:2048].bitcast(F32).rearrange("p (b t) -> p b t", b=8)[:, :, 0:P]
        xcb = PTB[:, 0:1024].rearrange("p (b t) -> p b t", b=8)[:, :, 0:P]
        gy = KTB[:, 0:2048].bitcast(F32).rearrange("p (b t) -> p b t", b=8)[:, :, 0:P]
        Bm, Bh, Bxcb, Bgy = B_SEL[0], B_PB[0], B_PTB[0], B_KTB

        def fork(parents, n):
            inh = [x for pb in parents for x in (([pb.w] if pb.w else []) + pb.r)]
            out = []
            for _ in range(n):
                b_ = Buf()
                b_.r = list(inh)
                out.append(b_)
            return out

        def join(parents, subs):
            for pb in parents:
                for b_ in subs:
                    pb.r += (([b_.w] if b_.w else []) + b_.r)

        Bx = fork([B_SC], 8); Br = fork([B_SC], 8); Bi = fork([B_SC], 8); Ba = fork([B_SC], 8)
        Bm8 = fork([Bm], 8); Bh8 = fork([Bh], 8)
        for j in range(4):
            for blk in range(8):
                if j == 0:
                    TS("dve", xc[:, blk, :], xr_ext[:, blk, tcol:tcol + P], cw[:, blk:blk + 1], cb[:, blk:blk + 1], ALU.mult, ALU.add,
                       [B_xr, B_cw, B_cb], [Bx[blk]])
                else:
                    STT(xc[:, blk, :], xr_ext[:, blk, tcol + j:tcol + j + P], cw[:, j * 8 + blk:j * 8 + blk + 1], xc[:, blk, :],
                        ALU.mult, ALU.add, [B_xr, B_cw, Bx[blk]], [Bx[blk]])
        CP("pool", xcb, xc, Bx, [Bxcb])
        for blk in range(8):
            pa, Bpa = next_ab()
            MM(pa[:, 0:P], wa_b[:, blk * 128:(blk + 1) * 128], xcb[:, blk, :], True, True, [B_wa, Bxcb], [Bpa])
            ACT(rr[:, blk, :], pa[:, 0:P], AF.Sigmoid, [Bpa, B_ba], [Br[blk]], bias=ba[:, blk:blk + 1])
            pa, Bpa = next_ab()
            MM(pa[:, 0:P], wx_b[:, blk * 128:(blk + 1) * 128], xcb[:, blk, :], True, True, [B_wx, Bxcb], [Bpa])
            ACT(ii[:, blk, :], pa[:, 0:P], AF.Sigmoid, [Bpa, B_bx], [Bi[blk]], bias=bx[:, blk:blk + 1])
        for blk in range(8):
            ACT(aa[:, blk, :], rr[:, blk, :], AF.Exp, [Br[blk], B_c12], [Ba[blk]], scale=c1[:, blk:blk + 1])
            ACT(mm_[:, blk, :], rr[:, blk, :], AF.Exp, [Br[blk], B_c12], [Bm8[blk]], scale=c2[:, blk:blk + 1])
        TS("dve", mm_, mm_, -1.0, 1.0, ALU.mult, ALU.add, Bm8, Bm8)
        ACT(mm_, mm_, AF.Sqrt, Bm8, Bm8)
        if first_flag == "always":
            MS("dve", mm_[:, :, 0:1], 1.0, Bm8)
        elif first_flag == "flag":
            TS("dve", mm_[:, :, 0:1], mm_[:, :, 0:1], flags[:, 0:1], flags[:, 1:2], ALU.mult, ALU.add, Bm8 + [B_flags], Bm8)
        TT("dve", ii, ii, xc, ALU.mult, Bi + Bx, Bi)
        TT("dve", ii, ii, mm_, ALU.mult, Bi + Bm8, Bi)
        for blk in range(8):
            S.op("dve", lambda e, blk=blk: e.tensor_tensor_scan(out=hh[:, blk, :], data0=aa[:, blk, :], data1=ii[:, blk, :],
                                                                initial=hprev[:, blk:blk + 1], op0=ALU.mult, op1=ALU.add),
                 [Ba[blk], Bi[blk], B_hprev], [Bh8[blk]])
        CP("dve", hprev[:, :].unsqueeze(2), hh[:, :, P - 1:P], Bh8, [B_hprev])
        if own:
            ACT(gy, yrT[:, :, tcol:tcol + P], AF.Gelu_apprx_tanh, [B_yr], [Bgy])
            TT("dve", catT[:, 0:8, tcol:tcol + P], hh, gy, ALU.mult, Bh8 + [Bgy], [B_cat[ti]])
        join([B_SC], Bx + Br + Bi + Ba)
        join([Bm], Bm8)
        join([Bh], Bh8)

    def attention_tile(seq, tcol, P, kpos, S_ctxpen, ti):
        S_t = kpos + P
        nb128 = (S_t + 127) // 128
        regs = seq.B_reg[0:nb128]
        kiTs = KTB
        DMA("sp", kiTs[:, 0:S_t], seq.kiT.ap()[:, 0:S_t], regs, [B_KTB])
        awi = small[:, 16:32]
        sgn = small[:, 32:48]
        wi = wi_t[:, ti, :]
        ACT(awi[:P], wi[:P], AF.Abs, [B_wi], [B_small], scale=0.03125)
        TS("dve", sgn[:P], wi[:P], 0.0, 2.0, ALU.is_gt, ALU.mult, [B_wi], [B_small])
        TS("dve", sgn[:P], sgn[:P], -1.0, None, ALU.add, None, [B_small], [B_small])
        dsg = PTB[:, 0:16 * 128].rearrange("p (h q) -> p h q", h=16)
        TT("dve", dsg[:P, :, :P], ident[:P, :P].unsqueeze(1).to_broadcast([P, 16, P]), sgn[:P, :].unsqueeze(2).to_broadcast([P, 16, P]),
           ALU.mult, [B_ident, B_small], [B_PTB[0]])
        score = SC
        idx_banks = [(psA[:, :], B_psA), (psB[:, :], B_psB), (psT[:, :].bitcast(F32), B_psT), (psT2[:, :].bitcast(F32), B_psT2),
                     (psACC[:, 1024:1536], B_acc[2]), (psACC[:, 1536:2048], B_acc[3])]
        B_tmp = [Buf() for _ in range(8)]
        for b_ in B_tmp:
            b_.r = [x for pb in B_PB for x in (([pb.w] if pb.w else []) + pb.r)]
        kk = 0
        IW = 256
        for s0 in range(0, S_t, IW):
            w = min(IW, S_t - s0)
            accb = (s0 // IW) % 2
            acc = psACC[:, accb * 512:(accb + 1) * 512]
            for h in range(16):
                c, base = h // 2, (h % 2) * 64
                pa, Bpa = idx_banks[kk % 6]
                tv = PB[:, (kk % 8) * 512:(kk % 8) * 512 + w]
                tb = B_tmp[kk % 8]
                kk += 1
                MM(pa[:P, 0:w], qiT[base:base + 64, c, tcol:tcol + P], kiTs[base:base + 64, s0:s0 + w], True, True, [B_qiT, B_KTB], [Bpa])
                if h % 2 == 0:
                    ACT(tv[:P], pa[:P, 0:w], AF.Relu, [Bpa, B_small], [tb], scale=awi[:P, h:h + 1])
                else:
                    TS("dve", tv[:P], pa[:P, 0:w], 0.0, awi[:P, h:h + 1], ALU.max, ALU.mult, [Bpa, B_small], [tb])
                MM(acc[:P, 0:w], dsg[:P, h, :P], tv[:P], h == 0, h == 15, [B_PTB[0], tb], [B_acc[accb]])
            CP("dve" if accb == 0 else "act", score[:P, s0:s0 + w], acc[:P, 0:w], [B_acc[accb]], [B_SC])
        for pb in B_PB:
            for b_ in B_tmp:
                pb.r += (([b_.w] if b_.w else []) + b_.r)
        lo = small[:, 2:3]; wdt = small[:, 3:4]; mid = small[:, 4:5]; cnt = small[:, 5:6]; stp = small[:, 6:7]
        S.op("dve", lambda e: e.tensor_reduce(out=lo[:P], in_=score[:P, 0:S_t], axis=AX.X, op=ALU.min), [B_SC], [B_small])
        S.op("dve", lambda e: e.tensor_reduce(out=wdt[:P], in_=score[:P, 0:S_t], axis=AX.X, op=ALU.max), [B_SC], [B_small])
        TT("dve", wdt[:P], wdt[:P], lo[:P], ALU.subtract, [B_small], [B_small])
        TS("dve", wdt[:P], wdt[:P], 1.0e-6, None, ALU.add, None, [B_small], [B_small])
        wk = small[:, 40:40 + NIT + 1]
        TS("dve", wk[:P], pow2[:P, :], wdt[:P], None, ALU.mult, None, [B_pow2, B_small], [B_small])
        if P == 128:
            MS("dve", score[0:64, kpos + 64:kpos + 128], NEG, [B_SC])
        if S_ctxpen > 0:
            TS("dve", score[:P, 0:S_ctxpen], score[:P, 0:S_ctxpen], flags[:P, 2:3], None, ALU.add, None, [B_SC, B_flags], [B_SC])
        junk = PB
        for k in range(1, NIT + 1):
            TT("dve", mid[:P], lo[:P], wk[:P, k:k + 1], ALU.add, [B_small], [B_small])
            TS("dve", junk[:P, 0:S_t], score[:P, 0:S_t], mid[:P], None, ALU.is_ge, ALU.add, [B_SC, B_small], [B_PB[0], B_PB[1], B_small],
               accum_out=cnt[:P])
            STT(stp[:P], cnt[:P], float(seq.topk) - 0.5, wk[:P, k:k + 1], ALU.is_ge, ALU.mult, [B_small], [B_small])
            TT("dve", lo[:P], lo[:P], stp[:P], ALU.add, [B_small], [B_small])
        sel = SEL
        TS("dve", sel[:P, 0:S_t], score[:P, 0:S_t], lo[:P], None, ALU.is_ge, None, [B_SC, B_small], [B_SEL[0], B_SEL[1]])
        L = SC
        mx8 = small[:, 8:16]
        rs = small[:, 48:56]
        po = psACC[:, 0:1024]
        nblk = (S_t + 511) // 512
        assert nblk <= 9
        band0 = kpos - 128
        for h in range(8):
            kTh = KTB
            DMA("sp", kTh[:, 0:S_t], seq.kT.ap()[h, :, 0:S_t], regs, [B_KTB])
            Vh = VB[:, 0:nb128 * 128].rearrange("p (n d) -> p n d", d=128)
            DMA("sp", Vh, seq.v.ap()[h, :, 0:nb128, :], regs, [B_VB])
            for bi, s0 in enumerate(range(0, S_t, 512)):
                w = min(512, S_t - s0)
                pa, Bpa = next_ab()
                MM(pa[:P, 0:w], qT[:, h, tcol:tcol + P], kTh[:, s0:s0 + w], True, True, [B_qT, B_KTB], [Bpa])
                S.op("dve", lambda e, pa=pa, s0=s0, w=w, bi=bi: e.tensor_scalar(
                    out=L[:P, s0:s0 + w], in0=pa[:P, 0:w], scalar1=128.0 ** -0.5, scalar2=None, op0=ALU.mult, op1=ALU.max,
                    accum_out=small[:P, 24 + bi:25 + bi]), [Bpa], [B_SC, B_small])
            b_lo = max(band0, 0)
            b_hi = min(kpos + 128, S_t)
            TT("dve", L[:P, b_lo:b_hi], L[:P, b_lo:b_hi], bband[:P, h, b_lo - band0:b_hi - band0], ALU.add, [B_SC, B_bband], [B_SC])
            S.op("dve", lambda e: e.tensor_reduce(out=mx8[:P, h:h + 1], in_=small[:P, 24:24 + nblk], axis=AX.X, op=ALU.max),
                 [B_small], [B_small])
            TS("dve", mx8[:P, h:h + 1], mx8[:P, h:h + 1], mxdb[:P, h:h + 1], -1.0, ALU.add, ALU.mult, [B_small, B_mxdb], [B_small])
            Pm = PB
            ACT(Pm[:P, 0:S_t], L[:P, 0:S_t], AF.Exp, [B_SC, B_small], [B_PB[0], B_PB[1]], bias=mx8[:P, h:h + 1])
            STT(Pm[:P, 0:S_t], Pm[:P, 0:S_t], 1.0, sel[:P, 0:S_t], ALU.mult, ALU.mult, [B_PB[0], B_PB[1], B_SEL[0], B_SEL[1]],
                [B_PB[0], B_PB[1], B_small], accum_out=rs[:P, h:h + 1])
            PT = PTB
            for j0 in range(0, nb128, 8):
                pt, Bpt = next_t()
                nj = min(8, nb128 - j0)
                for jj in range(nj):
                    j = j0 + jj
                    ws = min(128, S_t - j * 128)
                    TR(pt[0:ws, jj * 128:jj * 128 + P], Pm[:P, j * 128:j * 128 + ws], ident[:P, :P], [B_PB[0], B_PB[1], B_ident], [Bpt])
                ws_last = min(128, S_t - (j0 + nj - 1) * 128)
                if ws_last == 128:
                    CP("act" if (j0 // 8) % 2 == 0 else "dve", PT[:, j0 * 128:(j0 + nj) * 128], pt[:, 0:nj * 128], [Bpt], [B_PTB[0], B_PTB[1]])
                else:
                    if nj > 1:
                        CP("act", PT[:, j0 * 128:(j0 + nj - 1) * 128], pt[:, 0:(nj - 1) * 128], [Bpt], [B_PTB[0], B_PTB[1]])
                    CP("dve", PT[0:ws_last, (j0 + nj - 1) * 128:(j0 + nj) * 128], pt[0:ws_last, (nj - 1) * 128:nj * 128], [Bpt],
                       [B_PTB[0], B_PTB[1]])
            for j in range(nb128):
                ws = min(128, S_t - j * 128)
                MM(po[:P, h * 128:(h + 1) * 128], PT[0:ws, j * 128:j * 128 + P], Vh[0:ws, j, :], j == 0, j == nb128 - 1,
                   [B_PTB[0], B_PTB[1], B_VB], [B_acc[0], B_acc[1]])
        rinv = small[:, 56:64]
        S.op("dve", lambda e: e.reciprocal(out=rinv[:P], in_=rs[:P]), [B_small], [B_small])
        attn = tm16
        for h in range(8):
            ACT(attn[:P, h * 128:(h + 1) * 128], po[:P, h * 128:(h + 1) * 128], AF.Identity, [B_acc[0], B_acc[1], B_small], [B_tm16],
                scale=rinv[:P, h:h + 1])
        pt, Bpt = next_t()
        for h in range(8):
            TR(pt[:, h * 128:h * 128 + P], attn[:P, h * 128:(h + 1) * 128], ident[:P, :P], [B_tm16, B_ident], [Bpt])
        for h in range(8):
            CP("dve", catT[:, 8 + h, tcol:tcol + P], pt[:, h * 128:h * 128 + P], [Bpt], [B_cat[ti]])

    def mix_residual(x_rows, P, tcol, ti):
        for db in range(D // PW):
            wp, Bwp = load_piece(wout_d, db * PW, PW)
            bk = B_acc[(db * PW) // 512]
            for kc in range(KC):
                MM(psACC[:P, db * PW:(db + 1) * PW], catT[:, kc, tcol:tcol + P], wp[:, kc, :], kc == 0, kc == KC - 1, [B_cat[ti], Bwp], [bk])
        x1 = SC[:, 0:D]
        TT("dve", x1[:P], psACC[:P, :], gt1b[:P, :], ALU.mult, B_acc + [B_gtb], [B_SC])
        DMA("sp", xt[:P, :], x_rows, (), [B_xt])
        TT("dve", xt[:P, :], xt[:P, :], x1[:P], ALU.add, [B_xt, B_SC], [B_xt])

    def peer_tile(P, y_rows):
        rms_hnT(None, P, cur["b"], 0, 1)
        hn2 = sb2_hn2
        for half in range(2):
            pt, Bpt = next_t()
            for j in range(8):
                kc = half * 8 + j
                TR(pt[:P, j * 128:(j + 1) * 128], hnT[:, kc, 0:P], ident[:, :], [B_hnT, B_ident], [Bpt])
            CP("act" if half == 0 else "dve", hn2[:P, half * 1024:(half + 1) * 1024], pt[:P, :], [Bpt], [B_hn2])
        pq = VB[:, 0:2048].bitcast(F32).rearrange("p (c t) -> p c t", c=8)
        for pc in range(1024 // PW):
            wp, Bwp = load_piece(wq_d, pc * PW, PW)
            for o in range(0, PW, 128):
                c = (pc * PW + o) // 128
                pa, Bpa = next_ab()
                for kc in range(KC):
                    MM(pa[:, 0:P], wp[:, kc, o:o + 128], hnT[:, kc, 0:P], kc == 0, kc == KC - 1, [Bwp, B_hnT], [Bpa])
                CP("act", pq[:, c, 0:P], pa[:, 0:P], [Bpa], [B_VB])
        W0 = D
        s12 = SC[:, W0:W0 + 256].rearrange("p (a n) -> p a n", a=2)
        s12b = SC[:, W0 + 256:W0 + 512].rearrange("p (a n) -> p a n", a=2)
        cand = SC[:, W0 + 512:W0 + 768]
        candb = SC[:, W0 + 768:W0 + 1024]
        cidx = SC[:, W0 + 1024:W0 + 1280]
        jk = SC[:, W0 + 1280:W0 + 1536]
        v12 = pst_small[:, 0:32].rearrange("p (a k) -> p a k", a=2)
        i12 = pst_small_u[:, 32:64].rearrange("p (a k) -> p a k", a=2)
        i12f = pst_small[:, 64:96].rearrange("p (a k) -> p a k", a=2)
        ts_ = pst_small[:, 96:112]
        tj = pst_small_u[:, 112:128]
        tjf = pst_small[:, 128:144]
        ssum = pst_small[:, 144:145]
        negm = pst_small[:, 145:146]
        Bq = B_pst
        for h in range(8):
            for a in range(2):
                pa, Bpa = next_ab()
                MM(pa[:P, 0:128], pq[a * 64:(a + 1) * 64, h, 0:P], keysT[a * 64:(a + 1) * 64, h * 128:(h + 1) * 128], True, True,
                   [B_VB, B_keysT], [Bpa])
                CP("act", s12[:P, a, :], pa[:P, 0:128], [Bpa], [B_SC])
                S.op("dve", lambda e, a=a: e.max(out=v12[:P, a, 0:8], in_=s12[:P, a, :]), [B_SC], [Bq])
                S.op("dve", lambda e, a=a: e.match_replace(out=s12b[:P, a, :], in_to_replace=v12[:P, a, 0:8], in_values=s12[:P, a, :],
                                                          imm_value=NEG), [B_SC, Bq], [B_SC])
                S.op("dve", lambda e, a=a: e.max(out=v12[:P, a, 8:16], in_=s12b[:P, a, :]), [B_SC, Bq], [Bq])
                S.op("dve", lambda e, a=a: e.max_index(out=i12[:P, a, 0:8], in_max=v12[:P, a, 0:8], in_values=s12[:P, a, :]), [B_SC, Bq], [Bq])
                S.op("dve", lambda e, a=a: e.max_index(out=i12[:P, a, 8:16], in_max=v12[:P, a, 8:16], in_values=s12[:P, a, :]), [B_SC, Bq], [Bq])
            CP("dve", i12f[:P], i12[:P], [Bq], [Bq])
            c3 = cand.rearrange("p (a b) -> p a b", a=16)
            TT("dve", c3[:P], v12[:P, 0, :].unsqueeze(2).to_broadcast([P, 16, 16]), v12[:P, 1, :].unsqueeze(1).to_broadcast([P, 16, 16]),
               ALU.add, [Bq], [B_SC])
            ci3 = cidx.rearrange("p (a b) -> p a b", a=16)
            STT(ci3[:P], i12f[:P, 0, :].unsqueeze(2).to_broadcast([P, 16, 16]), 128.0, i12f[:P, 1, :].unsqueeze(1).to_broadcast([P, 16, 16]),
                ALU.mult, ALU.add, [Bq], [B_SC])
            S.op("dve", lambda e: e.max(out=ts_[:P, 0:8], in_=cand[:P, :]), [B_SC], [Bq])
            S.op("dve", lambda e: e.match_replace(out=candb[:P, :], in_to_replace=ts_[:P, 0:8], in_values=cand[:P, :], imm_value=NEG),
                 [B_SC, Bq], [B_SC])
            S.op("dve", lambda e: e.max(out=ts_[:P, 8:16], in_=candb[:P, :]), [B_SC, Bq], [Bq])
            S.op("dve", lambda e: e.max_index(out=tj[:P, 0:8], in_max=ts_[:P, 0:8], in_values=cand[:P, :]), [B_SC, Bq], [Bq])
            S.op("dve", lambda e: e.max_index(out=tj[:P, 8:16], in_max=ts_[:P, 8:16], in_values=cand[:P, :]), [B_SC, Bq], [Bq])
            CP("dve", tjf[:P], tj[:P], [Bq], [Bq])
            for k in range(16):
                STT(jk[:P, :], iota256[:P, :], tjf[:P, k:k + 1], cidx[:P, :], ALU.is_equal, ALU.mult, [B_iota, Bq, B_SC], [B_SC, B_eidx],
                    accum_out=eidxf[:P, h * 16 + k:h * 16 + k + 1])
            TS("dve", negm[:P], ts_[:P, 0:1], -1.0, None, ALU.mult, None, [Bq], [Bq])
            ACT(gw[:P, h * 16:(h + 1) * 16], ts_[:P, :], AF.Exp, [Bq], [B_gw, Bq], bias=negm[:P], accum_out=ssum[:P])
            S.op("dve", lambda e: e.reciprocal(out=ssum[:P], in_=ssum[:P]), [Bq], [Bq])
            TS("dve", gw[:P, h * 16:(h + 1) * 16], gw[:P, h * 16:(h + 1) * 16], ssum[:P], None, ALU.mult, None, [B_gw, Bq], [B_gw])
        CP("dve", eidx[:P, :], eidxf[:P, :], [B_eidx], [B_eidxi])
        NG = 5
        for sl in range(128):
            g, Bg = uvg[sl % NG], B_uvg[sl % NG]
            S.dma("pool", lambda e, g=g, sl=sl: e.indirect_dma_start(out=g[:P, :], out_offset=None, in_=uv_d.ap(),
                                                                     in_offset=bass.IndirectOffsetOnAxis(ap=eidx[:P, sl:sl + 1], axis=0)),
                  [B_eidxi, B_uv], Bg)
            STT(g[:P, 0:D], g[:P, 0:D], 1.0, hn2[:P, :], ALU.mult, ALU.mult, Bg + [B_hn2], [Bg[0], B_actv[sl % 8]],
                accum_out=actv[:P, sl:sl + 1])
            ACT(gel[:P, sl:sl + 1], actv[:P, sl:sl + 1], AF.Gelu_apprx_tanh, [B_actv[sl % 8]], [B_gel[sl % 8]])
            ACT(gel[:P, sl:sl + 1], gel[:P, sl:sl + 1], AF.Identity, [B_gel[sl % 8], B_gw], [B_gel[sl % 8]], scale=gw[:P, sl:sl + 1])
            dg, Bdg = dgs[sl % 2], B_dgs[sl % 2]
            ACT(dg[:P, :P], ident[:P, :P], AF.Identity, [B_ident, B_gel[sl % 8]], [Bdg], scale=gel[:P, sl:sl + 1])
            for nb in range(8):
                MM(psACC[:P, nb * 256:(nb + 1) * 256], dg[:P, :P], g[:P, D + nb * 256:D + (nb + 1) * 256],
                   sl == 0 and nb % 2 == 0, sl == 127, [Bdg] + Bg, [B_acc[nb // 2]], skip=True)
        x2 = SC[:, 0:D]
        TT("dve", x2[:P], psACC[:P, :], gt2b[:P, :], ALU.mult, B_acc + [B_gtb], [B_SC])
        TT("dve", x2[:P], x2[:P], xt[:P, :], ALU.add, [B_SC, B_xt], [B_SC])
        ms = small[:, 0:1]
        ACT(xsb[:P, :], x2[:P], AF.Square, [B_SC], [B_xsb, B_small], accum_out=ms[:P])
        TS("dve", ms[:P], ms[:P], 1.0 / D, EPS, ALU.mult, ALU.add, [B_small], [B_small])
        ACT(ms[:P], ms[:P], AF.Sqrt, [B_small], [B_small])
        S.op("dve", lambda e: e.reciprocal(out=small[:P, 1:2], in_=ms[:P]), [B_small], [B_small])
        STT(xt[:P, :], x2[:P], small[:P, 1:2], gfin[:P, :], ALU.mult, ALU.mult, [B_SC, B_small, B_gfin], [B_xt])
        DMA("pool", y_rows, xt[:P, :], [B_xt], [outbuf()])

    sb2_hn2 = sb([128, D], BF16, "hn2"); B_hn2 = Buf()
    pst_small = sb([128, 160], F32, "pst"); B_pst = Buf()
    pst_small_u = pst_small[:, :].bitcast(U32)
    eidxf = sb([128, 128], F32, "eidxf"); B_eidx = Buf()
    eidx = sb([128, 128], I32, "eidx"); B_eidxi = Buf()
    gw = sb([128, 128], F32, "gw"); B_gw = Buf()
    actv = sb([128, 128], F32, "actv"); B_actv = [Buf() for _ in range(8)]
    gel = sb([128, 128], F32, "gel"); B_gel = [Buf() for _ in range(8)]
    uvg = [SEL[:, 0:2 * D], PB[:, 0:2 * D], PTB[:, 0:2 * D], KTB[:, 0:2 * D], VB[:, 0:2 * D]]
    B_uvg = [B_SEL, B_PB, B_PTB, [B_KTB], [B_VB]]
    dgs = [sb([128, 128], BF16, "dg%d" % i) for i in range(2)]; B_dgs = [Buf(), Buf()]
    cur = {"b": 0}
    halo_st = sb([128, 8, 3], F32, "halo_st"); B_halo = Buf()

    def load_seq_mod(b):
        cur["b"] = b
        DMA("sp", gt1b[:], modrow_d.ap()[b:b + 1, 2 * D:3 * D].partition_broadcast(128), [B_modrow], [B_gtb])
        DMA("sp", gt2b[:], modrow_d.ap()[b:b + 1, 5 * D:6 * D].partition_broadcast(128), [B_modrow], [B_gtb])

    STAGE = cfg.get("STAGE", 99)
    pseq = make_seq("p", 0, SP_MAX, cfg["TOPK_P"], SP_MAX // 128)
    load_seq_mod(0)
    MS("dve", hprev[:], 0.0, [B_hprev])
    MS("dve", xr_ext[:, :, 0:3], 0.0, [B_xr])
    S_CTX = NCT * 128
    for g in range(NCT // GT if STAGE >= 2 else 0):
        T = GT * 128
        tiles = []
        for t in range(GT):
            row0 = (g * GT + t) * 128
            rms_hnT(x_ctx.ap()[row0:row0 + 128, :], 128, 0, t * 128, 0)
            tiles.append((t * 128, 128, row0))
        SUB = cfg.get("SUB", 99)
        if SUB >= 2:
            projections(pseq, T, tiles, False, g * T, None, None, None)
        for t in range(GT if SUB >= 3 else 0):
            lru_tile(t * 128, 128, False, "always" if (g == 0 and t == 0) else None, t)
        CP("dve", xr_ext[:, :, 0:3], xr_ext[:, :, T:T + 3], [B_xr], [B_xr])
    conv_join()
    TS("dve", hprev[:], hprev[:], flags[:, 0:1], None, ALU.mult, None, [B_hprev, B_flags], [B_hprev])
    TS("dve", xr_ext[:, :, 0:3], xr_ext[:, :, 0:3], flags[:, 0:1], None, ALU.mult, None, [B_xr, B_flags], [B_xr])
    for g in range(NOT // GT if STAGE >= 3 else 0):
        T = GT * 128
        tiles = []
        for t in range(GT):
            row0 = (g * GT + t) * 128
            rms_hnT(x_own.ap()[row0:row0 + 128, :], 128, 0, t * 128, 0)
            tiles.append((t * 128, 128, row0))
        projections(pseq, T, tiles, True, S_CTX + g * T, k_own_d.ap(), v_own_d.ap(), ki_own_d.ap())
        for t in range(GT if cfg.get("SUB3", 99) >= 2 else 0):
            lru_tile(t * 128, 128, True, "flag" if (g == 0 and t == 0) else None, t)
        if g == NOT // GT - 1 and cfg.get("SUB3", 99) >= 3:
            CP("dve", halo_st[:], xr_ext[:, :, T:T + 3], [B_xr], [B_halo])
            DMA("pool", conv_p_d.ap(), halo_st[:], [B_halo], [outbuf()])
            DMA("pool", lru_p_d.ap(), hprev[:], [B_hprev], [outbuf()])
        CP("dve", xr_ext[:, :, 0:3], xr_ext[:, :, T:T + 3], [B_xr], [B_xr])
        for t in range(GT):
            row0 = (g * GT + t) * 128
            if STAGE >= 4:
                attention_tile(pseq, t * 128, 128, S_CTX + g * T + t * 128, S_CTX, t)
            if STAGE >= 5:
                mix_residual(x_own.ap()[row0:row0 + 128, :], 128, t * 128, t)
            if STAGE >= 6:
                peer_tile(128, y_own_d.ap()[row0:row0 + 128, :])

    for sbi in range(2 if STAGE >= 7 else 0):
        sseq = make_seq("s%d" % sbi, 1 + sbi, SS_MAX, cfg["TOPK_S"], SS_MAX // 128)
        load_seq_mod(1 + sbi)
        kst = [PB[:, 0:4096].rearrange("p (n c) -> p n c", n=4), VB[:, 0:4096].rearrange("p (n c) -> p n c", n=4)]
        B_kst = [B_PB, [B_VB]]
        kts = [PTB[:, 0:4096].rearrange("p (h s) -> p h s", h=8), KTB[:, 0:4096].rearrange("p (h s) -> p h s", h=8)]
        B_kts = [B_PTB, [B_KTB]]
        vst = [SEL[:, 0:4096].rearrange("p (n c) -> p n c", n=4), SC[:, 0:2048].bitcast(BF16).rearrange("p (n c) -> p n c", n=4)]
        B_vst = [B_SEL, [B_SC]]
        for cb4 in range(PAST // 512):
            r0 = cb4 * 512
            i = cb4 % 2
            regs4 = sseq.B_reg[cb4 * 4:cb4 * 4 + 4]
            DMA("pool", kst[i], ck_d.ap()[sbi, r0:r0 + 512, :].rearrange("(n p) c -> p n c", p=128), (), B_kst[i])
            for n in range(4):
                pt, Bpt = next_t()
                for h in range(8):
                    TR(pt[:, h * 128:(h + 1) * 128], kst[i][:, n, h * 128:(h + 1) * 128], ident[:, :], B_kst[i] + [B_ident], [Bpt])
                CP("act" if n % 2 == 0 else "dve", kts[i][:, :, n * 128:(n + 1) * 128], pt[:, :].rearrange("p (h s) -> p h s", h=8),
                   [Bpt], B_kts[i])
            for h in range(8):
                DMA("sp", sseq.kT.ap()[h, :, r0:r0 + 512], kts[i][:, h, :], B_kts[i], regs4)
            DMA("pool", vst[i], cv_d.ap()[sbi, r0:r0 + 512, :].rearrange("(n p) c -> p n c", p=128), (), B_vst[i])
            for h in range(8):
                DMA("sp", sseq.v.ap()[h, :, cb4 * 4:cb4 * 4 + 4, :], vst[i][:, :, h * 128:(h + 1) * 128], B_vst[i], regs4)
            kis = xsb[:, 0:1024].rearrange("p (n c) -> p n c", n=4)
            DMA("pool", kis[:, :, 0:64], cki_d.ap()[sbi, r0:r0 + 512, :].rearrange("(n p) c -> p n c", p=128), (), [B_xsb])
            CP("dve", kis[:, :, 64:128], kis[:, :, 0:64], [B_xsb], [B_xsb])
            pt, Bpt = next_t()
            for n in range(4):
                TR(pt[:, n * 128:(n + 1) * 128], kis[:, n, 0:128], ident[:, :], [B_xsb, B_ident], [Bpt])
            CP("dve", sb2_hn2[:, 0:512], pt[:, 0:512], [Bpt], [B_hn2])
            DMA("sp", sseq.kiT.ap()[:, r0:r0 + 512], sb2_hn2[:, 0:512], [B_hn2], regs4)
        DMA("sp", hprev[:], stl_d.ap()[sbi], (), [B_hprev])
        DMA("sp", halo_st[:], stc_d.ap()[sbi], (), [B_halo])
        CP("dve", xr_ext[:, :, 0:3], halo_st[:], [B_halo], [B_xr])
        T = 64
        rms_hnT(xs_d.ap()[sbi], 64, 1 + sbi, 0, 0)
        tiles = [(0, 64, 0)]
        projections(sseq, T, tiles, True, PAST, ks_d.ap()[sbi], vs_d.ap()[sbi], kis_d.ap()[sbi])
        lru_tile(0, 64, True, None, 0)
        CP("dve", halo_st[:], xr_ext[:, :, T:T + 3], [B_xr], [B_halo])
        DMA("pool", conv_s_d.ap()[sbi], halo_st[:], [B_halo], [outbuf()])
        DMA("pool", lru_s_d.ap()[sbi], hprev[:], [B_hprev], [outbuf()])
        attention_tile(sseq, 0, 64, PAST, 0, 0)
        mix_residual(xs_d.ap()[sbi], 64, 0, 0)
        peer_tile(64, ys_d.ap()[sbi])

    S.final_waits("sp", OUTB)
    keys = S.semkey_list()
    semmap = {}
    for i, k in enumerate(keys):
        semmap[k] = es.enter_context(nc.semaphore("sem%d" % i))
    with nc.Block() as block:
        @block.tensor
        def _(e):
            S.emit("pe", e, semmap)

        @block.scalar
        def _(e):
            S.emit("act", e, semmap)

        @block.vector
        def _(e):
            S.emit("dve", e, semmap)

        @block.gpsimd
        def _(e):
            S.emit("pool", e, semmap)

        @block.sync
        def _(e):
            S.emit("sp", e, semmap)
    es.close()
    return nc, S


def _t5_onehot():
    import jax
    import jax.numpy as jnp
    with jax.default_device(jax.devices("cpu")[0]):
        rel = jnp.arange(-255, 128, dtype=jnp.int32)
        half = 16
        max_exact = 8
        ret = jnp.where(rel > 0, half, 0)
        n = jnp.abs(rel)
        nf = jnp.maximum(n, 1).astype(jnp.float32)
        large = max_exact + (jnp.log(nf / max_exact) / math.log(128 / max_exact) * (half - max_exact)).astype(jnp.int32)
        large = jnp.minimum(large, half - 1)
        bucket = np.asarray(ret + jnp.where(n < max_exact, n, large))
    oh = np.zeros((32, 383), np.float32)
    oh[bucket, np.arange(383)] = 1.0
    return oh


def chan_major(v):
    return np.ascontiguousarray(np.swapaxes(v.reshape(v.shape[:-1] + (8, 128)), -1, -2))


def prepare_inputs(cfg, x_prompt, x_sample, cache_k, cache_v, cache_kidx, state_lru, state_conv,
                   c_prompt, c_sample, w_ada, b_ada, g_norm1, g_norm2, w_in, conv_w, conv_b,
                   lru_wa, lru_ba, lru_wx, lru_bx, lru_lam, w_out, peer_wq, peer_keys,
                   peer_u, peer_v, rel_bias, g_final):
    NCT, NOT, PAST = cfg["NCT"], cfg["NOT"], cfg["PAST"]
    f = lambda a: np.ascontiguousarray(np.asarray(a, dtype=np.float32))
    shared = {
        "w_ada": f(w_ada[0]), "b_ada": f(b_ada[0]).reshape(1, -1),
        "g1T": f(g_norm1[0].reshape(16, 128).T), "g2T": f(g_norm2[0].reshape(16, 128).T),
        "w_in": f(w_in[0]),
        "conv_wT": f(np.transpose(conv_w[0].reshape(4, 8, 128), (2, 0, 1)).reshape(128, 32)),
        "conv_bT": f(conv_b[0].reshape(8, 128).T),
        "lru_waT": f(np.transpose(lru_wa[0], (1, 0, 2)).reshape(128, 1024)),
        "lru_wxT": f(np.transpose(lru_wx[0], (1, 0, 2)).reshape(128, 1024)),
        "lru_baT": f(lru_ba[0].reshape(8, 128).T), "lru_bxT": f(lru_bx[0].reshape(8, 128).T),
        "lru_lamT": f(lru_lam[0].reshape(8, 128).T),
        "w_out": f(w_out[0]), "peer_wq": f(peer_wq[0]),
        "peer_keysT": f(np.transpose(peer_keys[0], (0, 3, 1, 2)).reshape(128, 1024)),
        "peer_u": f(peer_u[0]), "peer_v": f(peer_v[0]),
        "rel_bias": f(rel_bias), "g_final": f(g_final).reshape(1, -1), "oh": _t5_onehot(),
    }
    maps = []
    HALF = NOT * 128
    for c in range(8):
        b, half = c // 2, c % 2
        m = dict(shared)
        m["x_ctx"] = f(x_prompt[b, 0:NCT * 128])
        m["x_own"] = f(x_prompt[b, half * HALF:(half + 1) * HALF])
        sl = slice(2 * c, 2 * c + 2)
        m["xs"] = f(x_sample[sl])
        m["ck"] = f(cache_k[0, sl].reshape(2, PAST, 1024))
        m["cv"] = f(cache_v[0, sl].reshape(2, PAST, 1024))
        m["cki"] = f(cache_kidx[0, sl])
        m["st_lru"] = f(chan_major(state_lru[0, sl]))
        m["st_conv"] = f(np.transpose(chan_major(state_conv[0, sl]), (0, 2, 3, 1)))
        c3 = np.stack([c_prompt[b], c_sample[2 * c], c_sample[2 * c + 1]])
        m["c3T"] = f(np.transpose(c3.reshape(3, 16, 128), (2, 1, 0)).reshape(128, 48))
        fl = np.zeros((128, 4), np.float32)
        fl[:, 0] = float(half)
        fl[:, 1] = 1.0 - float(half)
        fl[:, 2] = 0.0 if half else NEG
        m["flags"] = fl
        maps.append(m)
    return maps


def assemble(cfg, res):
    NOT, PAST = cfg["NOT"], cfg["PAST"]
    HALF = NOT * 128
    SEQ = 2 * HALF
    y_p = np.zeros((4, SEQ, D), np.float32)
    y_s = np.zeros((16, 64, D), np.float32)
    k_p = np.zeros((1, 4, SEQ, 8, 128), np.float32)
    v_p = np.zeros((1, 4, SEQ, 8, 128), np.float32)
    ki_p = np.zeros((1, 4, SEQ, 64), np.float32)
    lru_p = np.zeros((1, 4, 1024), np.float32)
    conv_p = np.zeros((1, 4, 3, 1024), np.float32)
    k_s = np.zeros((1, 16, 64, 8, 128), np.float32)
    v_s = np.zeros((1, 16, 64, 8, 128), np.float32)
    ki_s = np.zeros((1, 16, 64, 64), np.float32)
    lru_s = np.zeros((1, 16, 1024), np.float32)
    conv_s = np.zeros((1, 16, 3, 1024), np.float32)
    for c in range(8):
        r = res[c]
        b, half = c // 2, c % 2
        sl = slice(half * HALF, (half + 1) * HALF)
        y_p[b, sl] = r["y_own"]
        k_p[0, b, sl] = r["k_own"].reshape(HALF, 8, 128)
        v_p[0, b, sl] = r["v_own"].reshape(HALF, 8, 128)
        ki_p[0, b, sl] = r["ki_own"]
        if half == 1:
            lru_p[0, b] = r["lru_p"].T.reshape(1024)
            conv_p[0, b] = np.transpose(r["conv_p"], (2, 1, 0)).reshape(3, 1024)
        for i in range(2):
            sbi = 2 * c + i
            y_s[sbi] = r["ys"][i]
            k_s[0, sbi] = r["ks"][i].reshape(64, 8, 128)
            v_s[0, sbi] = r["vs"][i].reshape(64, 8, 128)
            ki_s[0, sbi] = r["kis"][i]
            lru_s[0, sbi] = r["lru_s"][i].T.reshape(1024)
            conv_s[0, sbi] = np.transpose(r["conv_s"][i], (2, 1, 0)).reshape(3, 1024)
    return (y_p, y_s, k_p, v_p, ki_p, lru_p, conv_p, k_s, v_s, ki_s, lru_s, conv_s)


_CACHE = {}


def kernel(**inputs):
    cfg = make_cfg(True)
    if "nc" not in _CACHE:
        _CACHE["nc"] = build(cfg)[0]
    nc = _CACHE["nc"]
    maps = prepare_inputs(cfg, **{k: np.asarray(v) for k, v in inputs.items()})
    res = run_bass_kernel_spmd(nc, maps, core_ids=list(range(8)))
    return assemble(cfg, res.results)
```

```python
import math
import os
from contextlib import ExitStack

import numpy as np
import concourse.bass as bass
import concourse.mybir as mybir
from concourse.bass_utils import run_bass_kernel_spmd

F32 = mybir.dt.float32
BF16 = mybir.dt.bfloat16
I32 = mybir.dt.int32
U32 = mybir.dt.uint32
AF = mybir.ActivationFunctionType
ALU = mybir.AluOpType
AX = mybir.AxisListType

D = 2048
KC = 16
DIN = 6224
NEG = -1.0e30
EPS = 1e-6
NIT = 20
PW = 256
SAME_ENGINE_SYNC = os.environ.get("SES", "1") == "1"


class Buf:
    __slots__ = ("w", "r", "x")

    def __init__(self, x=False):
        self.w = None
        self.r = []
        self.x = x


class Sched:
    ENG = ("pe", "act", "dve", "pool", "sp")

    def __init__(self, n_lanes=10):
        self.q = {e: [] for e in self.ENG}
        self.cnt = {e: 0 for e in self.ENG}
        self.known = {e: {} for e in self.ENG}
        self.n_lanes = n_lanes
        self.lane_val = {}
        self.lane_next = {"sp": 0, "act": 0, "pool": 0}
        self.ninst = 0

    def semkey_list(self):
        keys = list(self.ENG[:4])
        for qn in ("sp", "act", "pool"):
            for l in range(self.n_lanes):
                keys.append(("lane", qn, l))
        return keys

    def _deps(self, eng, reads, writes):
        need = {}

        def add(d):
            if d is None:
                return
            k, v = d
            if k == eng and (eng == "pe" or not SAME_ENGINE_SYNC):
                return
            if need.get(k, 0) < v:
                need[k] = v

        for b in reads:
            add(b.w)
            if b.x:
                for d in b.r:
                    if d[0] != eng:
                        add(d)
        for b in writes:
            add(b.w)
            for d in b.r:
                add(d)
        out = []
        kn = self.known[eng]
        for k, v in need.items():
            if kn.get(k, 0) >= v:
                continue
            kn[k] = v
            out.append((k, v))
        return out

    def op(self, eng, fn, reads=(), writes=()):
        waits = self._deps(eng, reads, writes)
        self.cnt[eng] += 1
        v = self.cnt[eng]
        self.q[eng].append((waits, fn, (eng, 1)))
        for b in reads:
            b.r.append((eng, v))
        for b in writes:
            b.w = (eng, v)
            b.r = []
        self.ninst += 1

    def dma(self, qeng, fn, reads=(), writes=()):
        lane = self.lane_next[qeng]
        self.lane_next[qeng] = (lane + 1) % self.n_lanes
        key = ("lane", qeng, lane)
        waits = self._deps(qeng, reads, writes)
        prev = self.lane_val.get(key, 0)
        if prev > 0 and self.known[qeng].get(key, 0) < prev:
            self.known[qeng][key] = prev
            waits.append((key, prev))
        v = prev + 16
        self.lane_val[key] = v
        self.q[qeng].append((waits, fn, (key, 16)))
        for b in reads:
            b.r.append((key, v))
        for b in writes:
            b.w = (key, v)
            b.r = []
        self.ninst += 1

    def final_waits(self, eng, bufs):
        waits = {}
        for b in bufs:
            if b.w is not None:
                k, v = b.w
                waits[k] = max(waits.get(k, 0), v)
        self.q[eng].append((list(waits.items()), None, None))

    def emit(self, eng, eobj, semmap):
        for waits, fn, inc in self.q[eng]:
            for k, v in waits:
                eobj.wait_ge(semmap[k], v)
            if fn is None:
                continue
            ins = fn(eobj)
            ins.then_inc(semmap[inc[0]], inc[1])


def make_cfg(full=True):
    if full:
        return dict(NCT=16, NOT=16, PAST=4096, GT=2, TOPK_P=256, TOPK_S=256)
    return dict(NCT=2, NOT=2, PAST=512, GT=2, TOPK_P=128, TOPK_S=144)


def build(cfg):
    NCT, NOT, PAST, GT = cfg["NCT"], cfg["NOT"], cfg["PAST"], cfg["GT"]
    SP_MAX = (NCT + NOT) * 128
    SS_MAX = PAST + 128
    SMAXT = max(SP_MAX, SS_MAX)
    AW = max(SMAXT, 4224)
    nc = bass.Bass("TRN2", target_bir_lowering=False)
    S = Sched()
    es = ExitStack()

    def din(name, shape, dt=F32):
        return nc.dram_tensor(name, list(shape), dt, kind="ExternalInput")

    def dout(name, shape, dt=F32):
        return nc.dram_tensor(name, list(shape), dt, kind="ExternalOutput")

    def dscr(name, shape, dt):
        return nc.dram_tensor(name, list(shape), dt)

    _cnt = [0]

    def sb(shape, dt, name=None):
        _cnt[0] += 1
        return es.enter_context(nc.sbuf_tensor("sb%d_%s" % (_cnt[0], name or "t"), list(shape), dt))

    def ps(shape, dt, name=None):
        _cnt[0] += 1
        return es.enter_context(nc.psum_tensor("ps%d_%s" % (_cnt[0], name or "t"), list(shape), dt))

    x_ctx = din("x_ctx", [NCT * 128, D])
    x_own = din("x_own", [NOT * 128, D])
    xs_d = din("xs", [2, 64, D])
    ck_d = din("ck", [2, PAST, 1024])
    cv_d = din("cv", [2, PAST, 1024])
    cki_d = din("cki", [2, PAST, 64])
    stl_d = din("st_lru", [2, 128, 8])
    stc_d = din("st_conv", [2, 128, 8, 3])
    c3T_d = din("c3T", [128, KC * 3])
    flags_d = din("flags", [128, 4])
    wada_d = din("w_ada", [D, 6 * D])
    bada_d = din("b_ada", [1, 6 * D])
    g1T_d = din("g1T", [128, KC])
    g2T_d = din("g2T", [128, KC])
    win_d = din("w_in", [D, DIN])
    cw_d = din("conv_wT", [128, 4 * 8])
    cb_d = din("conv_bT", [128, 8])
    wa_d = din("lru_waT", [128, 8 * 128])
    wx_d = din("lru_wxT", [128, 8 * 128])
    ba_d = din("lru_baT", [128, 8])
    bx_d = din("lru_bxT", [128, 8])
    lam_d = din("lru_lamT", [128, 8])
    wout_d = din("w_out", [D, D])
    wq_d = din("peer_wq", [D, 1024])
    pk_d = din("peer_keysT", [128, 8 * 128])
    pu_d = din("peer_u", [16384, D])
    pv_d = din("peer_v", [16384, D])
    rb_d = din("rel_bias", [32, 8])
    gf_d = din("g_final", [1, D])
    oh_d = din("oh", [32, 383])

    y_own_d = dout("y_own", [NOT * 128, D])
    ys_d = dout("ys", [2, 64, D])
    k_own_d = dout("k_own", [NOT * 128, 1024])
    v_own_d = dout("v_own", [NOT * 128, 1024])
    ki_own_d = dout("ki_own", [NOT * 128, 64])
    lru_p_d = dout("lru_p", [128, 8])
    conv_p_d = dout("conv_p", [128, 8, 3])
    ks_d = dout("ks", [2, 64, 1024])
    vs_d = dout("vs", [2, 64, 1024])
    kis_d = dout("kis", [2, 64, 64])
    lru_s_d = dout("lru_s", [2, 128, 8])
    conv_s_d = dout("conv_s", [2, 128, 8, 3])
    OUTB = []

    def outbuf():
        b = Buf()
        OUTB.append(b)
        return b

    modrow_d = dscr("modrow", [3, 6 * D], F32)
    B_modrow = Buf()
    bsc_d = dscr("bsc", [8, 128, 383], F32)
    bv_d = dscr("bv", [8, 383], F32)

    def ACT(out, in_, func, R, W, scale=1.0, bias=0.0, accum_out=None):
        if accum_out is None:
            S.op("act", lambda e: e.activation(out=out, in_=in_, func=func, scale=scale, bias=bias), R, W)
        else:
            S.op("act", lambda e: e.activation(out=out, in_=in_, func=func, scale=scale, bias=bias, accum_out=accum_out), R, W)

    def MM(out, lhsT, rhs, st, sp_, R, W, skip=False):
        if skip:
            S.op("pe", lambda e: e.matmul(out, lhsT=lhsT, rhs=rhs, start=st, stop=sp_, skip_group_check=True), R, W)
        else:
            S.op("pe", lambda e: e.matmul(out, lhsT=lhsT, rhs=rhs, start=st, stop=sp_), R, W)

    def TR(out, in_, ident, R, W):
        S.op("pe", lambda e: e.transpose(out=out, in_=in_, identity=ident), R, W)

    def TS(eng, out, in0, s1, s2, op0, op1, R, W, accum_out=None):
        if op1 is None:
            if accum_out is None:
                S.op(eng, lambda e: e.tensor_scalar(out=out, in0=in0, scalar1=s1, scalar2=None, op0=op0), R, W)
            else:
                raise ValueError
        elif accum_out is None:
            S.op(eng, lambda e: e.tensor_scalar(out=out, in0=in0, scalar1=s1, scalar2=s2, op0=op0, op1=op1), R, W)
        else:
            S.op(eng, lambda e: e.tensor_scalar(out=out, in0=in0, scalar1=s1, scalar2=s2, op0=op0, op1=op1, accum_out=accum_out), R, W)

    def TT(eng, out, in0, in1, op, R, W):
        S.op(eng, lambda e: e.tensor_tensor(out=out, in0=in0, in1=in1, op=op), R, W)

    def STT(out, in0, scalar, in1, op0, op1, R, W, accum_out=None):
        if accum_out is None:
            S.op("dve", lambda e: e.scalar_tensor_tensor(out=out, in0=in0, scalar=scalar, in1=in1, op0=op0, op1=op1), R, W)
        else:
            S.op("dve", lambda e: e.scalar_tensor_tensor(out=out, in0=in0, scalar=scalar, in1=in1, op0=op0, op1=op1, accum_out=accum_out), R, W)

    def CP(eng, out, in_, R, W):
        if eng == "act":
            S.op("act", lambda e: e.copy(out=out, in_=in_), R, W)
        else:
            S.op(eng, lambda e: e.tensor_copy(out=out, in_=in_), R, W)

    def MS(eng, ap, val, W):
        S.op(eng, lambda e: e.memset(ap, val), (), W)

    def DMA(q, out, in_, R, W):
        S.dma(q, lambda e: e.dma_start(out=out, in_=in_), R, W)

    identf = sb([128, 128], F32, "identf"); B_identf = Buf()
    ident = sb([128, 128], BF16, "ident"); B_ident = Buf()
    MS("pool", identf[:], 0.0, [B_identf])
    S.op("pool", lambda e: e.affine_select(out=identf[:], in_=identf[:], pattern=[[-1, 128]], compare_op=ALU.not_equal,
                                           fill=1.0, base=0, channel_multiplier=1), [B_identf], [B_identf])
    CP("dve", ident[:], identf[:], [B_identf], [B_ident])
    iota256 = sb([128, 256], F32, "iota"); B_iota = Buf()
    S.op("pool", lambda e: e.iota(iota256[:], pattern=[[1, 256]], base=0, channel_multiplier=0,
                                  allow_small_or_imprecise_dtypes=True), (), [B_iota])
    pow2 = sb([128, NIT + 1], F32, "pow2"); B_pow2 = Buf()
    for k in range(NIT + 1):
        MS("pool", pow2[:, k:k + 1], 2.0 ** (-k), [B_pow2])
    flags = sb([128, 4], F32, "flags"); B_flags = Buf()
    DMA("sp", flags[:], flags_d.ap(), (), [B_flags])

    def ldc(dram, shape, name):
        t = sb(shape, F32, name); b = Buf()
        DMA("sp", t[:], dram.ap(), (), [b])
        return t, b

    cw, B_cw = ldc(cw_d, [128, 32], "cw")
    cb, B_cb = ldc(cb_d, [128, 8], "cb")
    ba, B_ba = ldc(ba_d, [128, 8], "ba")
    bx, B_bx = ldc(bx_d, [128, 8], "bx")
    lam, B_lam = ldc(lam_d, [128, 8], "lam")
    g1T, B_g1T = ldc(g1T_d, [128, KC], "g1T")
    g2T, B_g2T = ldc(g2T_d, [128, KC], "g2T")
    keysT, B_keysT = ldc(pk_d, [128, 1024], "keysT")
    gfin = sb([128, D], F32, "gfin"); B_gfin = Buf()
    DMA("sp", gfin[:], gf_d.ap()[0:1, :].partition_broadcast(128), (), [B_gfin])
    bfar = sb([128, 8], F32, "bfar"); B_bfar = Buf()
    DMA("sp", bfar[:], rb_d.ap()[15:16, :].partition_broadcast(128), (), [B_bfar])
    wa_b = sb([128, 1024], BF16, "wa_b"); B_wa = Buf()
    wx_b = sb([128, 1024], BF16, "wx_b"); B_wx = Buf()
    DMA("pool", wa_b[:], wa_d.ap(), (), [B_wa])
    DMA("pool", wx_b[:], wx_d.ap(), (), [B_wx])
    c1 = sb([128, 8], F32, "c1"); c2 = sb([128, 8], F32, "c2"); B_c12 = Buf()
    ACT(c1[:], lam[:], AF.Exp, [B_lam], [B_c12], scale=-1.0)
    ACT(c1[:], c1[:], AF.Ln, [B_c12], [B_c12], bias=1.0)
    TS("dve", c2[:], c1[:], -16.0, None, ALU.mult, None, [B_c12], [B_c12])
    TS("dve", c1[:], c1[:], -8.0, None, ALU.mult, None, [B_c12], [B_c12])

    psA = ps([128, 512], F32, "psA"); B_psA = Buf(True)
    psB = ps([128, 512], F32, "psB"); B_psB = Buf(True)
    psT = ps([128, 1024], BF16, "psT"); B_psT = Buf(True)
    psT2 = ps([128, 1024], BF16, "psT2"); B_psT2 = Buf(True)
    psACC = ps([128, 2048], F32, "psACC"); B_acc = [Buf(True) for _ in range(4)]
    rot = {"ab": 0, "t": 0}

    def next_ab():
        rot["ab"] ^= 1
        return (psA, B_psA) if rot["ab"] else (psB, B_psB)

    def next_t():
        rot["t"] ^= 1
        return (psT, B_psT) if rot["t"] else (psT2, B_psT2)

    SC = sb([128, AW], F32, "SC"); B_SC = Buf()
    SEL = sb([128, AW], BF16, "SEL"); B_SEL = [Buf(), Buf()]
    PB = sb([128, AW], BF16, "PB"); B_PB = [Buf(), Buf()]
    PTB = sb([128, AW], BF16, "PTB"); B_PTB = [Buf(), Buf()]
    KTB = sb([128, AW], BF16, "KTB"); B_KTB = Buf()
    VB = sb([128, AW], BF16, "VB"); B_VB = Buf()
    small = sb([128, 64], F32, "small"); B_small = Buf()
    B_stg = [Buf(), Buf()]
    uv_d = dscr("uv_scr", [16384, 2 * D], BF16)
    B_uv = Buf()
    wscr = {}
    conv_pending = []
    for wd, ncols in ((win_d, DIN), (wout_d, D), (wq_d, 1024)):
        scr = dscr("bf_" + wd.name, [D, ncols], BF16)
        bufs = {}
        for part, p0 in enumerate(range(0, ncols, 2048)):
            p1 = min(ncols, p0 + 2048)
            for kc in range(KC):
                b_ = Buf()
                bufs[(kc, part)] = b_
                conv_pending.append((wd.ap()[kc * 128:(kc + 1) * 128, p0:p1], scr.ap()[kc * 128:(kc + 1) * 128, p0:p1], p1 - p0, b_))
        wscr[wd.name] = (scr, bufs)
    conv_bufs = []
    for r in range(128):
        for half, src in ((0, pu_d), (1, pv_d)):
            b_ = Buf()
            conv_bufs.append(b_)
            conv_pending.append((src.ap()[r * 128:(r + 1) * 128, :], uv_d.ap()[r * 128:(r + 1) * 128, half * D:(half + 1) * D], D, b_))
    conv_state = {"i": 0}

    def conv_step(n):
        for _ in range(n):
            if not conv_pending:
                return
            src, dst, width, b_ = conv_pending.pop(0)
            i = conv_state["i"]
            conv_state["i"] ^= 1
            stg = VB[:, i * D:i * D + width]
            DMA("pool", stg, src, (), [B_stg[i]])
            DMA("sp", dst, stg, [B_stg[i]], [b_])

    def conv_join():
        conv_step(10 ** 9)
        S.op("dve", lambda e: e.memset(small[:, 63:64], 0.0), conv_bufs, [B_uv, B_VB, B_stg[0], B_stg[1], B_small])

    NPB = 2
    wpb = [sb([128, KC, PW], BF16, "wp%d" % i) for i in range(NPB)]
    B_wpb = [Buf() for _ in range(NPB)]
    wrot = [0]

    def load_piece(wd, c0, width):
        i = wrot[0]
        wrot[0] = (i + 1) % NPB
        if wd.name in wscr:
            scr, bufs = wscr[wd.name]
            part = c0 // 2048
            while any(cp[3] is bufs[(kc, part)] for cp in conv_pending for kc in range(KC)):
                conv_step(1)
            src = scr.ap()[:, c0:c0 + width].rearrange("(kc p) c -> p kc c", p=128)
            DMA("pool", wpb[i][:, :, 0:width], src, [bufs[(kc, part)] for kc in range(KC)], [B_wpb[i]])
        else:
            src = wd.ap()[:, c0:c0 + width].rearrange("(kc p) c -> p kc c", p=128)
            DMA("pool", wpb[i][:, :, 0:width], src, (), [B_wpb[i]])
        conv_step(3)
        return wpb[i], B_wpb[i]

    c3T = sb([128, KC * 3], F32, "c3T"); B_c3T = Buf()
    DMA("sp", c3T[:], c3T_d.ap(), (), [B_c3T])
    sc3 = sb([128, KC * 3], BF16, "sc3"); B_sc3 = Buf()
    ACT(sc3[:], c3T[:], AF.Silu, [B_c3T], [B_sc3])
    modT = sb([128, 4, KC, 3], F32, "modT"); B_modT = Buf()
    mrow = [sb([3, PW], F32, "mrow%d" % i) for i in range(2)]; B_mrow = [Buf(), Buf()]
    bad = [sb([3, PW], F32, "bad%d" % i) for i in range(2)]; B_bad = [Buf(), Buf()]
    for pcs in range(6 * D // PW):
        c0 = pcs * PW
        wp, Bwp = load_piece(wada_d, c0, PW)
        pa, Bpa = next_ab()
        for kc in range(KC):
            MM(pa[0:3, 0:PW], sc3[:, kc * 3:kc * 3 + 3], wp[:, kc, :], kc == 0, kc == KC - 1, [B_sc3, Bwp], [Bpa])
        i = pcs % 2
        DMA("sp", bad[i][:], bada_d.ap()[0:1, c0:c0 + PW].partition_broadcast(3), (), [B_bad[i]])
        TT("dve", mrow[i][:], pa[0:3, 0:PW], bad[i][:], ALU.add, [Bpa, B_bad[i]], [B_mrow[i]])
        DMA("sp", modrow_d.ap()[:, c0:c0 + PW], mrow[i][:], [B_mrow[i]], [B_modrow])
        j6 = c0 // D
        if j6 in (0, 1, 3, 4):
            jj = {0: 0, 1: 1, 3: 2, 4: 3}[j6]
            for cc in range(PW // 128):
                kc = (c0 % D) // 128 + cc
                pa2, Bpa2 = next_ab()
                MM(pa2[:, 0:3], mrow[i][0:3, cc * 128:(cc + 1) * 128], identf[0:3, 0:3], True, True, [B_mrow[i], B_identf], [Bpa2])
                CP("dve", modT[:, jj, kc, :], pa2[:, 0:3], [Bpa2], [B_modT])
    for jj, (gT, Bg) in ((1, (g1T, B_g1T)), (3, (g2T, B_g2T))):
        TS("dve", modT[:, jj, :, :], modT[:, jj, :, :], 1.0, None, ALU.add, None, [B_modT], [B_modT])
        TT("dve", modT[:, jj, :, :], modT[:, jj, :, :], gT[:, :].unsqueeze(2).to_broadcast([128, KC, 3]), ALU.mult, [B_modT, Bg], [B_modT])

    rbt = sb([32, 8], F32, "rbt"); B_rbt = Buf()
    oht = sb([32, 383], F32, "oht"); B_oht = Buf()
    DMA("sp", rbt[:], rb_d.ap(), (), [B_rbt])
    DMA("sp", oht[:], oh_d.ap(), (), [B_oht])
    pa, Bpa = next_ab()
    MM(pa[0:8, 0:383], rbt[:, :], oht[:, :], True, True, [B_rbt, B_oht], [Bpa])
    bvs = sb([8, 383], F32, "bvs"); B_bvs = Buf()
    CP("dve", bvs[:], pa[0:8, 0:383], [Bpa], [B_bvs])
    B_bvd = Buf(); B_bscd = Buf()
    DMA("sp", bv_d.ap(), bvs[:], [B_bvs], [B_bvd])
    bband = sb([128, 8, 256], F32, "bband"); B_bband = Buf()
    brep = sb([128, 383], F32, "brep"); B_brep = Buf()
    for h in range(8):
        DMA("sp", brep[:], bv_d.ap()[h:h + 1, :].partition_broadcast(128), [B_bvd], [B_brep])
        DMA("sp", bsc_d.ap()[h], brep[:], [B_brep], [B_bscd])
        src = bass.AP(bsc_d, h * 128 * 383 + 127, [[382, 128], [1, 256]])
        DMA("sp", bband[:, h, :], src, [B_bscd], [B_bband])
    TT("dve", bband[:], bband[:], bfar[:, :].unsqueeze(2).to_broadcast([128, 8, 256]), ALU.subtract, [B_bband, B_bfar], [B_bband])
    mxdb = sb([128, 8], F32, "mxdb"); B_mxdb = Buf()
    S.op("dve", lambda e: e.tensor_reduce(out=mxdb[:], in_=bband[:], axis=AX.X, op=ALU.max), [B_bband], [B_mxdb])
    TS("dve", mxdb[:], mxdb[:], 0.0, None, ALU.max, None, [B_mxdb], [B_mxdb])

    TG = GT * 128
    hnT = sb([128, KC, TG], BF16, "hnT"); B_hnT = Buf()
    xr_ext = sb([128, 8, 3 + TG], F32, "xr_ext"); B_xr = Buf()
    yrT = sb([128, 8, TG], F32, "yrT"); B_yr = Buf()
    qT = sb([128, 8, TG], BF16, "qT"); B_qT = Buf()
    qiT = sb([128, 8, TG], BF16, "qiT"); B_qiT = Buf()
    kTg = sb([128, 8, TG], BF16, "kTg"); B_kTg = Buf()
    kiTg = sb([64, TG], BF16, "kiTg"); B_kiTg = Buf()
    catT = sb([128, KC, TG], BF16, "catT"); B_cat = [Buf() for _ in range(GT)]
    wi_t = sb([128, GT, 16], F32, "wi_t"); B_wi = Buf()
    hprev = sb([128, 8], F32, "hprev"); B_hprev = Buf()
    xt = sb([128, D], F32, "xt"); B_xt = Buf()
    xsb = sb([128, D], BF16, "xsb"); B_xsb = Buf()
    tm32 = sb([128, GT * 1024], F32, "tm32"); B_tm32 = Buf()
    tm16 = sb([128, 1024], BF16, "tm16"); B_tm16 = Buf()
    gt1b = sb([128, D], F32, "gt1b"); gt2b = sb([128, D], F32, "gt2b"); B_gtb = Buf()

    def rms_hnT(x_rows, P, b, tcol, which):
        jsh, jg = (0, 1) if which == 0 else (2, 3)
        if x_rows is not None:
            DMA("sp", xt[:P, :], x_rows, (), [B_xt])
        ms = small[:, 0:1]
        ACT(xsb[:P, :], xt[:P, :], AF.Square, [B_xt], [B_xsb, B_small], accum_out=ms[:P])
        TS("dve", ms[:P], ms[:P], 1.0 / D, EPS, ALU.mult, ALU.add, [B_small], [B_small])
        ACT(ms[:P], ms[:P], AF.Sqrt, [B_small], [B_small])
        S.op("dve", lambda e: e.reciprocal(out=small[:P, 1:2], in_=ms[:P]), [B_small], [B_small])
        TS("dve", xsb[:P, :], xt[:P, :], small[:P, 1:2], None, ALU.mult, None, [B_xt, B_small], [B_xsb])
        for half in range(2):
            pt, Bpt = next_t()
            for j in range(8):
                kc = half * 8 + j
                TR(pt[:, j * 128:j * 128 + P], xsb[:P, kc * 128:(kc + 1) * 128], ident[:P, :P], [B_xsb, B_ident], [Bpt])
            for j in range(8):
                kc = half * 8 + j
                ACT(hnT[:, kc, tcol:tcol + P], pt[:, j * 128:j * 128 + P], AF.Identity, [Bpt, B_modT], [B_hnT],
                    scale=modT[:, jg, kc, b:b + 1], bias=modT[:, jsh, kc, b:b + 1])

    class Seq:
        pass

    def make_seq(name, b, smax, topk, nreg):
        s = Seq()
        s.b = b
        s.topk = topk
        s.kT = dscr("kT_" + name, [8, 128, smax], BF16)
        s.v = dscr("v_" + name, [8, 128, smax // 128, 128], BF16)
        s.kiT = dscr("kiT_" + name, [128, smax], BF16)
        s.B_reg = [Buf() for _ in range(nreg)]
        return s

    def proj_cm(wp, Bwp, off, M, T, dest, Rd, Wd, eng="act"):
        pa, Bpa = next_ab()
        for kc in range(KC):
            MM(pa[0:M, 0:T], wp[:, kc, off:off + M], hnT[:, kc, 0:T], kc == 0, kc == KC - 1, [Bwp, B_hnT], [Bpa])
        CP(eng, dest, pa[0:M, 0:T], [Bpa] + Rd, Wd)

    def projections(seq, T, tiles, own, kpos0, k_out, v_out, ki_out):
        def pieces(c0, c1):
            return [(c, min(PW, c1 - c)) for c in range(c0, c1, PW)]
        for (c, w) in pieces(0, 1024):
            wp, Bwp = load_piece(win_d, c, w)
            for o in range(0, w, 128):
                blk = (c + o) // 128
                proj_cm(wp, Bwp, o, 128, T, xr_ext[:, blk, 3:3 + T], [], [B_xr])
        if own:
            for (c, w) in pieces(1024, 2048):
                wp, Bwp = load_piece(win_d, c, w)
                for o in range(0, w, 128):
                    blk = (c + o - 1024) // 128
                    proj_cm(wp, Bwp, o, 128, T, yrT[:, blk, 0:T], [], [B_yr])
            for (c, w) in pieces(2048, 3072):
                wp, Bwp = load_piece(win_d, c, w)
                for o in range(0, w, 128):
                    blk = (c + o - 2048) // 128
                    proj_cm(wp, Bwp, o, 128, T, qT[:, blk, 0:T], [], [B_qT])
        SUB4 = cfg.get("SUB4", 99) if own else 99
        if SUB4 < 2:
            return
        for (c, w) in pieces(3072, 4096):
            wp, Bwp = load_piece(win_d, c, w)
            for o in range(0, w, 128):
                blk = (c + o - 3072) // 128
                proj_cm(wp, Bwp, o, 128, T, kTg[:, blk, 0:T], [], [B_kTg], eng="dve")
            if own:
                for ti, (tcol, P, row0) in enumerate(tiles):
                    pa, Bpa = next_ab()
                    for kc in range(KC):
                        MM(pa[:P, 0:w], hnT[:, kc, tcol:tcol + P], wp[:, kc, 0:w], kc == 0, kc == KC - 1, [Bwp, B_hnT], [Bpa])
                    o0 = ti * 1024 + (c - 3072)
                    CP("dve", tm32[:P, o0:o0 + w], pa[:P, 0:w], [Bpa], [B_tm32])
                    if c + w == 4096 and not os.environ.get("NOKDMA"):
                        DMA("pool", k_out[row0:row0 + P, :], tm32[:P, ti * 1024:(ti + 1) * 1024], [B_tm32], [outbuf()])
        regs = [seq.B_reg[(kpos0 + t) // 128] for t in range(0, T, 128)]
        for h in range(8):
            DMA("sp", seq.kT.ap()[h, :, kpos0:kpos0 + T], kTg[:, h, 0:T], [B_kTg], regs)
        if SUB4 < 3:
            return
        for (tcol, P, row0) in tiles:
            for (c, w) in pieces(4096, 5120):
                wp, Bwp = load_piece(win_d, c, w)
                pa, Bpa = next_ab()
                for kc in range(KC):
                    MM(pa[:P, 0:w], hnT[:, kc, tcol:tcol + P], wp[:, kc, 0:w], kc == 0, kc == KC - 1, [Bwp, B_hnT], [Bpa])
                if own:
                    CP("dve", tm32[:P, (c - 4096):(c - 4096) + w], pa[:P, 0:w], [Bpa], [B_tm32])
                CP("act", tm16[:P, (c - 4096):(c - 4096) + w], pa[:P, 0:w], [Bpa], [B_tm16])
            if own:
                DMA("pool", v_out[row0:row0 + P, :], tm32[:P, 0:1024], [B_tm32], [outbuf()])
            kb = (kpos0 + tcol) // 128
            po = (kpos0 + tcol) % 128
            for h in range(8):
                DMA("sp", seq.v.ap()[h, po:po + P, kb, :], tm16[:P, h * 128:(h + 1) * 128], [B_tm16], [seq.B_reg[kb]])
        if SUB4 < 4:
            return
        if own:
            for (c, w) in pieces(5120, 6144):
                wp, Bwp = load_piece(win_d, c, w)
                for o in range(0, w, 128):
                    blk = (c + o - 5120) // 128
                    proj_cm(wp, Bwp, o, 128, T, qiT[:, blk, 0:T], [], [B_qiT])
        if SUB4 < 5:
            return
        wp, Bwp = load_piece(win_d, 6144, 80)
        proj_cm(wp, Bwp, 0, 64, T, kiTg[0:64, 0:T], [], [B_kiTg], eng="dve")
        DMA("sp", seq.kiT.ap()[0:64, kpos0:kpos0 + T], kiTg[0:64, 0:T], [B_kiTg], regs)
        DMA("sp", seq.kiT.ap()[64:128, kpos0:kpos0 + T], kiTg[0:64, 0:T], [B_kiTg], regs)
        if own:
            for ti, (tcol, P, row0) in enumerate(tiles):
                pa, Bpa = next_ab()
                for kc in range(KC):
                    MM(pa[:P, 0:80], hnT[:, kc, tcol:tcol + P], wp[:, kc, 0:80], kc == 0, kc == KC - 1, [Bwp, B_hnT], [Bpa])
                CP("dve", small[:P, 0:64], pa[:P, 0:64], [Bpa], [B_small])
                DMA("pool", ki_out[row0:row0 + P, :], small[:P, 0:64], [B_small], [outbuf()])
                CP("dve", wi_t[:P, ti, :], pa[:P, 64:80], [Bpa], [B_wi])

    def lru_tile(tcol, P, own, first_flag, ti):
        xc = SC[:, 0:1024].rearrange("p (b t) -> p b t", b=8)[:, :, 0:P]
        rr = SC[:, 1024:2048].rearrange("p (b t) -> p b t", b=8)[:, :, 0:P]
        ii = SC[:, 2048:3072].rearrange("p (b t) -> p b t", b=8)[:, :, 0:P]
        aa = SC[:, 3072:4096].rearrange("p (b t) -> p b t", b=8)[:, :, 0:P]
        mm_ = SEL[:, 0:2048].bitcast(F32).rearrange("p (b t) -> p b t", b=8)[:, :, 0:P]
        hh = PB[:, 0:2048].bitcast(F32).rearrange("p (b t) -> p b t", b=8)[:, :, 0:P]
        xcb = PTB[:, 0:1024].rearrange("p (b t) -> p b t", b=8)[:, :, 0:P]
        gy = KTB[:, 0:2048].bitcast(F32).rearrange("p (b t) -> p b t", b=8)[:, :, 0:P]
        Bm, Bh, Bxcb, Bgy = B_SEL[0], B_PB[0], B_PTB[0], B_KTB

        def fork(parents, n):
            inh = [x for pb in parents for x in (([pb.w] if pb.w else []) + pb.r)]
            out = []
            for _ in range(n):
                b_ = Buf()
                b_.r = list(inh)
                out.append(b_)
            return out

        def join(parents, subs):
            for pb in parents:
                for b_ in subs:
                    pb.r += (([b_.w] if b_.w else []) + b_.r)

        Bx = fork([B_SC], 8); Br = fork([B_SC], 8); Bi = fork([B_SC], 8); Ba = fork([B_SC], 8)
        Bm8 = fork([Bm], 8); Bh8 = fork([Bh], 8)
        for j in range(4):
            for blk in range(8):
                if j == 0:
                    TS("dve", xc[:, blk, :], xr_ext[:, blk, tcol:tcol + P], cw[:, blk:blk + 1], cb[:, blk:blk + 1], ALU.mult, ALU.add,
                       [B_xr, B_cw, B_cb], [Bx[blk]])
                else:
                    STT(xc[:, blk, :], xr_ext[:, blk, tcol + j:tcol + j + P], cw[:, j * 8 + blk:j * 8 + blk + 1], xc[:, blk, :],
                        ALU.mult, ALU.add, [B_xr, B_cw, Bx[blk]], [Bx[blk]])
        CP("pool", xcb, xc, Bx, [Bxcb])
        for blk in range(8):
            pa, Bpa = next_ab()
            MM(pa[:, 0:P], wa_b[:, blk * 128:(blk + 1) * 128], xcb[:, blk, :], True, True, [B_wa, Bxcb], [Bpa])
            ACT(rr[:, blk, :], pa[:, 0:P], AF.Sigmoid, [Bpa, B_ba], [Br[blk]], bias=ba[:, blk:blk + 1])
            pa, Bpa = next_ab()
            MM(pa[:, 0:P], wx_b[:, blk * 128:(blk + 1) * 128], xcb[:, blk, :], True, True, [B_wx, Bxcb], [Bpa])
            ACT(ii[:, blk, :], pa[:, 0:P], AF.Sigmoid, [Bpa, B_bx], [Bi[blk]], bias=bx[:, blk:blk + 1])
        for blk in range(8):
            ACT(aa[:, blk, :], rr[:, blk, :], AF.Exp, [Br[blk], B_c12], [Ba[blk]], scale=c1[:, blk:blk + 1])
            ACT(mm_[:, blk, :], rr[:, blk, :], AF.Exp, [Br[blk], B_c12], [Bm8[blk]], scale=c2[:, blk:blk + 1])
        TS("dve", mm_, mm_, -1.0, 1.0, ALU.mult, ALU.add, Bm8, Bm8)
        ACT(mm_, mm_, AF.Sqrt, Bm8, Bm8)
        if first_flag == "always":
            MS("dve", mm_[:, :, 0:1], 1.0, Bm8)
        elif first_flag == "flag":
            TS("dve", mm_[:, :, 0:1], mm_[:, :, 0:1], flags[:, 0:1], flags[:, 1:2], ALU.mult, ALU.add, Bm8 + [B_flags], Bm8)
        TT("dve", ii, ii, xc, ALU.mult, Bi + Bx, Bi)
        TT("dve", ii, ii, mm_, ALU.mult, Bi + Bm8, Bi)
        for blk in range(8):
            S.op("dve", lambda e, blk=blk: e.tensor_tensor_scan(out=hh[:, blk, :], data0=aa[:, blk, :], data1=ii[:, blk, :],
                                                                initial=hprev[:, blk:blk + 1], op0=ALU.mult, op1=ALU.add),
                 [Ba[blk], Bi[blk], B_hprev], [Bh8[blk]])
        CP("dve", hprev[:, :].unsqueeze(2), hh[:, :, P - 1:P], Bh8, [B_hprev])
        if own:
            ACT(gy, yrT[:, :, tcol:tcol + P], AF.Gelu_apprx_tanh, [B_yr], [Bgy])
            TT("dve", catT[:, 0:8, tcol:tcol + P], hh, gy, ALU.mult, Bh8 + [Bgy], [B_cat[ti]])
        join([B_SC], Bx + Br + Bi + Ba)
        join([Bm], Bm8)
        join([Bh], Bh8)

    def attention_tile(seq, tcol, P, kpos, S_ctxpen, ti):
        S_t = kpos + P
        nb128 = (S_t + 127) // 128
        regs = seq.B_reg[0:nb128]
        kiTs = KTB
        DMA("sp", kiTs[:, 0:S_t], seq.kiT.ap()[:, 0:S_t], regs, [B_KTB])
        awi = small[:, 16:32]
        sgn = small[:, 32:48]
        wi = wi_t[:, ti, :]
        ACT(awi[:P], wi[:P], AF.Abs, [B_wi], [B_small], scale=0.03125)
        TS("dve", sgn[:P], wi[:P], 0.0, 2.0, ALU.is_gt, ALU.mult, [B_wi], [B_small])
        TS("dve", sgn[:P], sgn[:P], -1.0, None, ALU.add, None, [B_small], [B_small])
        dsg = PTB[:, 0:16 * 128].rearrange("p (h q) -> p h q", h=16)
        TT("dve", dsg[:P, :, :P], ident[:P, :P].unsqueeze(1).to_broadcast([P, 16, P]), sgn[:P, :].unsqueeze(2).to_broadcast([P, 16, P]),
           ALU.mult, [B_ident, B_small], [B_PTB[0]])
        score = SC
        idx_banks = [(psA[:, :], B_psA), (psB[:, :], B_psB), (psT[:, :].bitcast(F32), B_psT), (psT2[:, :].bitcast(F32), B_psT2),
                     (psACC[:, 1024:1536], B_acc[2]), (psACC[:, 1536:2048], B_acc[3])]
        B_tmp = [Buf() for _ in range(8)]
        for b_ in B_tmp:
            b_.r = [x for pb in B_PB for x in (([pb.w] if pb.w else []) + pb.r)]
        kk = 0
        IW = 512
        SKW = 3
        items = []
        for s0 in range(0, S_t, IW):
            for h in range(16):
                items.append((s0, min(IW, S_t - s0), h, (s0 // IW) % 2))

        def idx_tail(k):
            s0, w, h, accb = items[k]
            acc = psACC[:, accb * 512:(accb + 1) * 512]
            tv = PB[:, (k % 8) * 512:(k % 8) * 512 + w]
            MM(acc[:P, 0:w], dsg[:P, h, :P], tv[:P], h == 0, h == 15, [B_PTB[0], B_tmp[k % 8]], [B_acc[accb]])
            if h == 15:
                CP("dve" if accb == 0 else "act", score[:P, s0:s0 + w], acc[:P, 0:w], [B_acc[accb]], [B_SC])

        for k, (s0, w, h, accb) in enumerate(items):
            c, base = h // 2, (h % 2) * 64
            pa, Bpa = idx_banks[k % 6]
            tv = PB[:, (k % 8) * 512:(k % 8) * 512 + w]
            tb = B_tmp[k % 8]
            MM(pa[:P, 0:w], qiT[base:base + 64, c, tcol:tcol + P], kiTs[base:base + 64, s0:s0 + w], True, True, [B_qiT, B_KTB], [Bpa])
            if h % 2 == 0:
                ACT(tv[:P], pa[:P, 0:w], AF.Relu, [Bpa, B_small], [tb], scale=awi[:P, h:h + 1])
            else:
                TS("dve", tv[:P], pa[:P, 0:w], 0.0, awi[:P, h:h + 1], ALU.max, ALU.mult, [Bpa, B_small], [tb])
            if k >= SKW:
                idx_tail(k - SKW)
        for k in range(max(0, len(items) - SKW), len(items)):
            idx_tail(k)
        for pb in B_PB:
            for b_ in B_tmp:
                pb.r += (([b_.w] if b_.w else []) + b_.r)
        lo = small[:, 2:3]; wdt = small[:, 3:4]; mid = small[:, 4:5]; cnt = small[:, 5:6]; stp = small[:, 6:7]
        S.op("dve", lambda e: e.tensor_reduce(out=lo[:P], in_=score[:P, 0:S_t], axis=AX.X, op=ALU.min), [B_SC], [B_small])
        S.op("dve", lambda e: e.tensor_reduce(out=wdt[:P], in_=score[:P, 0:S_t], axis=AX.X, op=ALU.max), [B_SC], [B_small])
        TT("dve", wdt[:P], wdt[:P], lo[:P], ALU.subtract, [B_small], [B_small])
        TS("dve", wdt[:P], wdt[:P], 1.0e-6, None, ALU.add, None, [B_small], [B_small])
        wk = small[:, 40:40 + NIT + 1]
        TS("dve", wk[:P], pow2[:P, :], wdt[:P], None, ALU.mult, None, [B_pow2, B_small], [B_small])
        if P == 128:
            MS("dve", score[0:64, kpos + 64:kpos + 128], NEG, [B_SC])
        if S_ctxpen > 0:
            TS("dve", score[:P, 0:S_ctxpen], score[:P, 0:S_ctxpen], flags[:P, 2:3], None, ALU.add, None, [B_SC, B_flags], [B_SC])
        junk = PB
        for k in range(1, NIT + 1):
            TT("dve", mid[:P], lo[:P], wk[:P, k:k + 1], ALU.add, [B_small], [B_small])
            TS("dve", junk[:P, 0:S_t], score[:P, 0:S_t], mid[:P], None, ALU.is_ge, ALU.add, [B_SC, B_small], [B_PB[0], B_PB[1], B_small],
               accum_out=cnt[:P])
            STT(stp[:P], cnt[:P], float(seq.topk) - 0.5, wk[:P, k:k + 1], ALU.is_ge, ALU.mult, [B_small], [B_small])
            TT("dve", lo[:P], lo[:P], stp[:P], ALU.add, [B_small], [B_small])
        sel = SEL
        TS("dve", sel[:P, 0:S_t], score[:P, 0:S_t], lo[:P], None, ALU.is_ge, None, [B_SC, B_small], [B_SEL[0], B_SEL[1]])
        L = SC
        mx8 = small[:, 8:16]
        rs = small[:, 48:56]
        po = psACC[:, 0:1024]
        nblk = (S_t + 511) // 512
        assert nblk <= 9
        band0 = kpos - 128
        for h in range(8):
            kTh = KTB
            DMA("sp", kTh[:, 0:S_t], seq.kT.ap()[h, :, 0:S_t], regs, [B_KTB])
            Vh = VB[:, 0:nb128 * 128].rearrange("p (n d) -> p n d", d=128)
            DMA("sp", Vh, seq.v.ap()[h, :, 0:nb128, :], regs, [B_VB])
            for bi, s0 in enumerate(range(0, S_t, 512)):
                w = min(512, S_t - s0)
                pa, Bpa = next_ab()
                MM(pa[:P, 0:w], qT[:, h, tcol:tcol + P], kTh[:, s0:s0 + w], True, True, [B_qT, B_KTB], [Bpa])
                S.op("dve", lambda e, pa=pa, s0=s0, w=w, bi=bi: e.tensor_scalar(
                    out=L[:P, s0:s0 + w], in0=pa[:P, 0:w], scalar1=128.0 ** -0.5, scalar2=None, op0=ALU.mult, op1=ALU.max,
                    accum_out=small[:P, 24 + bi:25 + bi]), [Bpa], [B_SC, B_small])
            b_lo = max(band0, 0)
            b_hi = min(kpos + 128, S_t)
            TT("dve", L[:P, b_lo:b_hi], L[:P, b_lo:b_hi], bband[:P, h, b_lo - band0:b_hi - band0], ALU.add, [B_SC, B_bband], [B_SC])
            S.op("dve", lambda e: e.tensor_reduce(out=mx8[:P, h:h + 1], in_=small[:P, 24:24 + nblk], axis=AX.X, op=ALU.max),
                 [B_small], [B_small])
            TS("dve", mx8[:P, h:h + 1], mx8[:P, h:h + 1], mxdb[:P, h:h + 1], -1.0, ALU.add, ALU.mult, [B_small, B_mxdb], [B_small])
            Pm = PB
            ACT(Pm[:P, 0:S_t], L[:P, 0:S_t], AF.Exp, [B_SC, B_small], [B_PB[0], B_PB[1]], bias=mx8[:P, h:h + 1])
            STT(Pm[:P, 0:S_t], Pm[:P, 0:S_t], 1.0, sel[:P, 0:S_t], ALU.mult, ALU.mult, [B_PB[0], B_PB[1], B_SEL[0], B_SEL[1]],
                [B_PB[0], B_PB[1], B_small], accum_out=rs[:P, h:h + 1])
            PT = PTB
            for j0 in range(0, nb128, 8):
                pt, Bpt = next_t()
                nj = min(8, nb128 - j0)
                for jj in range(nj):
                    j = j0 + jj
                    ws = min(128, S_t - j * 128)
                    TR(pt[0:ws, jj * 128:jj * 128 + P], Pm[:P, j * 128:j * 128 + ws], ident[:P, :P], [B_PB[0], B_PB[1], B_ident], [Bpt])
                ws_last = min(128, S_t - (j0 + nj - 1) * 128)
                if ws_last == 128:
                    CP("act" if (j0 // 8) % 2 == 0 else "dve", PT[:, j0 * 128:(j0 + nj) * 128], pt[:, 0:nj * 128], [Bpt], [B_PTB[0], B_PTB[1]])
                else:
                    if nj > 1:
                        CP("act", PT[:, j0 * 128:(j0 + nj - 1) * 128], pt[:, 0:(nj - 1) * 128], [Bpt], [B_PTB[0], B_PTB[1]])
                    CP("dve", PT[0:ws_last, (j0 + nj - 1) * 128:(j0 + nj) * 128], pt[0:ws_last, (nj - 1) * 128:nj * 128], [Bpt],
                       [B_PTB[0], B_PTB[1]])
            for j in range(nb128):
                ws = min(128, S_t - j * 128)
                MM(po[:P, h * 128:(h + 1) * 128], PT[0:ws, j * 128:j * 128 + P], Vh[0:ws, j, :], j == 0, j == nb128 - 1,
                   [B_PTB[0], B_PTB[1], B_VB], [B_acc[0], B_acc[1]])
        rinv = small[:, 56:64]
        S.op("dve", lambda e: e.reciprocal(out=rinv[:P], in_=rs[:P]), [B_small], [B_small])
        attn = tm16
        for h in range(8):
            ACT(attn[:P, h * 128:(h + 1) * 128], po[:P, h * 128:(h + 1) * 128], AF.Identity, [B_acc[0], B_acc[1], B_small], [B_tm16],
                scale=rinv[:P, h:h + 1])
        pt, Bpt = next_t()
        for h in range(8):
            TR(pt[:, h * 128:h * 128 + P], attn[:P, h * 128:(h + 1) * 128], ident[:P, :P], [B_tm16, B_ident], [Bpt])
        for h in range(8):
            CP("dve", catT[:, 8 + h, tcol:tcol + P], pt[:, h * 128:h * 128 + P], [Bpt], [B_cat[ti]])

    def mix_residual(x_rows, P, tcol, ti):
        for db in range(D // PW):
            wp, Bwp = load_piece(wout_d, db * PW, PW)
            bk = B_acc[(db * PW) // 512]
            for kc in range(KC):
                MM(psACC[:P, db * PW:(db + 1) * PW], catT[:, kc, tcol:tcol + P], wp[:, kc, :], kc == 0, kc == KC - 1, [B_cat[ti], Bwp], [bk])
        x1 = SC[:, 0:D]
        TT("dve", x1[:P], psACC[:P, :], gt1b[:P, :], ALU.mult, B_acc + [B_gtb], [B_SC])
        DMA("sp", xt[:P, :], x_rows, (), [B_xt])
        TT("dve", xt[:P, :], xt[:P, :], x1[:P], ALU.add, [B_xt, B_SC], [B_xt])

    def peer_tile(P, y_rows):
        rms_hnT(None, P, cur["b"], 0, 1)
        hn2 = sb2_hn2
        for half in range(2):
            pt, Bpt = next_t()
            for j in range(8):
                kc = half * 8 + j
                TR(pt[:P, j * 128:(j + 1) * 128], hnT[:, kc, 0:P], ident[:, :], [B_hnT, B_ident], [Bpt])
            CP("act" if half == 0 else "dve", hn2[:P, half * 1024:(half + 1) * 1024], pt[:P, :], [Bpt], [B_hn2])
        pq = VB[:, 0:2048].bitcast(F32).rearrange("p (c t) -> p c t", c=8)
        for pc in range(1024 // PW):
            wp, Bwp = load_piece(wq_d, pc * PW, PW)
            for o in range(0, PW, 128):
                c = (pc * PW + o) // 128
                pa, Bpa = next_ab()
                for kc in range(KC):
                    MM(pa[:, 0:P], wp[:, kc, o:o + 128], hnT[:, kc, 0:P], kc == 0, kc == KC - 1, [Bwp, B_hnT], [Bpa])
                CP("act", pq[:, c, 0:P], pa[:, 0:P], [Bpa], [B_VB])
        W0 = D
        s12 = SC[:, W0:W0 + 256].rearrange("p (a n) -> p a n", a=2)
        s12b = SC[:, W0 + 256:W0 + 512].rearrange("p (a n) -> p a n", a=2)
        cand = SC[:, W0 + 512:W0 + 768]
        candb = SC[:, W0 + 768:W0 + 1024]
        cidx = SC[:, W0 + 1024:W0 + 1280]
        jk = SC[:, W0 + 1280:W0 + 1536]
        v12 = pst_small[:, 0:32].rearrange("p (a k) -> p a k", a=2)
        i12 = pst_small_u[:, 32:64].rearrange("p (a k) -> p a k", a=2)
        i12f = pst_small[:, 64:96].rearrange("p (a k) -> p a k", a=2)
        ts_ = pst_small[:, 96:112]
        tj = pst_small_u[:, 112:128]
        tjf = pst_small[:, 128:144]
        ssum = pst_small[:, 144:145]
        negm = pst_small[:, 145:146]
        Bq = B_pst
        for h in range(8):
            for a in range(2):
                pa, Bpa = next_ab()
                MM(pa[:P, 0:128], pq[a * 64:(a + 1) * 64, h, 0:P], keysT[a * 64:(a + 1) * 64, h * 128:(h + 1) * 128], True, True,
                   [B_VB, B_keysT], [Bpa])
                CP("act", s12[:P, a, :], pa[:P, 0:128], [Bpa], [B_SC])
                S.op("dve", lambda e, a=a: e.max(out=v12[:P, a, 0:8], in_=s12[:P, a, :]), [B_SC], [Bq])
                S.op("dve", lambda e, a=a: e.match_replace(out=s12b[:P, a, :], in_to_replace=v12[:P, a, 0:8], in_values=s12[:P, a, :],
                                                          imm_value=NEG), [B_SC, Bq], [B_SC])
                S.op("dve", lambda e, a=a: e.max(out=v12[:P, a, 8:16], in_=s12b[:P, a, :]), [B_SC, Bq], [Bq])
                S.op("dve", lambda e, a=a: e.max_index(out=i12[:P, a, 0:8], in_max=v12[:P, a, 0:8], in_values=s12[:P, a, :]), [B_SC, Bq], [Bq])
                S.op("dve", lambda e, a=a: e.max_index(out=i12[:P, a, 8:16], in_max=v12[:P, a, 8:16], in_values=s12[:P, a, :]), [B_SC, Bq], [Bq])
            CP("dve", i12f[:P], i12[:P], [Bq], [Bq])
            c3 = cand.rearrange("p (a b) -> p a b", a=16)
            TT("dve", c3[:P], v12[:P, 0, :].unsqueeze(2).to_broadcast([P, 16, 16]), v12[:P, 1, :].unsqueeze(1).to_broadcast([P, 16, 16]),
               ALU.add, [Bq], [B_SC])
            ci3 = cidx.rearrange("p (a b) -> p a b", a=16)
            STT(ci3[:P], i12f[:P, 0, :].unsqueeze(2).to_broadcast([P, 16, 16]), 128.0, i12f[:P, 1, :].unsqueeze(1).to_broadcast([P, 16, 16]),
                ALU.mult, ALU.add, [Bq], [B_SC])
            S.op("dve", lambda e: e.max(out=ts_[:P, 0:8], in_=cand[:P, :]), [B_SC], [Bq])
            S.op("dve", lambda e: e.match_replace(out=candb[:P, :], in_to_replace=ts_[:P, 0:8], in_values=cand[:P, :], imm_value=NEG),
                 [B_SC, Bq], [B_SC])
            S.op("dve", lambda e: e.max(out=ts_[:P, 8:16], in_=candb[:P, :]), [B_SC, Bq], [Bq])
            S.op("dve", lambda e: e.max_index(out=tj[:P, 0:8], in_max=ts_[:P, 0:8], in_values=cand[:P, :]), [B_SC, Bq], [Bq])
            S.op("dve", lambda e: e.max_index(out=tj[:P, 8:16], in_max=ts_[:P, 8:16], in_values=cand[:P, :]), [B_SC, Bq], [Bq])
            CP("dve", tjf[:P], tj[:P], [Bq], [Bq])
            for k in range(16):
                STT(jk[:P, :], iota256[:P, :], tjf[:P, k:k + 1], cidx[:P, :], ALU.is_equal, ALU.mult, [B_iota, Bq, B_SC], [B_SC, B_eidx],
                    accum_out=eidxf[:P, h * 16 + k:h * 16 + k + 1])
            TS("dve", negm[:P], ts_[:P, 0:1], -1.0, None, ALU.mult, None, [Bq], [Bq])
            ACT(gw[:P, h * 16:(h + 1) * 16], ts_[:P, :], AF.Exp, [Bq], [B_gw, Bq], bias=negm[:P], accum_out=ssum[:P])
            S.op("dve", lambda e: e.reciprocal(out=ssum[:P], in_=ssum[:P]), [Bq], [Bq])
            TS("dve", gw[:P, h * 16:(h + 1) * 16], gw[:P, h * 16:(h + 1) * 16], ssum[:P], None, ALU.mult, None, [B_gw, Bq], [B_gw])
        CP("dve", eidx[:P, :], eidxf[:P, :], [B_eidx], [B_eidxi])
        NG = 5
        for sl in range(128):
            g, Bg = uvg[sl % NG], B_uvg[sl % NG]
            S.dma("pool", lambda e, g=g, sl=sl: e.indirect_dma_start(out=g[:P, :], out_offset=None, in_=uv_d.ap(),
                                                                     in_offset=bass.IndirectOffsetOnAxis(ap=eidx[:P, sl:sl + 1], axis=0)),
                  [B_eidxi, B_uv], Bg)
            STT(g[:P, 0:D], g[:P, 0:D], 1.0, hn2[:P, :], ALU.mult, ALU.mult, Bg + [B_hn2], [Bg[0], B_actv[sl % 8]],
                accum_out=actv[:P, sl:sl + 1])
            ACT(gel[:P, sl:sl + 1], actv[:P, sl:sl + 1], AF.Gelu_apprx_tanh, [B_actv[sl % 8]], [B_gel[sl % 8]])
            ACT(gel[:P, sl:sl + 1], gel[:P, sl:sl + 1], AF.Identity, [B_gel[sl % 8], B_gw], [B_gel[sl % 8]], scale=gw[:P, sl:sl + 1])
            dg, Bdg = dgs[sl % 2], B_dgs[sl % 2]
            ACT(dg[:P, :P], ident[:P, :P], AF.Identity, [B_ident, B_gel[sl % 8]], [Bdg], scale=gel[:P, sl:sl + 1])
            for nb in range(4):
                MM(psACC[:P, nb * 512:(nb + 1) * 512], dg[:P, :P], g[:P, D + nb * 512:D + (nb + 1) * 512], sl == 0, sl == 127,
                   [Bdg] + Bg, [B_acc[nb]])
        x2 = SC[:, 0:D]
        TT("dve", x2[:P], psACC[:P, :], gt2b[:P, :], ALU.mult, B_acc + [B_gtb], [B_SC])
        TT("dve", x2[:P], x2[:P], xt[:P, :], ALU.add, [B_SC, B_xt], [B_SC])
        ms = small[:, 0:1]
        ACT(xsb[:P, :], x2[:P], AF.Square, [B_SC], [B_xsb, B_small], accum_out=ms[:P])
        TS("dve", ms[:P], ms[:P], 1.0 / D, EPS, ALU.mult, ALU.add, [B_small], [B_small])
        ACT(ms[:P], ms[:P], AF.Sqrt, [B_small], [B_small])
        S.op("dve", lambda e: e.reciprocal(out=small[:P, 1:2], in_=ms[:P]), [B_small], [B_small])
        STT(xt[:P, :], x2[:P], small[:P, 1:2], gfin[:P, :], ALU.mult, ALU.mult, [B_SC, B_small, B_gfin], [B_xt])
        DMA("pool", y_rows, xt[:P, :], [B_xt], [outbuf()])

    sb2_hn2 = sb([128, D], BF16, "hn2"); B_hn2 = Buf()
    pst_small = sb([128, 160], F32, "pst"); B_pst = Buf()
    pst_small_u = pst_small[:, :].bitcast(U32)
    eidxf = sb([128, 128], F32, "eidxf"); B_eidx = Buf()
    eidx = sb([128, 128], I32, "eidx"); B_eidxi = Buf()
    gw = sb([128, 128], F32, "gw"); B_gw = Buf()
    actv = sb([128, 128], F32, "actv"); B_actv = [Buf() for _ in range(8)]
    gel = sb([128, 128], F32, "gel"); B_gel = [Buf() for _ in range(8)]
    uvg = [SEL[:, 0:2 * D], PB[:, 0:2 * D], PTB[:, 0:2 * D], KTB[:, 0:2 * D], VB[:, 0:2 * D]]
    B_uvg = [B_SEL, B_PB, B_PTB, [B_KTB], [B_VB]]
    dgs = [sb([128, 128], BF16, "dg%d" % i) for i in range(2)]; B_dgs = [Buf(), Buf()]
    cur = {"b": 0}
    halo_st = sb([128, 8, 3], F32, "halo_st"); B_halo = Buf()

    def load_seq_mod(b):
        cur["b"] = b
        DMA("sp", gt1b[:], modrow_d.ap()[b:b + 1, 2 * D:3 * D].partition_broadcast(128), [B_modrow], [B_gtb])
        DMA("sp", gt2b[:], modrow_d.ap()[b:b + 1, 5 * D:6 * D].partition_broadcast(128), [B_modrow], [B_gtb])

    STAGE = cfg.get("STAGE", 99)
    pseq = make_seq("p", 0, SP_MAX, cfg["TOPK_P"], SP_MAX // 128)
    load_seq_mod(0)
    MS("dve", hprev[:], 0.0, [B_hprev])
    MS("dve", xr_ext[:, :, 0:3], 0.0, [B_xr])
    S_CTX = NCT * 128
    for g in range(NCT // GT if STAGE >= 2 else 0):
        T = GT * 128
        tiles = []
        for t in range(GT):
            row0 = (g * GT + t) * 128
            rms_hnT(x_ctx.ap()[row0:row0 + 128, :], 128, 0, t * 128, 0)
            tiles.append((t * 128, 128, row0))
        SUB = cfg.get("SUB", 99)
        if SUB >= 2:
            projections(pseq, T, tiles, False, g * T, None, None, None)
        for t in range(GT if SUB >= 3 else 0):
            lru_tile(t * 128, 128, False, "always" if (g == 0 and t == 0) else None, t)
        CP("dve", xr_ext[:, :, 0:3], xr_ext[:, :, T:T + 3], [B_xr], [B_xr])
    conv_join()
    TS("dve", hprev[:], hprev[:], flags[:, 0:1], None, ALU.mult, None, [B_hprev, B_flags], [B_hprev])
    TS("dve", xr_ext[:, :, 0:3], xr_ext[:, :, 0:3], flags[:, 0:1], None, ALU.mult, None, [B_xr, B_flags], [B_xr])
    for g in range(NOT // GT if STAGE >= 3 else 0):
        T = GT * 128
        tiles = []
        for t in range(GT):
            row0 = (g * GT + t) * 128
            rms_hnT(x_own.ap()[row0:row0 + 128, :], 128, 0, t * 128, 0)
            tiles.append((t * 128, 128, row0))
        projections(pseq, T, tiles, True, S_CTX + g * T, k_own_d.ap(), v_own_d.ap(), ki_own_d.ap())
        for t in range(GT if cfg.get("SUB3", 99) >= 2 else 0):
            lru_tile(t * 128, 128, True, "flag" if (g == 0 and t == 0) else None, t)
        if g == NOT // GT - 1 and cfg.get("SUB3", 99) >= 3:
            CP("dve", halo_st[:], xr_ext[:, :, T:T + 3], [B_xr], [B_halo])
            DMA("pool", conv_p_d.ap(), halo_st[:], [B_halo], [outbuf()])
            DMA("pool", lru_p_d.ap(), hprev[:], [B_hprev], [outbuf()])
        CP("dve", xr_ext[:, :, 0:3], xr_ext[:, :, T:T + 3], [B_xr], [B_xr])
        for t in range(GT):
            row0 = (g * GT + t) * 128
            if STAGE >= 4:
                attention_tile(pseq, t * 128, 128, S_CTX + g * T + t * 128, S_CTX, t)
            if STAGE >= 5:
                mix_residual(x_own.ap()[row0:row0 + 128, :], 128, t * 128, t)
            if STAGE >= 6:
                peer_tile(128, y_own_d.ap()[row0:row0 + 128, :])

    for sbi in range(2 if STAGE >= 7 else 0):
        sseq = make_seq("s%d" % sbi, 1 + sbi, SS_MAX, cfg["TOPK_S"], SS_MAX // 128)
        load_seq_mod(1 + sbi)
        kst = [PB[:, 0:4096].rearrange("p (n c) -> p n c", n=4), VB[:, 0:4096].rearrange("p (n c) -> p n c", n=4)]
        B_kst = [B_PB, [B_VB]]
        kts = [PTB[:, 0:4096].rearrange("p (h s) -> p h s", h=8), KTB[:, 0:4096].rearrange("p (h s) -> p h s", h=8)]
        B_kts = [B_PTB, [B_KTB]]
        vst = [SEL[:, 0:4096].rearrange("p (n c) -> p n c", n=4), SC[:, 0:2048].bitcast(BF16).rearrange("p (n c) -> p n c", n=4)]
        B_vst = [B_SEL, [B_SC]]
        for cb4 in range(PAST // 512):
            r0 = cb4 * 512
            i = cb4 % 2
            regs4 = sseq.B_reg[cb4 * 4:cb4 * 4 + 4]
            DMA("pool", kst[i], ck_d.ap()[sbi, r0:r0 + 512, :].rearrange("(n p) c -> p n c", p=128), (), B_kst[i])
            for n in range(4):
                pt, Bpt = next_t()
                for h in range(8):
                    TR(pt[:, h * 128:(h + 1) * 128], kst[i][:, n, h * 128:(h + 1) * 128], ident[:, :], B_kst[i] + [B_ident], [Bpt])
                CP("act" if n % 2 == 0 else "dve", kts[i][:, :, n * 128:(n + 1) * 128], pt[:, :].rearrange("p (h s) -> p h s", h=8),
                   [Bpt], B_kts[i])
            for h in range(8):
                DMA("sp", sseq.kT.ap()[h, :, r0:r0 + 512], kts[i][:, h, :], B_kts[i], regs4)
            DMA("pool", vst[i], cv_d.ap()[sbi, r0:r0 + 512, :].rearrange("(n p) c -> p n c", p=128), (), B_vst[i])
            for h in range(8):
                DMA("sp", sseq.v.ap()[h, :, cb4 * 4:cb4 * 4 + 4, :], vst[i][:, :, h * 128:(h + 1) * 128], B_vst[i], regs4)
            kis = xsb[:, 0:1024].rearrange("p (n c) -> p n c", n=4)
            DMA("pool", kis[:, :, 0:64], cki_d.ap()[sbi, r0:r0 + 512, :].rearrange("(n p) c -> p n c", p=128), (), [B_xsb])
            CP("dve", kis[:, :, 64:128], kis[:, :, 0:64], [B_xsb], [B_xsb])
            pt, Bpt = next_t()
            for n in range(4):
                TR(pt[:, n * 128:(n + 1) * 128], kis[:, n, 0:128], ident[:, :], [B_xsb, B_ident], [Bpt])
            CP("dve", sb2_hn2[:, 0:512], pt[:, 0:512], [Bpt], [B_hn2])
            DMA("sp", sseq.kiT.ap()[:, r0:r0 + 512], sb2_hn2[:, 0:512], [B_hn2], regs4)
        DMA("sp", hprev[:], stl_d.ap()[sbi], (), [B_hprev])
        DMA("sp", halo_st[:], stc_d.ap()[sbi], (), [B_halo])
        CP("dve", xr_ext[:, :, 0:3], halo_st[:], [B_halo], [B_xr])
        T = 64
        rms_hnT(xs_d.ap()[sbi], 64, 1 + sbi, 0, 0)
        tiles = [(0, 64, 0)]
        projections(sseq, T, tiles, True, PAST, ks_d.ap()[sbi], vs_d.ap()[sbi], kis_d.ap()[sbi])
        lru_tile(0, 64, True, None, 0)
        CP("dve", halo_st[:], xr_ext[:, :, T:T + 3], [B_xr], [B_halo])
        DMA("pool", conv_s_d.ap()[sbi], halo_st[:], [B_halo], [outbuf()])
        DMA("pool", lru_s_d.ap()[sbi], hprev[:], [B_hprev], [outbuf()])
        attention_tile(sseq, 0, 64, PAST, 0, 0)
        mix_residual(xs_d.ap()[sbi], 64, 0, 0)
        peer_tile(64, ys_d.ap()[sbi])

    S.final_waits("sp", OUTB)
    keys = S.semkey_list()
    semmap = {}
    for i, k in enumerate(keys):
        semmap[k] = es.enter_context(nc.semaphore("sem%d" % i))
    with nc.Block() as block:
        @block.tensor
        def _(e):
            S.emit("pe", e, semmap)

        @block.scalar
        def _(e):
            S.emit("act", e, semmap)

        @block.vector
        def _(e):
            S.emit("dve", e, semmap)

        @block.gpsimd
        def _(e):
            S.emit("pool", e, semmap)

        @block.sync
        def _(e):
            S.emit("sp", e, semmap)
    es.close()
    return nc, S


def _t5_onehot():
    import jax
    import jax.numpy as jnp
    with jax.default_device(jax.devices("cpu")[0]):
        rel = jnp.arange(-255, 128, dtype=jnp.int32)
        half = 16
        max_exact = 8
        ret = jnp.where(rel > 0, half, 0)
        n = jnp.abs(rel)
        nf = jnp.maximum(n, 1).astype(jnp.float32)
        large = max_exact + (jnp.log(nf / max_exact) / math.log(128 / max_exact) * (half - max_exact)).astype(jnp.int32)
        large = jnp.minimum(large, half - 1)
        bucket = np.asarray(ret + jnp.where(n < max_exact, n, large))
    oh = np.zeros((32, 383), np.float32)
    oh[bucket, np.arange(383)] = 1.0
    return oh


def chan_major(v):
    return np.ascontiguousarray(np.swapaxes(v.reshape(v.shape[:-1] + (8, 128)), -1, -2))


def prepare_inputs(cfg, x_prompt, x_sample, cache_k, cache_v, cache_kidx, state_lru, state_conv,
                   c_prompt, c_sample, w_ada, b_ada, g_norm1, g_norm2, w_in, conv_w, conv_b,
                   lru_wa, lru_ba, lru_wx, lru_bx, lru_lam, w_out, peer_wq, peer_keys,
                   peer_u, peer_v, rel_bias, g_final):
    NCT, NOT, PAST = cfg["NCT"], cfg["NOT"], cfg["PAST"]
    f = lambda a: np.ascontiguousarray(np.asarray(a, dtype=np.float32))
    shared = {
        "w_ada": f(w_ada[0]), "b_ada": f(b_ada[0]).reshape(1, -1),
        "g1T": f(g_norm1[0].reshape(16, 128).T), "g2T": f(g_norm2[0].reshape(16, 128).T),
        "w_in": f(w_in[0]),
        "conv_wT": f(np.transpose(conv_w[0].reshape(4, 8, 128), (2, 0, 1)).reshape(128, 32)),
        "conv_bT": f(conv_b[0].reshape(8, 128).T),
        "lru_waT": f(np.transpose(lru_wa[0], (1, 0, 2)).reshape(128, 1024)),
        "lru_wxT": f(np.transpose(lru_wx[0], (1, 0, 2)).reshape(128, 1024)),
        "lru_baT": f(lru_ba[0].reshape(8, 128).T), "lru_bxT": f(lru_bx[0].reshape(8, 128).T),
        "lru_lamT": f(lru_lam[0].reshape(8, 128).T),
        "w_out": f(w_out[0]), "peer_wq": f(peer_wq[0]),
        "peer_keysT": f(np.transpose(peer_keys[0], (0, 3, 1, 2)).reshape(128, 1024)),
        "peer_u": f(peer_u[0]), "peer_v": f(peer_v[0]),
        "rel_bias": f(rel_bias), "g_final": f(g_final).reshape(1, -1), "oh": _t5_onehot(),
    }
    maps = []
    HALF = NOT * 128
    for c in range(8):
        b, half = c // 2, c % 2
        m = dict(shared)
        m["x_ctx"] = f(x_prompt[b, 0:NCT * 128])
        m["x_own"] = f(x_prompt[b, half * HALF:(half + 1) * HALF])
        sl = slice(2 * c, 2 * c + 2)
        m["xs"] = f(x_sample[sl])
        m["ck"] = f(cache_k[0, sl].reshape(2, PAST, 1024))
        m["cv"] = f(cache_v[0, sl].reshape(2, PAST, 1024))
        m["cki"] = f(cache_kidx[0, sl])
        m["st_lru"] = f(chan_major(state_lru[0, sl]))
        m["st_conv"] = f(np.transpose(chan_major(state_conv[0, sl]), (0, 2, 3, 1)))
        c3 = np.stack([c_prompt[b], c_sample[2 * c], c_sample[2 * c + 1]])
        m["c3T"] = f(np.transpose(c3.reshape(3, 16, 128), (2, 1, 0)).reshape(128, 48))
        fl = np.zeros((128, 4), np.float32)
        fl[:, 0] = float(half)
        fl[:, 1] = 1.0 - float(half)
        fl[:, 2] = 0.0 if half else NEG
        m["flags"] = fl
        maps.append(m)
    return maps


def assemble(cfg, res):
    NOT, PAST = cfg["NOT"], cfg["PAST"]
    HALF = NOT * 128
    SEQ = 2 * HALF
    y_p = np.zeros((4, SEQ, D), np.float32)
    y_s = np.zeros((16, 64, D), np.float32)
    k_p = np.zeros((1, 4, SEQ, 8, 128), np.float32)
    v_p = np.zeros((1, 4, SEQ, 8, 128), np.float32)
    ki_p = np.zeros((1, 4, SEQ, 64), np.float32)
    lru_p = np.zeros((1, 4, 1024), np.float32)
    conv_p = np.zeros((1, 4, 3, 1024), np.float32)
    k_s = np.zeros((1, 16, 64, 8, 128), np.float32)
    v_s = np.zeros((1, 16, 64, 8, 128), np.float32)
    ki_s = np.zeros((1, 16, 64, 64), np.float32)
    lru_s = np.zeros((1, 16, 1024), np.float32)
    conv_s = np.zeros((1, 16, 3, 1024), np.float32)
    for c in range(8):
        r = res[c]
        b, half = c // 2, c % 2
        sl = slice(half * HALF, (half + 1) * HALF)
        y_p[b, sl] = r["y_own"]
        k_p[0, b, sl] = r["k_own"].reshape(HALF, 8, 128)
        v_p[0, b, sl] = r["v_own"].reshape(HALF, 8, 128)
        ki_p[0, b, sl] = r["ki_own"]
        if half == 1:
            lru_p[0, b] = r["lru_p"].T.reshape(1024)
            conv_p[0, b] = np.transpose(r["conv_p"], (2, 1, 0)).reshape(3, 1024)
        for i in range(2):
            sbi = 2 * c + i
            y_s[sbi] = r["ys"][i]
            k_s[0, sbi] = r["ks"][i].reshape(64, 8, 128)
            v_s[0, sbi] = r["vs"][i].reshape(64, 8, 128)
            ki_s[0, sbi] = r["kis"][i]
            lru_s[0, sbi] = r["lru_s"][i].T.reshape(1024)
            conv_s[0, sbi] = np.transpose(r["conv_s"][i], (2, 1, 0)).reshape(3, 1024)
    return (y_p, y_s, k_p, v_p, ki_p, lru_p, conv_p, k_s, v_s, ki_s, lru_s, conv_s)


_CACHE = {}


def kernel(**inputs):
    cfg = make_cfg(True)
    if "nc" not in _CACHE:
        _CACHE["nc"] = build(cfg)[0]
    nc = _CACHE["nc"]
    maps = prepare_inputs(cfg, **{k: np.asarray(v) for k, v in inputs.items()})
    res = run_bass_kernel_spmd(nc, maps, core_ids=list(range(8)))
    return assemble(cfg, res.results)
```

```python
import math
import os
from contextlib import ExitStack

import numpy as np
import concourse.bass as bass
import concourse.mybir as mybir
from concourse.bass_utils import run_bass_kernel_spmd

F32 = mybir.dt.float32
BF16 = mybir.dt.bfloat16
I32 = mybir.dt.int32
U32 = mybir.dt.uint32
AF = mybir.ActivationFunctionType
ALU = mybir.AluOpType
AX = mybir.AxisListType

D = 2048
KC = 16
DIN = 6224
NEG = -1.0e30
EPS = 1e-6
NIT = 20
PW = 256
SAME_ENGINE_SYNC = os.environ.get("SES", "1") == "1"


class Buf:
    __slots__ = ("w", "r", "x")

    def __init__(self, x=False):
        self.w = None
        self.r = []
        self.x = x


class Sched:
    ENG = ("pe", "act", "dve", "pool", "sp")

    def __init__(self, n_lanes=10):
        self.q = {e: [] for e in self.ENG}
        self.cnt = {e: 0 for e in self.ENG}
        self.known = {e: {} for e in self.ENG}
        self.n_lanes = n_lanes
        self.lane_val = {}
        self.lane_next = {"sp": 0, "act": 0, "pool": 0}
        self.ninst = 0

    def semkey_list(self):
        keys = list(self.ENG[:4])
        for qn in ("sp", "act", "pool"):
            for l in range(self.n_lanes):
                keys.append(("lane", qn, l))
        return keys

    def _deps(self, eng, reads, writes):
        need = {}

        def add(d):
            if d is None:
                return
            k, v = d
            if k == eng and (eng == "pe" or not SAME_ENGINE_SYNC):
                return
            if need.get(k, 0) < v:
                need[k] = v

        for b in reads:
            add(b.w)
            if b.x:
                for d in b.r:
                    if d[0] != eng:
                        add(d)
        for b in writes:
            add(b.w)
            for d in b.r:
                add(d)
        out = []
        kn = self.known[eng]
        for k, v in need.items():
            if kn.get(k, 0) >= v:
                continue
            kn[k] = v
            out.append((k, v))
        return out

    def op(self, eng, fn, reads=(), writes=()):
        waits = self._deps(eng, reads, writes)
        self.cnt[eng] += 1
        v = self.cnt[eng]
        self.q[eng].append((waits, fn, (eng, 1)))
        for b in reads:
            b.r.append((eng, v))
        for b in writes:
            b.w = (eng, v)
            b.r = []
        self.ninst += 1

    def dma(self, qeng, fn, reads=(), writes=()):
        lane = self.lane_next[qeng]
        self.lane_next[qeng] = (lane + 1) % self.n_lanes
        key = ("lane", qeng, lane)
        waits = self._deps(qeng, reads, writes)
        prev = self.lane_val.get(key, 0)
        if prev > 0 and self.known[qeng].get(key, 0) < prev:
            self.known[qeng][key] = prev
            waits.append((key, prev))
        v = prev + 16
        self.lane_val[key] = v
        self.q[qeng].append((waits, fn, (key, 16)))
        for b in reads:
            b.r.append((key, v))
        for b in writes:
            b.w = (key, v)
            b.r = []
        self.ninst += 1

    def final_waits(self, eng, bufs):
        waits = {}
        for b in bufs:
            if b.w is not None:
                k, v = b.w
                waits[k] = max(waits.get(k, 0), v)
        self.q[eng].append((list(waits.items()), None, None))

    def emit(self, eng, eobj, semmap):
        for waits, fn, inc in self.q[eng]:
            for k, v in waits:
                eobj.wait_ge(semmap[k], v)
            if fn is None:
                continue
            ins = fn(eobj)
            ins.then_inc(semmap[inc[0]], inc[1])


def make_cfg(full=True):
    if full:
        return dict(NCT=16, NOT=16, PAST=4096, GT=2, TOPK_P=256, TOPK_S=256)
    return dict(NCT=2, NOT=2, PAST=512, GT=2, TOPK_P=128, TOPK_S=144)


def build(cfg):
    NCT, NOT, PAST, GT = cfg["NCT"], cfg["NOT"], cfg["PAST"], cfg["GT"]
    SP_MAX = (NCT + NOT) * 128
    SS_MAX = PAST + 128
    SMAXT = max(SP_MAX, SS_MAX)
    AW = max(SMAXT, 4224)
    nc = bass.Bass("TRN2", target_bir_lowering=False)
    S = Sched()
    es = ExitStack()

    def din(name, shape, dt=F32):
        return nc.dram_tensor(name, list(shape), dt, kind="ExternalInput")

    def dout(name, shape, dt=F32):
        return nc.dram_tensor(name, list(shape), dt, kind="ExternalOutput")

    def dscr(name, shape, dt):
        return nc.dram_tensor(name, list(shape), dt)

    _cnt = [0]

    def sb(shape, dt, name=None):
        _cnt[0] += 1
        return es.enter_context(nc.sbuf_tensor("sb%d_%s" % (_cnt[0], name or "t"), list(shape), dt))

    def ps(shape, dt, name=None):
        _cnt[0] += 1
        return es.enter_context(nc.psum_tensor("ps%d_%s" % (_cnt[0], name or "t"), list(shape), dt))

    x_ctx = din("x_ctx", [NCT * 128, D])
    x_own = din("x_own", [NOT * 128, D])
    xs_d = din("xs", [2, 64, D])
    ck_d = din("ck", [2, PAST, 1024])
    cv_d = din("cv", [2, PAST, 1024])
    cki_d = din("cki", [2, PAST, 64])
    stl_d = din("st_lru", [2, 128, 8])
    stc_d = din("st_conv", [2, 128, 8, 3])
    c3T_d = din("c3T", [128, KC * 3])
    flags_d = din("flags", [128, 4])
    wada_d = din("w_ada", [D, 6 * D])
    bada_d = din("b_ada", [1, 6 * D])
    g1T_d = din("g1T", [128, KC])
    g2T_d = din("g2T", [128, KC])
    win_d = din("w_in", [D, DIN])
    cw_d = din("conv_wT", [128, 4 * 8])
    cb_d = din("conv_bT", [128, 8])
    wa_d = din("lru_waT", [128, 8 * 128])
    wx_d = din("lru_wxT", [128, 8 * 128])
    ba_d = din("lru_baT", [128, 8])
    bx_d = din("lru_bxT", [128, 8])
    lam_d = din("lru_lamT", [128, 8])
    wout_d = din("w_out", [D, D])
    wq_d = din("peer_wq", [D, 1024])
    pk_d = din("peer_keysT", [128, 8 * 128])
    pu_d = din("peer_u", [16384, D])
    pv_d = din("peer_v", [16384, D])
    rb_d = din("rel_bias", [32, 8])
    gf_d = din("g_final", [1, D])
    oh_d = din("oh", [32, 383])

    y_own_d = dout("y_own", [NOT * 128, D])
    ys_d = dout("ys", [2, 64, D])
    k_own_d = dout("k_own", [NOT * 128, 1024])
    v_own_d = dout("v_own", [NOT * 128, 1024])
    ki_own_d = dout("ki_own", [NOT * 128, 64])
    lru_p_d = dout("lru_p", [128, 8])
    conv_p_d = dout("conv_p", [128, 8, 3])
    ks_d = dout("ks", [2, 64, 1024])
    vs_d = dout("vs", [2, 64, 1024])
    kis_d = dout("kis", [2, 64, 64])
    lru_s_d = dout("lru_s", [2, 128, 8])
    conv_s_d = dout("conv_s", [2, 128, 8, 3])
    OUTB = []

    def outbuf():
        b = Buf()
        OUTB.append(b)
        return b

    modrow_d = dscr("modrow", [3, 6 * D], F32)
    B_modrow = Buf()
    bsc_d = dscr("bsc", [8, 128, 383], F32)
    bv_d = dscr("bv", [8, 383], F32)

    def ACT(out, in_, func, R, W, scale=1.0, bias=0.0, accum_out=None):
        if accum_out is None:
            S.op("act", lambda e: e.activation(out=out, in_=in_, func=func, scale=scale, bias=bias), R, W)
        else:
            S.op("act", lambda e: e.activation(out=out, in_=in_, func=func, scale=scale, bias=bias, accum_out=accum_out), R, W)

    def MM(out, lhsT, rhs, st, sp_, R, W, skip=False):
        if skip:
            S.op("pe", lambda e: e.matmul(out, lhsT=lhsT, rhs=rhs, start=st, stop=sp_, skip_group_check=True), R, W)
        else:
            S.op("pe", lambda e: e.matmul(out, lhsT=lhsT, rhs=rhs, start=st, stop=sp_), R, W)

    def TR(out, in_, ident, R, W):
        S.op("pe", lambda e: e.transpose(out=out, in_=in_, identity=ident), R, W)

    def TS(eng, out, in0, s1, s2, op0, op1, R, W, accum_out=None):
        if op1 is None:
            if accum_out is None:
                S.op(eng, lambda e: e.tensor_scalar(out=out, in0=in0, scalar1=s1, scalar2=None, op0=op0), R, W)
            else:
                raise ValueError
        elif accum_out is None:
            S.op(eng, lambda e: e.tensor_scalar(out=out, in0=in0, scalar1=s1, scalar2=s2, op0=op0, op1=op1), R, W)
        else:
            S.op(eng, lambda e: e.tensor_scalar(out=out, in0=in0, scalar1=s1, scalar2=s2, op0=op0, op1=op1, accum_out=accum_out), R, W)

    def TT(eng, out, in0, in1, op, R, W):
        S.op(eng, lambda e: e.tensor_tensor(out=out, in0=in0, in1=in1, op=op), R, W)

    def STT(out, in0, scalar, in1, op0, op1, R, W, accum_out=None):
        if accum_out is None:
            S.op("dve", lambda e: e.scalar_tensor_tensor(out=out, in0=in0, scalar=scalar, in1=in1, op0=op0, op1=op1), R, W)
        else:
            S.op("dve", lambda e: e.scalar_tensor_tensor(out=out, in0=in0, scalar=scalar, in1=in1, op0=op0, op1=op1, accum_out=accum_out), R, W)

    def CP(eng, out, in_, R, W):
        if eng == "act":
            S.op("act", lambda e: e.copy(out=out, in_=in_), R, W)
        else:
            S.op(eng, lambda e: e.tensor_copy(out=out, in_=in_), R, W)

    def MS(eng, ap, val, W):
        S.op(eng, lambda e: e.memset(ap, val), (), W)

    def DMA(q, out, in_, R, W):
        S.dma(q, lambda e: e.dma_start(out=out, in_=in_), R, W)

    identf = sb([128, 128], F32, "identf"); B_identf = Buf()
    ident = sb([128, 128], BF16, "ident"); B_ident = Buf()
    MS("pool", identf[:], 0.0, [B_identf])
    S.op("pool", lambda e: e.affine_select(out=identf[:], in_=identf[:], pattern=[[-1, 128]], compare_op=ALU.not_equal,
                                           fill=1.0, base=0, channel_multiplier=1), [B_identf], [B_identf])
    CP("dve", ident[:], identf[:], [B_identf], [B_ident])
    iota256 = sb([128, 256], F32, "iota"); B_iota = Buf()
    S.op("pool", lambda e: e.iota(iota256[:], pattern=[[1, 256]], base=0, channel_multiplier=0,
                                  allow_small_or_imprecise_dtypes=True), (), [B_iota])
    pow2 = sb([128, NIT + 1], F32, "pow2"); B_pow2 = Buf()
    for k in range(NIT + 1):
        MS("pool", pow2[:, k:k + 1], 2.0 ** (-k), [B_pow2])
    flags = sb([128, 4], F32, "flags"); B_flags = Buf()
    DMA("sp", flags[:], flags_d.ap(), (), [B_flags])

    def ldc(dram, shape, name):
        t = sb(shape, F32, name); b = Buf()
        DMA("sp", t[:], dram.ap(), (), [b])
        return t, b

    cw, B_cw = ldc(cw_d, [128, 32], "cw")
    cb, B_cb = ldc(cb_d, [128, 8], "cb")
    ba, B_ba = ldc(ba_d, [128, 8], "ba")
    bx, B_bx = ldc(bx_d, [128, 8], "bx")
    lam, B_lam = ldc(lam_d, [128, 8], "lam")
    g1T, B_g1T = ldc(g1T_d, [128, KC], "g1T")
    g2T, B_g2T = ldc(g2T_d, [128, KC], "g2T")
    keysT, B_keysT = ldc(pk_d, [128, 1024], "keysT")
    gfin = sb([128, D], F32, "gfin"); B_gfin = Buf()
    DMA("sp", gfin[:], gf_d.ap()[0:1, :].partition_broadcast(128), (), [B_gfin])
    bfar = sb([128, 8], F32, "bfar"); B_bfar = Buf()
    DMA("sp", bfar[:], rb_d.ap()[15:16, :].partition_broadcast(128), (), [B_bfar])
    wa_b = sb([128, 1024], BF16, "wa_b"); B_wa = Buf()
    wx_b = sb([128, 1024], BF16, "wx_b"); B_wx = Buf()
    DMA("pool", wa_b[:], wa_d.ap(), (), [B_wa])
    DMA("pool", wx_b[:], wx_d.ap(), (), [B_wx])
    c1 = sb([128, 8], F32, "c1"); c2 = sb([128, 8], F32, "c2"); B_c12 = Buf()
    ACT(c1[:], lam[:], AF.Exp, [B_lam], [B_c12], scale=-1.0)
    ACT(c1[:], c1[:], AF.Ln, [B_c12], [B_c12], bias=1.0)
    TS("dve", c2[:], c1[:], -16.0, None, ALU.mult, None, [B_c12], [B_c12])
    TS("dve", c1[:], c1[:], -8.0, None, ALU.mult, None, [B_c12], [B_c12])

    psA = ps([128, 512], F32, "psA"); B_psA = Buf(True)
    psB = ps([128, 512], F32, "psB"); B_psB = Buf(True)
    psT = ps([128, 1024], BF16, "psT"); B_psT = Buf(True)
    psT2 = ps([128, 1024], BF16, "psT2"); B_psT2 = Buf(True)
    psACC = ps([128, 2048], F32, "psACC"); B_acc = [Buf(True) for _ in range(4)]
    rot = {"ab": 0, "t": 0}

    def next_ab():
        rot["ab"] ^= 1
        return (psA, B_psA) if rot["ab"] else (psB, B_psB)

    def next_t():
        rot["t"] ^= 1
        return (psT, B_psT) if rot["t"] else (psT2, B_psT2)

    SC = sb([128, AW], F32, "SC"); B_SC = Buf()
    SEL = sb([128, AW], BF16, "SEL"); B_SEL = [Buf(), Buf()]
    PB = sb([128, AW], BF16, "PB"); B_PB = [Buf(), Buf()]
    PTB = sb([128, AW], BF16, "PTB"); B_PTB = [Buf(), Buf()]
    KTB = sb([128, AW], BF16, "KTB"); B_KTB = Buf()
    VB = sb([128, AW], BF16, "VB"); B_VB = Buf()
    small = sb([128, 64], F32, "small"); B_small = Buf()
    B_stg = [Buf(), Buf()]
    uv_d = dscr("uv_scr", [16384, 2 * D], BF16)
    B_uv = Buf()
    wscr = {}
    conv_pending = []
    for wd, ncols in ((win_d, DIN), (wout_d, D), (wq_d, 1024)):
        scr = dscr("bf_" + wd.name, [D, ncols], BF16)
        bufs = {}
        for part, p0 in enumerate(range(0, ncols, 2048)):
            p1 = min(ncols, p0 + 2048)
            for kc in range(KC):
                b_ = Buf()
                bufs[(kc, part)] = b_
                conv_pending.append((wd.ap()[kc * 128:(kc + 1) * 128, p0:p1], scr.ap()[kc * 128:(kc + 1) * 128, p0:p1], p1 - p0, b_))
        wscr[wd.name] = (scr, bufs)
    conv_bufs = []
    for r in range(128):
        for half, src in ((0, pu_d), (1, pv_d)):
            b_ = Buf()
            conv_bufs.append(b_)
            conv_pending.append((src.ap()[r * 128:(r + 1) * 128, :], uv_d.ap()[r * 128:(r + 1) * 128, half * D:(half + 1) * D], D, b_))
    conv_state = {"i": 0}

    def conv_step(n):
        for _ in range(n):
            if not conv_pending:
                return
            src, dst, width, b_ = conv_pending.pop(0)
            i = conv_state["i"]
            conv_state["i"] ^= 1
            stg = VB[:, i * D:i * D + width]
            DMA("pool", stg, src, (), [B_stg[i]])
            DMA("sp", dst, stg, [B_stg[i]], [b_])

    def conv_join():
        conv_step(10 ** 9)
        S.op("dve", lambda e: e.memset(small[:, 63:64], 0.0), conv_bufs, [B_uv, B_VB, B_stg[0], B_stg[1], B_small])

    NPB = 2
    wpb = [sb([128, KC, PW], BF16, "wp%d" % i) for i in range(NPB)]
    B_wpb = [Buf() for _ in range(NPB)]
    wrot = [0]

    def load_piece(wd, c0, width):
        i = wrot[0]
        wrot[0] = (i + 1) % NPB
        if wd.name in wscr:
            scr, bufs = wscr[wd.name]
            part = c0 // 2048
            while any(cp[3] is bufs[(kc, part)] for cp in conv_pending for kc in range(KC)):
                conv_step(1)
            src = scr.ap()[:, c0:c0 + width].rearrange("(kc p) c -> p kc c", p=128)
            DMA("pool", wpb[i][:, :, 0:width], src, [bufs[(kc, part)] for kc in range(KC)], [B_wpb[i]])
        else:
            src = wd.ap()[:, c0:c0 + width].rearrange("(kc p) c -> p kc c", p=128)
            DMA("pool", wpb[i][:, :, 0:width], src, (), [B_wpb[i]])
        conv_step(3)
        return wpb[i], B_wpb[i]

    c3T = sb([128, KC * 3], F32, "c3T"); B_c3T = Buf()
    DMA("sp", c3T[:], c3T_d.ap(), (), [B_c3T])
    sc3 = sb([128, KC * 3], BF16, "sc3"); B_sc3 = Buf()
    ACT(sc3[:], c3T[:], AF.Silu, [B_c3T], [B_sc3])
    modT = sb([128, 4, KC, 3], F32, "modT"); B_modT = Buf()
    mrow = [sb([3, PW], F32, "mrow%d" % i) for i in range(2)]; B_mrow = [Buf(), Buf()]
    bad = [sb([3, PW], F32, "bad%d" % i) for i in range(2)]; B_bad = [Buf(), Buf()]
    for pcs in range(6 * D // PW):
        c0 = pcs * PW
        wp, Bwp = load_piece(wada_d, c0, PW)
        pa, Bpa = next_ab()
        for kc in range(KC):
            MM(pa[0:3, 0:PW], sc3[:, kc * 3:kc * 3 + 3], wp[:, kc, :], kc == 0, kc == KC - 1, [B_sc3, Bwp], [Bpa])
        i = pcs % 2
        DMA("sp", bad[i][:], bada_d.ap()[0:1, c0:c0 + PW].partition_broadcast(3), (), [B_bad[i]])
        TT("dve", mrow[i][:], pa[0:3, 0:PW], bad[i][:], ALU.add, [Bpa, B_bad[i]], [B_mrow[i]])
        DMA("sp", modrow_d.ap()[:, c0:c0 + PW], mrow[i][:], [B_mrow[i]], [B_modrow])
        j6 = c0 // D
        if j6 in (0, 1, 3, 4):
            jj = {0: 0, 1: 1, 3: 2, 4: 3}[j6]
            for cc in range(PW // 128):
                kc = (c0 % D) // 128 + cc
                pa2, Bpa2 = next_ab()
                MM(pa2[:, 0:3], mrow[i][0:3, cc * 128:(cc + 1) * 128], identf[0:3, 0:3], True, True, [B_mrow[i], B_identf], [Bpa2])
                CP("dve", modT[:, jj, kc, :], pa2[:, 0:3], [Bpa2], [B_modT])
    for jj, (gT, Bg) in ((1, (g1T, B_g1T)), (3, (g2T, B_g2T))):
        TS("dve", modT[:, jj, :, :], modT[:, jj, :, :], 1.0, None, ALU.add, None, [B_modT], [B_modT])
        TT("dve", modT[:, jj, :, :], modT[:, jj, :, :], gT[:, :].unsqueeze(2).to_broadcast([128, KC, 3]), ALU.mult, [B_modT, Bg], [B_modT])

    rbt = sb([32, 8], F32, "rbt"); B_rbt = Buf()
    oht = sb([32, 383], F32, "oht"); B_oht = Buf()
    DMA("sp", rbt[:], rb_d.ap(), (), [B_rbt])
    DMA("sp", oht[:], oh_d.ap(), (), [B_oht])
    pa, Bpa = next_ab()
    MM(pa[0:8, 0:383], rbt[:, :], oht[:, :], True, True, [B_rbt, B_oht], [Bpa])
    bvs = sb([8, 383], F32, "bvs"); B_bvs = Buf()
    CP("dve", bvs[:], pa[0:8, 0:383], [Bpa], [B_bvs])
    B_bvd = Buf(); B_bscd = Buf()
    DMA("sp", bv_d.ap(), bvs[:], [B_bvs], [B_bvd])
    bband = sb([128, 8, 256], F32, "bband"); B_bband = Buf()
    brep = sb([128, 383], F32, "brep"); B_brep = Buf()
    for h in range(8):
        DMA("sp", brep[:], bv_d.ap()[h:h + 1, :].partition_broadcast(128), [B_bvd], [B_brep])
        DMA("sp", bsc_d.ap()[h], brep[:], [B_brep], [B_bscd])
        src = bass.AP(bsc_d, h * 128 * 383 + 127, [[382, 128], [1, 256]])
        DMA("sp", bband[:, h, :], src, [B_bscd], [B_bband])
    TT("dve", bband[:], bband[:], bfar[:, :].unsqueeze(2).to_broadcast([128, 8, 256]), ALU.subtract, [B_bband, B_bfar], [B_bband])
    mxdb = sb([128, 8], F32, "mxdb"); B_mxdb = Buf()
    S.op("dve", lambda e: e.tensor_reduce(out=mxdb[:], in_=bband[:], axis=AX.X, op=ALU.max), [B_bband], [B_mxdb])
    TS("dve", mxdb[:], mxdb[:], 0.0, None, ALU.max, None, [B_mxdb], [B_mxdb])

    TG = GT * 128
    hnT = sb([128, KC, TG], BF16, "hnT"); B_hnT = Buf()
    xr_ext = sb([128, 8, 3 + TG], F32, "xr_ext"); B_xr = Buf()
    yrT = sb([128, 8, TG], F32, "yrT"); B_yr = Buf()
    qT = sb([128, 8, TG], BF16, "qT"); B_qT = Buf()
    qiT = sb([128, 8, TG], BF16, "qiT"); B_qiT = Buf()
    kTg = sb([128, 8, TG], BF16, "kTg"); B_kTg = Buf()
    kiTg = sb([64, TG], BF16, "kiTg"); B_kiTg = Buf()
    catT = sb([128, KC, TG], BF16, "catT"); B_cat = [Buf() for _ in range(GT)]
    wi_t = sb([128, GT, 16], F32, "wi_t"); B_wi = Buf()
    hprev = sb([128, 8], F32, "hprev"); B_hprev = Buf()
    xt = sb([128, D], F32, "xt"); B_xt = Buf()
    xsb = sb([128, D], BF16, "xsb"); B_xsb = Buf()
    tm32 = sb([128, GT * 1024], F32, "tm32"); B_tm32 = Buf()
    tm16 = sb([128, 1024], BF16, "tm16"); B_tm16 = Buf()
    gt1b = sb([128, D], F32, "gt1b"); gt2b = sb([128, D], F32, "gt2b"); B_gtb = Buf()

    def rms_hnT(x_rows, P, b, tcol, which):
        jsh, jg = (0, 1) if which == 0 else (2, 3)
        if x_rows is not None:
            DMA("sp", xt[:P, :], x_rows, (), [B_xt])
        ms = small[:, 0:1]
        ACT(xsb[:P, :], xt[:P, :], AF.Square, [B_xt], [B_xsb, B_small], accum_out=ms[:P])
        TS("dve", ms[:P], ms[:P], 1.0 / D, EPS, ALU.mult, ALU.add, [B_small], [B_small])
        ACT(ms[:P], ms[:P], AF.Sqrt, [B_small], [B_small])
        S.op("dve", lambda e: e.reciprocal(out=small[:P, 1:2], in_=ms[:P]), [B_small], [B_small])
        TS("dve", xsb[:P, :], xt[:P, :], small[:P, 1:2], None, ALU.mult, None, [B_xt, B_small], [B_xsb])
        for half in range(2):
            pt, Bpt = next_t()
            for j in range(8):
                kc = half * 8 + j
                TR(pt[:, j * 128:j * 128 + P], xsb[:P, kc * 128:(kc + 1) * 128], ident[:P, :P], [B_xsb, B_ident], [Bpt])
            for j in range(8):
                kc = half * 8 + j
                ACT(hnT[:, kc, tcol:tcol + P], pt[:, j * 128:j * 128 + P], AF.Identity, [Bpt, B_modT], [B_hnT],
                    scale=modT[:, jg, kc, b:b + 1], bias=modT[:, jsh, kc, b:b + 1])

    class Seq:
        pass

    def make_seq(name, b, smax, topk, nreg):
        s = Seq()
        s.b = b
        s.topk = topk
        s.kT = dscr("kT_" + name, [8, 128, smax], BF16)
        s.v = dscr("v_" + name, [8, 128, smax // 128, 128], BF16)
        s.kiT = dscr("kiT_" + name, [128, smax], BF16)
        s.B_reg = [Buf() for _ in range(nreg)]
        return s

    def proj_cm(wp, Bwp, off, M, T, dest, Rd, Wd, eng="act"):
        pa, Bpa = next_ab()
        for kc in range(KC):
            MM(pa[0:M, 0:T], wp[:, kc, off:off + M], hnT[:, kc, 0:T], kc == 0, kc == KC - 1, [Bwp, B_hnT], [Bpa])
        CP(eng, dest, pa[0:M, 0:T], [Bpa] + Rd, Wd)

    def projections(seq, T, tiles, own, kpos0, k_out, v_out, ki_out):
        def pieces(c0, c1):
            return [(c, min(PW, c1 - c)) for c in range(c0, c1, PW)]
        for (c, w) in pieces(0, 1024):
            wp, Bwp = load_piece(win_d, c, w)
            for o in range(0, w, 128):
                blk = (c + o) // 128
                proj_cm(wp, Bwp, o, 128, T, xr_ext[:, blk, 3:3 + T], [], [B_xr])
        if own:
            for (c, w) in pieces(1024, 2048):
                wp, Bwp = load_piece(win_d, c, w)
                for o in range(0, w, 128):
                    blk = (c + o - 1024) // 128
                    proj_cm(wp, Bwp, o, 128, T, yrT[:, blk, 0:T], [], [B_yr])
            for (c, w) in pieces(2048, 3072):
                wp, Bwp = load_piece(win_d, c, w)
                for o in range(0, w, 128):
                    blk = (c + o - 2048) // 128
                    proj_cm(wp, Bwp, o, 128, T, qT[:, blk, 0:T], [], [B_qT])
        SUB4 = cfg.get("SUB4", 99) if own else 99
        if SUB4 < 2:
            return
        for (c, w) in pieces(3072, 4096):
            wp, Bwp = load_piece(win_d, c, w)
            for o in range(0, w, 128):
                blk = (c + o - 3072) // 128
                proj_cm(wp, Bwp, o, 128, T, kTg[:, blk, 0:T], [], [B_kTg], eng="dve")
            if own:
                for ti, (tcol, P, row0) in enumerate(tiles):
                    pa, Bpa = next_ab()
                    for kc in range(KC):
                        MM(pa[:P, 0:w], hnT[:, kc, tcol:tcol + P], wp[:, kc, 0:w], kc == 0, kc == KC - 1, [Bwp, B_hnT], [Bpa])
                    o0 = ti * 1024 + (c - 3072)
                    CP("dve", tm32[:P, o0:o0 + w], pa[:P, 0:w], [Bpa], [B_tm32])
                    if c + w == 4096 and not os.environ.get("NOKDMA"):
                        DMA("pool", k_out[row0:row0 + P, :], tm32[:P, ti * 1024:(ti + 1) * 1024], [B_tm32], [outbuf()])
        regs = [seq.B_reg[(kpos0 + t) // 128] for t in range(0, T, 128)]
        for h in range(8):
            DMA("sp", seq.kT.ap()[h, :, kpos0:kpos0 + T], kTg[:, h, 0:T], [B_kTg], regs)
        if SUB4 < 3:
            return
        for (tcol, P, row0) in tiles:
            for (c, w) in pieces(4096, 5120):
                wp, Bwp = load_piece(win_d, c, w)
                pa, Bpa = next_ab()
                for kc in range(KC):
                    MM(pa[:P, 0:w], hnT[:, kc, tcol:tcol + P], wp[:, kc, 0:w], kc == 0, kc == KC - 1, [Bwp, B_hnT], [Bpa])
                if own:
                    CP("dve", tm32[:P, (c - 4096):(c - 4096) + w], pa[:P, 0:w], [Bpa], [B_tm32])
                CP("act", tm16[:P, (c - 4096):(c - 4096) + w], pa[:P, 0:w], [Bpa], [B_tm16])
            if own:
                DMA("pool", v_out[row0:row0 + P, :], tm32[:P, 0:1024], [B_tm32], [outbuf()])
            kb = (kpos0 + tcol) // 128
            po = (kpos0 + tcol) % 128
            for h in range(8):
                DMA("sp", seq.v.ap()[h, po:po + P, kb, :], tm16[:P, h * 128:(h + 1) * 128], [B_tm16], [seq.B_reg[kb]])
        if SUB4 < 4:
            return
        if own:
            for (c, w) in pieces(5120, 6144):
                wp, Bwp = load_piece(win_d, c, w)
                for o in range(0, w, 128):
                    blk = (c + o - 5120) // 128
                    proj_cm(wp, Bwp, o, 128, T, qiT[:, blk, 0:T], [], [B_qiT])
        if SUB4 < 5:
            return
        wp, Bwp = load_piece(win_d, 6144, 80)
        proj_cm(wp, Bwp, 0, 64, T, kiTg[0:64, 0:T], [], [B_kiTg], eng="dve")
        DMA("sp", seq.kiT.ap()[0:64, kpos0:kpos0 + T], kiTg[0:64, 0:T], [B_kiTg], regs)
        DMA("sp", seq.kiT.ap()[64:128, kpos0:kpos0 + T], kiTg[0:64, 0:T], [B_kiTg], regs)
        if own:
            for ti, (tcol, P, row0) in enumerate(tiles):
                pa, Bpa = next_ab()
                for kc in range(KC):
                    MM(pa[:P, 0:80], hnT[:, kc, tcol:tcol + P], wp[:, kc, 0:80], kc == 0, kc == KC - 1, [Bwp, B_hnT], [Bpa])
                CP("dve", small[:P, 0:64], pa[:P, 0:64], [Bpa], [B_small])
                DMA("pool", ki_out[row0:row0 + P, :], small[:P, 0:64], [B_small], [outbuf()])
                CP("dve", wi_t[:P, ti, :], pa[:P, 64:80], [Bpa], [B_wi])

    def lru_tile(tcol, P, own, first_flag, ti):
        xc = SC[:, 0:1024].rearrange("p (b t) -> p b t", b=8)[:, :, 0:P]
        rr = SC[:, 1024:2048].rearrange("p (b t) -> p b t", b=8)[:, :, 0:P]
        ii = SC[:, 2048:3072].rearrange("p (b t) -> p b t", b=8)[:, :, 0:P]
        aa = SC[:, 3072:4096].rearrange("p (b t) -> p b t", b=8)[:, :, 0:P]
        mm_ = SEL[:, 0:2048].bitcast(F32).rearrange("p (b t) -> p b t", b=8)[:, :, 0:P]
        hh = PB[:, 0:2048].bitcast(F32).rearrange("p (b t) -> p b t", b=8)[:, :, 0:P]
        xcb = PTB[:, 0:1024].rearrange("p (b t) -> p b t", b=8)[:, :, 0:P]
        gy = KTB[:, 0:2048].bitcast(F32).rearrange("p (b t) -> p b t", b=8)[:, :, 0:P]
        Bm, Bh, Bxcb, Bgy = B_SEL[0], B_PB[0], B_PTB[0], B_KTB

        def fork(parents, n):
            inh = [x for pb in parents for x in (([pb.w] if pb.w else []) + pb.r)]
            out = []
            for _ in range(n):
                b_ = Buf()
                b_.r = list(inh)
                out.append(b_)
            return out

        def join(parents, subs):
            for pb in parents:
                for b_ in subs:
                    pb.r += (([b_.w] if b_.w else []) + b_.r)

        Bx = fork([B_SC], 8); Br = fork([B_SC], 8); Bi = fork([B_SC], 8); Ba = fork([B_SC], 8)
        Bm8 = fork([Bm], 8); Bh8 = fork([Bh], 8)
        for j in range(4):
            for blk in range(8):
                if j == 0:
                    TS("dve", xc[:, blk, :], xr_ext[:, blk, tcol:tcol + P], cw[:, blk:blk + 1], cb[:, blk:blk + 1], ALU.mult, ALU.add,
                       [B_xr, B_cw, B_cb], [Bx[blk]])
                else:
                    STT(xc[:, blk, :], xr_ext[:, blk, tcol + j:tcol + j + P], cw[:, j * 8 + blk:j * 8 + blk + 1], xc[:, blk, :],
                        ALU.mult, ALU.add, [B_xr, B_cw, Bx[blk]], [Bx[blk]])
        CP("pool", xcb, xc, Bx, [Bxcb])
        for blk in range(8):
            pa, Bpa = next_ab()
            MM(pa[:, 0:P], wa_b[:, blk * 128:(blk + 1) * 128], xcb[:, blk, :], True, True, [B_wa, Bxcb], [Bpa])
            ACT(rr[:, blk, :], pa[:, 0:P], AF.Sigmoid, [Bpa, B_ba], [Br[blk]], bias=ba[:, blk:blk + 1])
            pa, Bpa = next_ab()
            MM(pa[:, 0:P], wx_b[:, blk * 128:(blk + 1) * 128], xcb[:, blk, :], True, True, [B_wx, Bxcb], [Bpa])
            ACT(ii[:, blk, :], pa[:, 0:P], AF.Sigmoid, [Bpa, B_bx], [Bi[blk]], bias=bx[:, blk:blk + 1])
        for blk in range(8):
            ACT(aa[:, blk, :], rr[:, blk, :], AF.Exp, [Br[blk], B_c12], [Ba[blk]], scale=c1[:, blk:blk + 1])
            ACT(mm_[:, blk, :], rr[:, blk, :], AF.Exp, [Br[blk], B_c12], [Bm8[blk]], scale=c2[:, blk:blk + 1])
        TS("dve", mm_, mm_, -1.0, 1.0, ALU.mult, ALU.add, Bm8, Bm8)
        ACT(mm_, mm_, AF.Sqrt, Bm8, Bm8)
        if first_flag == "always":
            MS("dve", mm_[:, :, 0:1], 1.0, Bm8)
        elif first_flag == "flag":
            TS("dve", mm_[:, :, 0:1], mm_[:, :, 0:1], flags[:, 0:1], flags[:, 1:2], ALU.mult, ALU.add, Bm8 + [B_flags], Bm8)
        TT("dve", ii, ii, xc, ALU.mult, Bi + Bx, Bi)
        TT("dve", ii, ii, mm_, ALU.mult, Bi + Bm8, Bi)
        for blk in range(8):
            S.op("dve", lambda e, blk=blk: e.tensor_tensor_scan(out=hh[:, blk, :], data0=aa[:, blk, :], data1=ii[:, blk, :],
                                                                initial=hprev[:, blk:blk + 1], op0=ALU.mult, op1=ALU.add),
                 [Ba[blk], Bi[blk], B_hprev], [Bh8[blk]])
        CP("dve", hprev[:, :].unsqueeze(2), hh[:, :, P - 1:P], Bh8, [B_hprev])
        if own:
            ACT(gy, yrT[:, :, tcol:tcol + P], AF.Gelu_apprx_tanh, [B_yr], [Bgy])
            TT("dve", catT[:, 0:8, tcol:tcol + P], hh, gy, ALU.mult, Bh8 + [Bgy], [B_cat[ti]])
        join([B_SC], Bx + Br + Bi + Ba)
        join([Bm], Bm8)
        join([Bh], Bh8)

    def attention_tile(seq, tcol, P, kpos, S_ctxpen, ti):
        S_t = kpos + P
        nb128 = (S_t + 127) // 128
        regs = seq.B_reg[0:nb128]
        kiTs = KTB
        DMA("sp", kiTs[:, 0:S_t], seq.kiT.ap()[:, 0:S_t], regs, [B_KTB])
        awi = small[:, 16:32]
        sgn = small[:, 32:48]
        wi = wi_t[:, ti, :]
        ACT(awi[:P], wi[:P], AF.Abs, [B_wi], [B_small], scale=0.03125)
        TS("dve", sgn[:P], wi[:P], 0.0, 2.0, ALU.is_gt, ALU.mult, [B_wi], [B_small])
        TS("dve", sgn[:P], sgn[:P], -1.0, None, ALU.add, None, [B_small], [B_small])
        dsg = PTB[:, 0:16 * 128].rearrange("p (h q) -> p h q", h=16)
        TT("dve", dsg[:P, :, :P], ident[:P, :P].unsqueeze(1).to_broadcast([P, 16, P]), sgn[:P, :].unsqueeze(2).to_broadcast([P, 16, P]),
           ALU.mult, [B_ident, B_small], [B_PTB[0]])
        score = SC
        idx_banks = [(psA[:, :], B_psA), (psB[:, :], B_psB), (psT[:, :].bitcast(F32), B_psT), (psT2[:, :].bitcast(F32), B_psT2),
                     (psACC[:, 1024:1536], B_acc[2]), (psACC[:, 1536:2048], B_acc[3])]
        B_tmp = [Buf() for _ in range(8)]
        for b_ in B_tmp:
            b_.r = [x for pb in B_PB for x in (([pb.w] if pb.w else []) + pb.r)]
        kk = 0
        IW = 512
        SKW = 3
        items = []
        for s0 in range(0, S_t, IW):
            for h in range(16):
                items.append((s0, min(IW, S_t - s0), h, (s0 // IW) % 2))

        def idx_tail(k):
            s0, w, h, accb = items[k]
            acc = psACC[:, accb * 512:(accb + 1) * 512]
            tv = PB[:, (k % 8) * 512:(k % 8) * 512 + w]
            MM(acc[:P, 0:w], dsg[:P, h, :P], tv[:P], h == 0, h == 15, [B_PTB[0], B_tmp[k % 8]], [B_acc[accb]])
            if h == 15:
                CP("dve" if accb == 0 else "act", score[:P, s0:s0 + w], acc[:P, 0:w], [B_acc[accb]], [B_SC])

        for k, (s0, w, h, accb) in enumerate(items):
            c, base = h // 2, (h % 2) * 64
            pa, Bpa = idx_banks[k % 6]
            tv = PB[:, (k % 8) * 512:(k % 8) * 512 + w]
            tb = B_tmp[k % 8]
            MM(pa[:P, 0:w], qiT[base:base + 64, c, tcol:tcol + P], kiTs[base:base + 64, s0:s0 + w], True, True, [B_qiT, B_KTB], [Bpa])
            if h % 2 == 0:
                ACT(tv[:P], pa[:P, 0:w], AF.Relu, [Bpa, B_small], [tb], scale=awi[:P, h:h + 1])
            else:
                TS("dve", tv[:P], pa[:P, 0:w], 0.0, awi[:P, h:h + 1], ALU.max, ALU.mult, [Bpa, B_small], [tb])
            if k >= SKW:
                idx_tail(k - SKW)
        for k in range(max(0, len(items) - SKW), len(items)):
            idx_tail(k)
        for pb in B_PB:
            for b_ in B_tmp:
                pb.r += (([b_.w] if b_.w else []) + b_.r)
        lo = small[:, 2:3]; wdt = small[:, 3:4]; mid = small[:, 4:5]; cnt = small[:, 5:6]; stp = small[:, 6:7]
        S.op("dve", lambda e: e.tensor_reduce(out=lo[:P], in_=score[:P, 0:S_t], axis=AX.X, op=ALU.min), [B_SC], [B_small])
        S.op("dve", lambda e: e.tensor_reduce(out=wdt[:P], in_=score[:P, 0:S_t], axis=AX.X, op=ALU.max), [B_SC], [B_small])
        TT("dve", wdt[:P], wdt[:P], lo[:P], ALU.subtract, [B_small], [B_small])
        TS("dve", wdt[:P], wdt[:P], 1.0e-6, None, ALU.add, None, [B_small], [B_small])
        wk = small[:, 40:40 + NIT + 1]
        TS("dve", wk[:P], pow2[:P, :], wdt[:P], None, ALU.mult, None, [B_pow2, B_small], [B_small])
        if P == 128:
            MS("dve", score[0:64, kpos + 64:kpos + 128], NEG, [B_SC])
        if S_ctxpen > 0:
            TS("dve", score[:P, 0:S_ctxpen], score[:P, 0:S_ctxpen], flags[:P, 2:3], None, ALU.add, None, [B_SC, B_flags], [B_SC])
        junk = PB
        for k in range(1, NIT + 1):
            TT("dve", mid[:P], lo[:P], wk[:P, k:k + 1], ALU.add, [B_small], [B_small])
            TS("dve", junk[:P, 0:S_t], score[:P, 0:S_t], mid[:P], None, ALU.is_ge, ALU.add, [B_SC, B_small], [B_PB[0], B_PB[1], B_small],
               accum_out=cnt[:P])
            STT(stp[:P], cnt[:P], float(seq.topk) - 0.5, wk[:P, k:k + 1], ALU.is_ge, ALU.mult, [B_small], [B_small])
            TT("dve", lo[:P], lo[:P], stp[:P], ALU.add, [B_small], [B_small])
        sel = SEL
        TS("dve", sel[:P, 0:S_t], score[:P, 0:S_t], lo[:P], None, ALU.is_ge, None, [B_SC, B_small], [B_SEL[0], B_SEL[1]])
        L = SC
        mx8 = small[:, 8:16]
        rs = small[:, 48:56]
        po = psACC[:, 0:1024]
        nblk = (S_t + 511) // 512
        assert nblk <= 9
        band0 = kpos - 128
        for h in range(8):
            kTh = KTB
            DMA("sp", kTh[:, 0:S_t], seq.kT.ap()[h, :, 0:S_t], regs, [B_KTB])
            Vh = VB[:, 0:nb128 * 128].rearrange("p (n d) -> p n d", d=128)
            DMA("sp", Vh, seq.v.ap()[h, :, 0:nb128, :], regs, [B_VB])
            for bi, s0 in enumerate(range(0, S_t, 512)):
                w = min(512, S_t - s0)
                pa, Bpa = next_ab()
                MM(pa[:P, 0:w], qT[:, h, tcol:tcol + P], kTh[:, s0:s0 + w], True, True, [B_qT, B_KTB], [Bpa])
                S.op("dve", lambda e, pa=pa, s0=s0, w=w, bi=bi: e.tensor_scalar(
                    out=L[:P, s0:s0 + w], in0=pa[:P, 0:w], scalar1=128.0 ** -0.5, scalar2=None, op0=ALU.mult, op1=ALU.max,
                    accum_out=small[:P, 24 + bi:25 + bi]), [Bpa], [B_SC, B_small])
            b_lo = max(band0, 0)
            b_hi = min(kpos + 128, S_t)
            TT("dve", L[:P, b_lo:b_hi], L[:P, b_lo:b_hi], bband[:P, h, b_lo - band0:b_hi - band0], ALU.add, [B_SC, B_bband], [B_SC])
            S.op("dve", lambda e: e.tensor_reduce(out=mx8[:P, h:h + 1], in_=small[:P, 24:24 + nblk], axis=AX.X, op=ALU.max),
                 [B_small], [B_small])
            TS("dve", mx8[:P, h:h + 1], mx8[:P, h:h + 1], mxdb[:P, h:h + 1], -1.0, ALU.add, ALU.mult, [B_small, B_mxdb], [B_small])
            Pm = PB
            ACT(Pm[:P, 0:S_t], L[:P, 0:S_t], AF.Exp, [B_SC, B_small], [B_PB[0], B_PB[1]], bias=mx8[:P, h:h + 1])
            STT(Pm[:P, 0:S_t], Pm[:P, 0:S_t], 1.0, sel[:P, 0:S_t], ALU.mult, ALU.mult, [B_PB[0], B_PB[1], B_SEL[0], B_SEL[1]],
                [B_PB[0], B_PB[1], B_small], accum_out=rs[:P, h:h + 1])
            PT = PTB
            for j0 in range(0, nb128, 8):
                pt, Bpt = next_t()
                nj = min(8, nb128 - j0)
                for jj in range(nj):
                    j = j0 + jj
                    ws = min(128, S_t - j * 128)
                    TR(pt[0:ws, jj * 128:jj * 128 + P], Pm[:P, j * 128:j * 128 + ws], ident[:P, :P], [B_PB[0], B_PB[1], B_ident], [Bpt])
                ws_last = min(128, S_t - (j0 + nj - 1) * 128)
                if ws_last == 128:
                    CP("act" if (j0 // 8) % 2 == 0 else "dve", PT[:, j0 * 128:(j0 + nj) * 128], pt[:, 0:nj * 128], [Bpt], [B_PTB[0], B_PTB[1]])
                else:
                    if nj > 1:
                        CP("act", PT[:, j0 * 128:(j0 + nj - 1) * 128], pt[:, 0:(nj - 1) * 128], [Bpt], [B_PTB[0], B_PTB[1]])
                    CP("dve", PT[0:ws_last, (j0 + nj - 1) * 128:(j0 + nj) * 128], pt[0:ws_last, (nj - 1) * 128:nj * 128], [Bpt],
                       [B_PTB[0], B_PTB[1]])
            for j in range(nb128):
                ws = min(128, S_t - j * 128)
                MM(po[:P, h * 128:(h + 1) * 128], PT[0:ws, j * 128:j * 128 + P], Vh[0:ws, j, :], j == 0, j == nb128 - 1,
                   [B_PTB[0], B_PTB[1], B_VB], [B_acc[0], B_acc[1]])
        rinv = small[:, 56:64]
        S.op("dve", lambda e: e.reciprocal(out=rinv[:P], in_=rs[:P]), [B_small], [B_small])
        attn = tm16
        for h in range(8):
            ACT(attn[:P, h * 128:(h + 1) * 128], po[:P, h * 128:(h + 1) * 128], AF.Identity, [B_acc[0], B_acc[1], B_small], [B_tm16],
                scale=rinv[:P, h:h + 1])
        pt, Bpt = next_t()
        for h in range(8):
            TR(pt[:, h * 128:h * 128 + P], attn[:P, h * 128:(h + 1) * 128], ident[:P, :P], [B_tm16, B_ident], [Bpt])
        for h in range(8):
            CP("dve", catT[:, 8 + h, tcol:tcol + P], pt[:, h * 128:h * 128 + P], [Bpt], [B_cat[ti]])

    def mix_residual(x_rows, P, tcol, ti):
        for db in range(D // PW):
            wp, Bwp = load_piece(wout_d, db * PW, PW)
            bk = B_acc[(db * PW) // 512]
            for kc in range(KC):
                MM(psACC[:P, db * PW:(db + 1) * PW], catT[:, kc, tcol:tcol + P], wp[:, kc, :], kc == 0, kc == KC - 1, [B_cat[ti], Bwp], [bk])
        x1 = SC[:, 0:D]
        TT("dve", x1[:P], psACC[:P, :], gt1b[:P, :], ALU.mult, B_acc + [B_gtb], [B_SC])
        DMA("sp", xt[:P, :], x_rows, (), [B_xt])
        TT("dve", xt[:P, :], xt[:P, :], x1[:P], ALU.add, [B_xt, B_SC], [B_xt])

    def peer_tile(P, y_rows):
        rms_hnT(None, P, cur["b"], 0, 1)
        hn2 = sb2_hn2
        for half in range(2):
            pt, Bpt = next_t()
            for j in range(8):
                kc = half * 8 + j
                TR(pt[:P, j * 128:(j + 1) * 128], hnT[:, kc, 0:P], ident[:, :], [B_hnT, B_ident], [Bpt])
            CP("act" if half == 0 else "dve", hn2[:P, half * 1024:(half + 1) * 1024], pt[:P, :], [Bpt], [B_hn2])
        pq = tm32[:, 0:1024].rearrange("p (c t) -> p c t", c=8)
        for pc in range(1024 // PW):
            wp, Bwp = load_piece(wq_d, pc * PW, PW)
            for o in range(0, PW, 128):
                c = (pc * PW + o) // 128
                pa, Bpa = next_ab()
                for kc in range(KC):
                    MM(pa[:, 0:P], wp[:, kc, o:o + 128], hnT[:, kc, 0:P], kc == 0, kc == KC - 1, [Bwp, B_hnT], [Bpa])
                CP("act", pq[:, c, 0:P], pa[:, 0:P], [Bpa], [B_tm32])
        W0 = D
        s12 = SC[:, W0:W0 + 256].rearrange("p (a n) -> p a n", a=2)
        s12b = SC[:, W0 + 256:W0 + 512].rearrange("p (a n) -> p a n", a=2)
        cand = SC[:, W0 + 512:W0 + 768]
        candb = SC[:, W0 + 768:W0 + 1024]
        cidx = SC[:, W0 + 1024:W0 + 1280]
        jk = SC[:, W0 + 1280:W0 + 1536]
        v12 = pst_small[:, 0:32].rearrange("p (a k) -> p a k", a=2)
        i12 = pst_small_u[:, 32:64].rearrange("p (a k) -> p a k", a=2)
        i12f = pst_small[:, 64:96].rearrange("p (a k) -> p a k", a=2)
        ts_ = pst_small[:, 96:112]
        tj = pst_small_u[:, 112:128]
        tjf = pst_small[:, 128:144]
        ssum = pst_small[:, 144:145]
        negm = pst_small[:, 145:146]
        Bq = B_pst
        Be_h = [Buf() for _ in range(8)]
        Bei_h = [Buf() for _ in range(8)]
        Bgw_h = [Buf() for _ in range(8)]

        def head_ops(h):
            for a in range(2):
                pa, Bpa = next_ab()
                MM(pa[:P, 0:128], pq[a * 64:(a + 1) * 64, h, 0:P], keysT[a * 64:(a + 1) * 64, h * 128:(h + 1) * 128], True, True,
                   [B_tm32, B_keysT], [Bpa])
                CP("act", s12[:P, a, :], pa[:P, 0:128], [Bpa], [B_SC])
                S.op("dve", lambda e, a=a: e.max(out=v12[:P, a, 0:8], in_=s12[:P, a, :]), [B_SC], [Bq])
                S.op("dve", lambda e, a=a: e.match_replace(out=s12b[:P, a, :], in_to_replace=v12[:P, a, 0:8], in_values=s12[:P, a, :],
                                                          imm_value=NEG), [B_SC, Bq], [B_SC])
                yield
                S.op("dve", lambda e, a=a: e.max(out=v12[:P, a, 8:16], in_=s12b[:P, a, :]), [B_SC, Bq], [Bq])
                S.op("dve", lambda e, a=a: e.max_index(out=i12[:P, a, 0:8], in_max=v12[:P, a, 0:8], in_values=s12[:P, a, :]), [B_SC, Bq], [Bq])
                S.op("dve", lambda e, a=a: e.max_index(out=i12[:P, a, 8:16], in_max=v12[:P, a, 8:16], in_values=s12[:P, a, :]), [B_SC, Bq], [Bq])
                yield
            CP("dve", i12f[:P], i12[:P], [Bq], [Bq])
            c3 = cand.rearrange("p (a b) -> p a b", a=16)
            TT("dve", c3[:P], v12[:P, 0, :].unsqueeze(2).to_broadcast([P, 16, 16]), v12[:P, 1, :].unsqueeze(1).to_broadcast([P, 16, 16]),
               ALU.add, [Bq], [B_SC])
            ci3 = cidx.rearrange("p (a b) -> p a b", a=16)
            STT(ci3[:P], i12f[:P, 0, :].unsqueeze(2).to_broadcast([P, 16, 16]), 128.0, i12f[:P, 1, :].unsqueeze(1).to_broadcast([P, 16, 16]),
                ALU.mult, ALU.add, [Bq], [B_SC])
            yield
            S.op("dve", lambda e: e.max(out=ts_[:P, 0:8], in_=cand[:P, :]), [B_SC], [Bq])
            S.op("dve", lambda e: e.match_replace(out=candb[:P, :], in_to_replace=ts_[:P, 0:8], in_values=cand[:P, :], imm_value=NEG),
                 [B_SC, Bq], [B_SC])
            S.op("dve", lambda e: e.max(out=ts_[:P, 8:16], in_=candb[:P, :]), [B_SC, Bq], [Bq])
            yield
            S.op("dve", lambda e: e.max_index(out=tj[:P, 0:8], in_max=ts_[:P, 0:8], in_values=cand[:P, :]), [B_SC, Bq], [Bq])
            S.op("dve", lambda e: e.max_index(out=tj[:P, 8:16], in_max=ts_[:P, 8:16], in_values=cand[:P, :]), [B_SC, Bq], [Bq])
            CP("dve", tjf[:P], tj[:P], [Bq], [Bq])
            yield
            for k in range(16):
                STT(jk[:P, :], iota256[:P, :], tjf[:P, k:k + 1], cidx[:P, :], ALU.is_equal, ALU.mult, [B_iota, Bq, B_SC], [B_SC, Be_h[h]],
                    accum_out=eidxf[:P, h * 16 + k:h * 16 + k + 1])
                if k % 2 == 1:
                    yield
            TS("dve", negm[:P], ts_[:P, 0:1], -1.0, None, ALU.mult, None, [Bq], [Bq])
            ACT(gw[:P, h * 16:(h + 1) * 16], ts_[:P, :], AF.Exp, [Bq], [Bgw_h[h], Bq], bias=negm[:P], accum_out=ssum[:P])
            S.op("dve", lambda e: e.reciprocal(out=ssum[:P], in_=ssum[:P]), [Bq], [Bq])
            TS("dve", gw[:P, h * 16:(h + 1) * 16], gw[:P, h * 16:(h + 1) * 16], ssum[:P], None, ALU.mult, None, [Bgw_h[h], Bq], [Bgw_h[h]])
            CP("dve", eidx[:P, h * 16:(h + 1) * 16], eidxf[:P, h * 16:(h + 1) * 16], [Be_h[h]], [Bei_h[h]])
            yield

        NG = 5

        def slot(sl):
            h = sl // 16
            g, Bg = uvg[sl % NG], B_uvg[sl % NG]
            S.dma("pool", lambda e: e.indirect_dma_start(out=g[:P, :], out_offset=None, in_=uv_d.ap(),
                                                         in_offset=bass.IndirectOffsetOnAxis(ap=eidx[:P, sl:sl + 1], axis=0)),
                  [Bei_h[h], B_uv], Bg)
            STT(g[:P, 0:D], g[:P, 0:D], 1.0, hn2[:P, :], ALU.mult, ALU.mult, Bg + [B_hn2], [Bg[0], B_actv[sl % 8]],
                accum_out=actv[:P, sl:sl + 1])
            ACT(gel[:P, sl:sl + 1], actv[:P, sl:sl + 1], AF.Gelu_apprx_tanh, [B_actv[sl % 8]], [B_gel[sl % 8]])
            ACT(gel[:P, sl:sl + 1], gel[:P, sl:sl + 1], AF.Identity, [B_gel[sl % 8], Bgw_h[h]], [B_gel[sl % 8]], scale=gw[:P, sl:sl + 1])
            dg, Bdg = dgs[sl % 2], B_dgs[sl % 2]
            ACT(dg[:P, :P], ident[:P, :P], AF.Identity, [B_ident, B_gel[sl % 8]], [Bdg], scale=gel[:P, sl:sl + 1])
            for nb in range(4):
                MM(psACC[:P, nb * 512:(nb + 1) * 512], dg[:P, :P], g[:P, D + nb * 512:D + (nb + 1) * 512], sl == 0, sl == 127,
                   [Bdg] + Bg, [B_acc[nb]])

        LAG = 2
        next_slot = 0
        for h in range(8):
            avail = max(0, (h - LAG + 1)) * 16
            for _ in head_ops(h):
                if next_slot < avail:
                    slot(next_slot)
                    next_slot += 1
        while next_slot < 128:
            slot(next_slot)
            next_slot += 1
        x2 = SC[:, 0:D]
        TT("dve", x2[:P], psACC[:P, :], gt2b[:P, :], ALU.mult, B_acc + [B_gtb], [B_SC])
        TT("dve", x2[:P], x2[:P], xt[:P, :], ALU.add, [B_SC, B_xt], [B_SC])
        ms = small[:, 0:1]
        ACT(xsb[:P, :], x2[:P], AF.Square, [B_SC], [B_xsb, B_small], accum_out=ms[:P])
        TS("dve", ms[:P], ms[:P], 1.0 / D, EPS, ALU.mult, ALU.add, [B_small], [B_small])
        ACT(ms[:P], ms[:P], AF.Sqrt, [B_small], [B_small])
        S.op("dve", lambda e: e.reciprocal(out=small[:P, 1:2], in_=ms[:P]), [B_small], [B_small])
        STT(xt[:P, :], x2[:P], small[:P, 1:2], gfin[:P, :], ALU.mult, ALU.mult, [B_SC, B_small, B_gfin], [B_xt])
        DMA("pool", y_rows, xt[:P, :], [B_xt], [outbuf()])

    sb2_hn2 = sb([128, D], BF16, "hn2"); B_hn2 = Buf()
    pst_small = sb([128, 160], F32, "pst"); B_pst = Buf()
    pst_small_u = pst_small[:, :].bitcast(U32)
    eidxf = sb([128, 128], F32, "eidxf"); B_eidx = Buf()
    eidx = sb([128, 128], I32, "eidx"); B_eidxi = Buf()
    gw = sb([128, 128], F32, "gw"); B_gw = Buf()
    actv = sb([128, 128], F32, "actv"); B_actv = [Buf() for _ in range(8)]
    gel = sb([128, 128], F32, "gel"); B_gel = [Buf() for _ in range(8)]
    uvg = [SEL[:, 0:2 * D], PB[:, 0:2 * D], PTB[:, 0:2 * D], KTB[:, 0:2 * D], VB[:, 0:2 * D]]
    B_uvg = [B_SEL, B_PB, B_PTB, [B_KTB], [B_VB]]
    dgs = [sb([128, 128], BF16, "dg%d" % i) for i in range(2)]; B_dgs = [Buf(), Buf()]
    cur = {"b": 0}
    halo_st = sb([128, 8, 3], F32, "halo_st"); B_halo = Buf()

    def load_seq_mod(b):
        cur["b"] = b
        DMA("sp", gt1b[:], modrow_d.ap()[b:b + 1, 2 * D:3 * D].partition_broadcast(128), [B_modrow], [B_gtb])
        DMA("sp", gt2b[:], modrow_d.ap()[b:b + 1, 5 * D:6 * D].partition_broadcast(128), [B_modrow], [B_gtb])

    STAGE = cfg.get("STAGE", 99)
    pseq = make_seq("p", 0, SP_MAX, cfg["TOPK_P"], SP_MAX // 128)
    load_seq_mod(0)
    MS("dve", hprev[:], 0.0, [B_hprev])
    MS("dve", xr_ext[:, :, 0:3], 0.0, [B_xr])
    S_CTX = NCT * 128
    for g in range(NCT // GT if STAGE >= 2 else 0):
        T = GT * 128
        tiles = []
        for t in range(GT):
            row0 = (g * GT + t) * 128
            rms_hnT(x_ctx.ap()[row0:row0 + 128, :], 128, 0, t * 128, 0)
            tiles.append((t * 128, 128, row0))
        SUB = cfg.get("SUB", 99)
        if SUB >= 2:
            projections(pseq, T, tiles, False, g * T, None, None, None)
        for t in range(GT if SUB >= 3 else 0):
            lru_tile(t * 128, 128, False, "always" if (g == 0 and t == 0) else None, t)
        CP("dve", xr_ext[:, :, 0:3], xr_ext[:, :, T:T + 3], [B_xr], [B_xr])
    conv_join()
    TS("dve", hprev[:], hprev[:], flags[:, 0:1], None, ALU.mult, None, [B_hprev, B_flags], [B_hprev])
    TS("dve", xr_ext[:, :, 0:3], xr_ext[:, :, 0:3], flags[:, 0:1], None, ALU.mult, None, [B_xr, B_flags], [B_xr])
    for g in range(NOT // GT if STAGE >= 3 else 0):
        T = GT * 128
        tiles = []
        for t in range(GT):
            row0 = (g * GT + t) * 128
            rms_hnT(x_own.ap()[row0:row0 + 128, :], 128, 0, t * 128, 0)
            tiles.append((t * 128, 128, row0))
        projections(pseq, T, tiles, True, S_CTX + g * T, k_own_d.ap(), v_own_d.ap(), ki_own_d.ap())
        for t in range(GT if cfg.get("SUB3", 99) >= 2 else 0):
            lru_tile(t * 128, 128, True, "flag" if (g == 0 and t == 0) else None, t)
        if g == NOT // GT - 1 and cfg.get("SUB3", 99) >= 3:
            CP("dve", halo_st[:], xr_ext[:, :, T:T + 3], [B_xr], [B_halo])
            DMA("pool", conv_p_d.ap(), halo_st[:], [B_halo], [outbuf()])
            DMA("pool", lru_p_d.ap(), hprev[:], [B_hprev], [outbuf()])
        CP("dve", xr_ext[:, :, 0:3], xr_ext[:, :, T:T + 3], [B_xr], [B_xr])
        for t in range(GT):
            row0 = (g * GT + t) * 128
            if STAGE >= 4:
                attention_tile(pseq, t * 128, 128, S_CTX + g * T + t * 128, S_CTX, t)
            if STAGE >= 5:
                mix_residual(x_own.ap()[row0:row0 + 128, :], 128, t * 128, t)
            if STAGE >= 6:
                peer_tile(128, y_own_d.ap()[row0:row0 + 128, :])

    for sbi in range(2 if STAGE >= 7 else 0):
        sseq = make_seq("s%d" % sbi, 1 + sbi, SS_MAX, cfg["TOPK_S"], SS_MAX // 128)
        load_seq_mod(1 + sbi)
        kst = [PB[:, 0:4096].rearrange("p (n c) -> p n c", n=4), VB[:, 0:4096].rearrange("p (n c) -> p n c", n=4)]
        B_kst = [B_PB, [B_VB]]
        kts = [PTB[:, 0:4096].rearrange("p (h s) -> p h s", h=8), KTB[:, 0:4096].rearrange("p (h s) -> p h s", h=8)]
        B_kts = [B_PTB, [B_KTB]]
        vst = [SEL[:, 0:4096].rearrange("p (n c) -> p n c", n=4), SC[:, 0:2048].bitcast(BF16).rearrange("p (n c) -> p n c", n=4)]
        B_vst = [B_SEL, [B_SC]]
        for cb4 in range(PAST // 512):
            r0 = cb4 * 512
            i = cb4 % 2
            regs4 = sseq.B_reg[cb4 * 4:cb4 * 4 + 4]
            DMA("pool", kst[i], ck_d.ap()[sbi, r0:r0 + 512, :].rearrange("(n p) c -> p n c", p=128), (), B_kst[i])
            for n in range(4):
                pt, Bpt = next_t()
                for h in range(8):
                    TR(pt[:, h * 128:(h + 1) * 128], kst[i][:, n, h * 128:(h + 1) * 128], ident[:, :], B_kst[i] + [B_ident], [Bpt])
                CP("act" if n % 2 == 0 else "dve", kts[i][:, :, n * 128:(n + 1) * 128], pt[:, :].rearrange("p (h s) -> p h s", h=8),
                   [Bpt], B_kts[i])
            for h in range(8):
                DMA("sp", sseq.kT.ap()[h, :, r0:r0 + 512], kts[i][:, h, :], B_kts[i], regs4)
            DMA("pool", vst[i], cv_d.ap()[sbi, r0:r0 + 512, :].rearrange("(n p) c -> p n c", p=128), (), B_vst[i])
            for h in range(8):
                DMA("sp", sseq.v.ap()[h, :, cb4 * 4:cb4 * 4 + 4, :], vst[i][:, :, h * 128:(h + 1) * 128], B_vst[i], regs4)
            kis = xsb[:, 0:1024].rearrange("p (n c) -> p n c", n=4)
            DMA("pool", kis[:, :, 0:64], cki_d.ap()[sbi, r0:r0 + 512, :].rearrange("(n p) c -> p n c", p=128), (), [B_xsb])
            CP("dve", kis[:, :, 64:128], kis[:, :, 0:64], [B_xsb], [B_xsb])
            pt, Bpt = next_t()
            for n in range(4):
                TR(pt[:, n * 128:(n + 1) * 128], kis[:, n, 0:128], ident[:, :], [B_xsb, B_ident], [Bpt])
            CP("dve", sb2_hn2[:, 0:512], pt[:, 0:512], [Bpt], [B_hn2])
            DMA("sp", sseq.kiT.ap()[:, r0:r0 + 512], sb2_hn2[:, 0:512], [B_hn2], regs4)
        DMA("sp", hprev[:], stl_d.ap()[sbi], (), [B_hprev])
        DMA("sp", halo_st[:], stc_d.ap()[sbi], (), [B_halo])
        CP("dve", xr_ext[:, :, 0:3], halo_st[:], [B_halo], [B_xr])
        T = 64
        rms_hnT(xs_d.ap()[sbi], 64, 1 + sbi, 0, 0)
        tiles = [(0, 64, 0)]
        projections(sseq, T, tiles, True, PAST, ks_d.ap()[sbi], vs_d.ap()[sbi], kis_d.ap()[sbi])
        lru_tile(0, 64, True, None, 0)
        CP("dve", halo_st[:], xr_ext[:, :, T:T + 3], [B_xr], [B_halo])
        DMA("pool", conv_s_d.ap()[sbi], halo_st[:], [B_halo], [outbuf()])
        DMA("pool", lru_s_d.ap()[sbi], hprev[:], [B_hprev], [outbuf()])
        attention_tile(sseq, 0, 64, PAST, 0, 0)
        mix_residual(xs_d.ap()[sbi], 64, 0, 0)
        peer_tile(64, ys_d.ap()[sbi])

    S.final_waits("sp", OUTB)
    keys = S.semkey_list()
    semmap = {}
    for i, k in enumerate(keys):
        semmap[k] = es.enter_context(nc.semaphore("sem%d" % i))
    with nc.Block() as block:
        @block.tensor
        def _(e):
            S.emit("pe", e, semmap)

        @block.scalar
        def _(e):
            S.emit("act", e, semmap)

        @block.vector
        def _(e):
            S.emit("dve", e, semmap)

        @block.gpsimd
        def _(e):
            S.emit("pool", e, semmap)

        @block.sync
        def _(e):
            S.emit("sp", e, semmap)
    es.close()
    return nc, S


def _t5_onehot():
    import jax
    import jax.numpy as jnp
    with jax.default_device(jax.devices("cpu")[0]):
        rel = jnp.arange(-255, 128, dtype=jnp.int32)
        half = 16
        max_exact = 8
        ret = jnp.where(rel > 0, half, 0)
        n = jnp.abs(rel)
        nf = jnp.maximum(n, 1).astype(jnp.float32)
        large = max_exact + (jnp.log(nf / max_exact) / math.log(128 / max_exact) * (half - max_exact)).astype(jnp.int32)
        large = jnp.minimum(large, half - 1)
        bucket = np.asarray(ret + jnp.where(n < max_exact, n, large))
    oh = np.zeros((32, 383), np.float32)
    oh[bucket, np.arange(383)] = 1.0
    return oh


def chan_major(v):
    return np.ascontiguousarray(np.swapaxes(v.reshape(v.shape[:-1] + (8, 128)), -1, -2))


def prepare_inputs(cfg, x_prompt, x_sample, cache_k, cache_v, cache_kidx, state_lru, state_conv,
                   c_prompt, c_sample, w_ada, b_ada, g_norm1, g_norm2, w_in, conv_w, conv_b,
                   lru_wa, lru_ba, lru_wx, lru_bx, lru_lam, w_out, peer_wq, peer_keys,
                   peer_u, peer_v, rel_bias, g_final):
    NCT, NOT, PAST = cfg["NCT"], cfg["NOT"], cfg["PAST"]
    f = lambda a: np.ascontiguousarray(np.asarray(a, dtype=np.float32))
    shared = {
        "w_ada": f(w_ada[0]), "b_ada": f(b_ada[0]).reshape(1, -1),
        "g1T": f(g_norm1[0].reshape(16, 128).T), "g2T": f(g_norm2[0].reshape(16, 128).T),
        "w_in": f(w_in[0]),
        "conv_wT": f(np.transpose(conv_w[0].reshape(4, 8, 128), (2, 0, 1)).reshape(128, 32)),
        "conv_bT": f(conv_b[0].reshape(8, 128).T),
        "lru_waT": f(np.transpose(lru_wa[0], (1, 0, 2)).reshape(128, 1024)),
        "lru_wxT": f(np.transpose(lru_wx[0], (1, 0, 2)).reshape(128, 1024)),
        "lru_baT": f(lru_ba[0].reshape(8, 128).T), "lru_bxT": f(lru_bx[0].reshape(8, 128).T),
        "lru_lamT": f(lru_lam[0].reshape(8, 128).T),
        "w_out": f(w_out[0]), "peer_wq": f(peer_wq[0]),
        "peer_keysT": f(np.transpose(peer_keys[0], (0, 3, 1, 2)).reshape(128, 1024)),
        "peer_u": f(peer_u[0]), "peer_v": f(peer_v[0]),
        "rel_bias": f(rel_bias), "g_final": f(g_final).reshape(1, -1), "oh": _t5_onehot(),
    }
    maps = []
    HALF = NOT * 128
    for c in range(8):
        b, half = c // 2, c % 2
        m = dict(shared)
        m["x_ctx"] = f(x_prompt[b, 0:NCT * 128])
        m["x_own"] = f(x_prompt[b, half * HALF:(half + 1) * HALF])
        sl = slice(2 * c, 2 * c + 2)
        m["xs"] = f(x_sample[sl])
        m["ck"] = f(cache_k[0, sl].reshape(2, PAST, 1024))
        m["cv"] = f(cache_v[0, sl].reshape(2, PAST, 1024))
        m["cki"] = f(cache_kidx[0, sl])
        m["st_lru"] = f(chan_major(state_lru[0, sl]))
        m["st_conv"] = f(np.transpose(chan_major(state_conv[0, sl]), (0, 2, 3, 1)))
        c3 = np.stack([c_prompt[b], c_sample[2 * c], c_sample[2 * c + 1]])
        m["c3T"] = f(np.transpose(c3.reshape(3, 16, 128), (2, 1, 0)).reshape(128, 48))
        fl = np.zeros((128, 4), np.float32)
        fl[:, 0] = float(half)
        fl[:, 1] = 1.0 - float(half)
        fl[:, 2] = 0.0 if half else NEG
        m["flags"] = fl
        maps.append(m)
    return maps


def assemble(cfg, res):
    NOT, PAST = cfg["NOT"], cfg["PAST"]
    HALF = NOT * 128
    SEQ = 2 * HALF
    y_p = np.zeros((4, SEQ, D), np.float32)
    y_s = np.zeros((16, 64, D), np.float32)
    k_p = np.zeros((1, 4, SEQ, 8, 128), np.float32)
    v_p = np.zeros((1, 4, SEQ, 8, 128), np.float32)
    ki_p = np.zeros((1, 4, SEQ, 64), np.float32)
    lru_p = np.zeros((1, 4, 1024), np.float32)
    conv_p = np.zeros((1, 4, 3, 1024), np.float32)
    k_s = np.zeros((1, 16, 64, 8, 128), np.float32)
    v_s = np.zeros((1, 16, 64, 8, 128), np.float32)
    ki_s = np.zeros((1, 16, 64, 64), np.float32)
    lru_s = np.zeros((1, 16, 1024), np.float32)
    conv_s = np.zeros((1, 16, 3, 1024), np.float32)
    for c in range(8):
        r = res[c]
        b, half = c // 2, c % 2
        sl = slice(half * HALF, (half + 1) * HALF)
        y_p[b, sl] = r["y_own"]
        k_p[0, b, sl] = r["k_own"].reshape(HALF, 8, 128)
        v_p[0, b, sl] = r["v_own"].reshape(HALF, 8, 128)
        ki_p[0, b, sl] = r["ki_own"]
        if half == 1:
            lru_p[0, b] = r["lru_p"].T.reshape(1024)
            conv_p[0, b] = np.transpose(r["conv_p"], (2, 1, 0)).reshape(3, 1024)
        for i in range(2):
            sbi = 2 * c + i
            y_s[sbi] = r["ys"][i]
            k_s[0, sbi] = r["ks"][i].reshape(64, 8, 128)
            v_s[0, sbi] = r["vs"][i].reshape(64, 8, 128)
            ki_s[0, sbi] = r["kis"][i]
            lru_s[0, sbi] = r["lru_s"][i].T.reshape(1024)
            conv_s[0, sbi] = np.transpose(r["conv_s"][i], (2, 1, 0)).reshape(3, 1024)
    return (y_p, y_s, k_p, v_p, ki_p, lru_p, conv_p, k_s, v_s, ki_s, lru_s, conv_s)


_CACHE = {}


def kernel(**inputs):
    cfg = make_cfg(True)
    if "nc" not in _CACHE:
        _CACHE["nc"] = build(cfg)[0]
    nc = _CACHE["nc"]
    maps = prepare_inputs(cfg, **{k: np.asarray(v) for k, v in inputs.items()})
    res = run_bass_kernel_spmd(nc, maps, core_ids=list(range(8)))
    return assemble(cfg, res.results)
```
